# Optimizing a Trainium2 kernel written in Bass

```python
import jax, jax.numpy as jnp
from jax import lax
import numpy as np

D_MODEL = 1024
BATCH = 8
SEQ = 2048
DEPTH = 4

N_MIXERS = 2
MIX_HEADS = 6
HEAD_DIM = 128
MIX_WIDTH = MIX_HEADS * HEAD_DIM
N_XATTN_HEADS = 4
XATTN_HEAD_DIM = 64
XATTN_WIDTH = N_XATTN_HEADS * XATTN_HEAD_DIM
N_MEM = 256
IN_WIDTH = 4 * MIX_WIDTH + XATTN_WIDTH
CAT_WIDTH = MIX_WIDTH + XATTN_WIDTH
D_FF = -(-(8 * D_MODEL) // (3 * 256)) * 256
N_HGRN_LAYERS = (DEPTH + N_MIXERS - 1) // N_MIXERS
N_RET_LAYERS = DEPTH - N_HGRN_LAYERS
HGRN_CHUNK = 32
RET_CHUNK = 64
ROPE_BASE = 10000.0
EPS = 1e-6
EXP_CLAMP = 30.0

kernel_name = "hgrn2_retention_interleaved_memxattn"


def rmsnorm(x, w):
    xf = x.astype(jnp.float32)
    y = xf * lax.rsqrt(jnp.mean(xf * xf, axis=-1, keepdims=True) + EPS)
    return (y * w.astype(jnp.float32)).astype(x.dtype)


def to_heads(t, h):
    b, s, _ = t.shape
    return t.reshape(b, s, h, -1)


def to_chunks(t, c):
    b, s, h, d = t.shape
    return t.reshape(b, s // c, c, h, d).transpose(0, 3, 1, 2, 4)


def from_chunks(t):
    b, h, n, c, d = t.shape
    return t.transpose(0, 2, 3, 1, 4).reshape(b, n * c, h, d)


def rope(t, positions):
    half = t.shape[-1] // 2
    inv_freq = ROPE_BASE ** (-jnp.linspace(0.0, 1.0, half, dtype=jnp.float32))
    ang = positions.astype(jnp.float32)[:, :, None] * inv_freq
    cos, sin = jnp.cos(ang)[:, :, None, :], jnp.sin(ang)[:, :, None, :]
    t1, t2 = t[..., :half].astype(jnp.float32), t[..., half:].astype(jnp.float32)
    return jnp.concatenate([t1 * cos - t2 * sin, t1 * sin + t2 * cos], axis=-1)


def hgrn2_chunked(q, k, v, g):
    c = HGRN_CHUNK
    q, k, v, g = (to_chunks(t, c) for t in (q, k, v, g))
    q, k = q.astype(jnp.float32), k.astype(jnp.float32)
    b = jnp.cumsum(g.astype(jnp.float32), axis=3)
    ref = b[:, :, :, c // 2 - 1:c // 2, :]
    b_last = b[:, :, :, -1:, :]
    scores = jnp.einsum('bhnck,bhnsk->bhncs', q * jnp.exp(b - ref), k * jnp.exp(ref - b))
    causal = jnp.tril(jnp.ones((c, c), dtype=bool))
    scores = jnp.where(causal, scores, 0.0)
    o_intra = jnp.einsum('bhncs,bhnsv->bhncv', scores, v)
    q_out = jnp.moveaxis(q * jnp.exp(b), 2, 0)
    k_st = jnp.moveaxis(k * jnp.exp(b_last - b), 2, 0)
    v_c = jnp.moveaxis(v, 2, 0)
    dec = jnp.moveaxis(jnp.exp(b_last[:, :, :, 0, :]), 2, 0)

    def step(s, xs):
        qo, ks, vc, dc = xs
        o = jnp.einsum('bhck,bhkv->bhcv', qo, s)
        s = s * dc[..., None] + jnp.einsum('bhck,bhcv->bhkv', ks, vc)
        return s, o

    bsz, h, _, _, dk = q.shape
    s0 = jnp.zeros((bsz, h, dk, v.shape[-1]), jnp.float32)
    _, o_inter = lax.scan(step, s0, (q_out, k_st, v_c, dec))
    return from_chunks(o_intra + jnp.moveaxis(o_inter, 0, 2))


def retention_chunked(q, k, v, log_gamma):
    c = RET_CHUNK
    q, k, v = (to_chunks(t, c) for t in (q, k, v))
    pos = jnp.arange(c, dtype=jnp.float32)
    lg = log_gamma[:, None]
    rel = pos[:, None] - pos[None, :]
    decay_mask = jnp.where(rel >= 0, jnp.exp(lg[:, :, None] * jnp.maximum(rel, 0.0)), 0.0)
    scores = jnp.einsum('bhncd,bhnsd->bhncs', q, k) * decay_mask[None, :, None]
    o_intra = jnp.einsum('bhncs,bhnsv->bhncv', scores, v)
    q_dec = jnp.exp(lg * (pos + 1.0))[None, :, :, None]
    k_dec = jnp.exp(lg * (c - 1.0 - pos))[None, :, :, None]
    chunk_dec = jnp.exp(log_gamma * c)[None, :, None, None]

    def step(s, xs):
        qc, kc, vc = xs
        o = jnp.einsum('bhcd,bhdv->bhcv', qc, s) * q_dec
        s = s * chunk_dec + jnp.einsum('bhcd,bhcv->bhdv', kc * k_dec, vc)
        return s, o

    bsz, h, _, _, dk = q.shape
    s0 = jnp.zeros((bsz, h, dk, v.shape[-1]), jnp.float32)
    xs = (jnp.moveaxis(q, 2, 0), jnp.moveaxis(k, 2, 0), jnp.moveaxis(v, 2, 0))
    _, o_inter = lax.scan(step, s0, xs)
    return from_chunks(o_intra + jnp.moveaxis(o_inter, 0, 2))


def memory_cross_attention(xq, mem_k, mem_v):
    s = jnp.einsum('bthd,bmhd->bhtm', xq, mem_k).astype(jnp.float32) * (XATTN_HEAD_DIM ** -0.5)
    p = jax.nn.softmax(s, axis=-1)
    return jnp.einsum('bhtm,bmhd->bthd', p.astype(mem_v.dtype), mem_v)


def setup_inputs(seed: int = 0) -> dict:
    key = jax.random.key(seed)
    ks = jax.random.split(key, 20)
    f32 = jnp.float32

    def w(k, shape, fan_in):
        return jax.random.normal(k, shape, f32) * (fan_in ** -0.5)

    def gain(k, shape):
        return 1.0 + 0.02 * jax.random.normal(k, shape, f32)

    offset = jax.random.randint(ks[2], (BATCH, 1), 0, 1024, dtype=jnp.int32)
    positions = offset + jnp.arange(SEQ, dtype=jnp.int32)[None, :]
    return {
        "x": jax.random.normal(ks[0], (BATCH, SEQ, D_MODEL), f32),
        "mem": jax.random.normal(ks[1], (BATCH, N_MEM, D_MODEL), f32),
        "positions": positions,
        "norm_mix": gain(ks[3], (DEPTH, D_MODEL)),
        "w_in": w(ks[4], (DEPTH, D_MODEL, IN_WIDTH), D_MODEL),
        "w_out": w(ks[5], (DEPTH, CAT_WIDTH, D_MODEL), CAT_WIDTH),
        "norm_mem": gain(ks[6], (DEPTH, D_MODEL)),
        "w_mem_kv": w(ks[7], (DEPTH, D_MODEL, 2 * XATTN_WIDTH), D_MODEL),
        "hgrn_lb_logits": 0.1 * jax.random.normal(ks[8], (N_HGRN_LAYERS, MIX_WIDTH), f32),
        "hgrn_out_norm": gain(ks[9], (N_HGRN_LAYERS, MIX_WIDTH)),
        "ret_out_norm": gain(ks[10], (N_RET_LAYERS, MIX_HEADS, HEAD_DIM)),
        "norm_ffn": gain(ks[11], (DEPTH, D_MODEL)),
        "w_ffn_in": w(ks[12], (DEPTH, D_MODEL, 2 * D_FF), D_MODEL),
        "w_ffn_out": w(ks[13], (DEPTH, D_FF, D_MODEL), D_FF),
        "norm_final": gain(ks[14], (D_MODEL,)),
    }


def reference(x, mem, positions, norm_mix, w_in, w_out, norm_mem, w_mem_kv, hgrn_lb_logits,
              hgrn_out_norm, ret_out_norm, norm_ffn, w_ffn_in, w_ffn_out, norm_final):
    bsz, seq, _ = x.shape
    p_lb = jax.nn.softmax(hgrn_lb_logits.astype(jnp.float32), axis=0)
    lower_bounds = jnp.cumsum(p_lb, axis=0) - p_lb[0]
    log_gamma = jnp.log(1.0 - 2.0 ** (-5.0 - jnp.arange(MIX_HEADS, dtype=jnp.float32)))

    for i in range(DEPTH):
        h = rmsnorm(x, norm_mix[i])
        z = h @ w_in[i]
        za = z[..., 0 * MIX_WIDTH:1 * MIX_WIDTH]
        zb = z[..., 1 * MIX_WIDTH:2 * MIX_WIDTH]
        zc = z[..., 2 * MIX_WIDTH:3 * MIX_WIDTH]
        zg = z[..., 3 * MIX_WIDTH:4 * MIX_WIDTH]
        zx = z[..., 4 * MIX_WIDTH:]

        if i % N_MIXERS == 0:
            j = i // N_MIXERS
            lb = lower_bounds[j]
            fr = zb.astype(jnp.float32)
            g = jax.nn.log_sigmoid(fr) + jnp.log1p(lb * jnp.exp(jnp.minimum(-fr, EXP_CLAMP)))
            k_in = (1.0 - lb) * jax.nn.sigmoid(-fr)
            o = hgrn2_chunked(to_heads(jax.nn.silu(za), MIX_HEADS), to_heads(k_in, MIX_HEADS),
                              to_heads(zc, MIX_HEADS), to_heads(g, MIX_HEADS))
            o = o.reshape(bsz, seq, MIX_WIDTH)
            mix = rmsnorm(o, hgrn_out_norm[j]) * jax.nn.sigmoid(zg.astype(jnp.float32))
        else:
            j = i // N_MIXERS
            q = rope(to_heads(za, MIX_HEADS), positions)
            k = rope(to_heads(zb, MIX_HEADS), positions) * (HEAD_DIM ** -0.5)
            v = to_heads(zc, MIX_HEADS).astype(jnp.float32)
            o = retention_chunked(q, k, v, log_gamma)
            o = rmsnorm(o, ret_out_norm[j]).reshape(bsz, seq, MIX_WIDTH)
            mix = o * jax.nn.silu(zg.astype(jnp.float32))

        memh = rmsnorm(mem, norm_mem[i])
        mkv = memh @ w_mem_kv[i]
        mem_k = to_heads(mkv[..., :XATTN_WIDTH], N_XATTN_HEADS)
        mem_v = to_heads(mkv[..., XATTN_WIDTH:], N_XATTN_HEADS)
        xo = memory_cross_attention(to_heads(zx, N_XATTN_HEADS), mem_k, mem_v)
        xo = xo.reshape(bsz, seq, XATTN_WIDTH)

        cat = jnp.concatenate([mix.astype(x.dtype), xo.astype(x.dtype)], axis=-1)
        x = x + cat @ w_out[i]

        hf = rmsnorm(x, norm_ffn[i])
        gu = hf @ w_ffn_in[i]
        x = x + (jax.nn.silu(gu[..., :D_FF]) * gu[..., D_FF:]) @ w_ffn_out[i]

    return rmsnorm(x, norm_final)
```

```python
import numpy as np
from contextlib import ExitStack
import concourse.bass as bass
import concourse.mybir as mybir
from concourse.bass_utils import run_bass_kernel_spmd

F32 = mybir.dt.float32
BF16 = mybir.dt.bfloat16
I32 = mybir.dt.int32
AF = mybir.ActivationFunctionType
ALU = mybir.AluOpType

D = 1024
T = 2048
NL = 4
KC = 8
MIXW = 768
INW = 3328
DFF = 2816
FC = 22
NMEM = 256
EPS = 1e-6


class Buf:
    __slots__ = ("name", "w", "r", "kids")

    def __init__(self, name="", kids=()):
        self.name = name
        self.w = None
        self.r = []
        self.kids = tuple(kids)


def _expand(bufs):
    out = []
    for b in bufs:
        out.append(b)
        out.extend(b.kids)
    return out


class Op:
    __slots__ = ("eng", "fn", "deps", "sig", "signal", "dma", "idx")


class Sched:
    ENGS = ("pe", "act", "dve", "pool", "sp")

    def __init__(self, nc):
        self.nc = nc
        self.ops = {e: [] for e in self.ENGS}
        self.n = 0
        self.phase = 0
        self.dma_sems = {}
        self.eng_sems = {}
        self._sem_ctx = []
        self.pending_dma = []

    def _new_sem(self, name):
        cm = self.nc.semaphore(name)
        s = cm.__enter__()
        self._sem_ctx.append(cm)
        return s

    def close(self):
        for cm in reversed(self._sem_ctx):
            cm.__exit__(None, None, None)

    def _deps(self, op, reads, writes):
        reads = _expand(reads)
        writes = _expand(writes)
        deps = []
        for b in reads:
            if b.w is not None:
                deps.append(b.w)
        for b in writes:
            if b.w is not None:
                deps.append(b.w)
            deps.extend(b.r)
        for b in reads:
            b.r.append(op)
        for b in writes:
            b.w = op
            b.r = []
        out = []
        seen = set()
        for d in deps:
            if d is op or id(d) in seen:
                continue
            seen.add(id(d))
            if d.eng == "pe" and op.eng == "pe" and not d.dma and not op.dma:
                continue
            out.append(d)
        return out

    def op(self, eng, fn, reads=(), writes=(), extra=()):
        o = Op()
        o.eng = eng
        o.fn = fn
        o.dma = False
        o.sig = False
        o.idx = self.n
        self.n += 1
        o.deps = self._deps(o, reads, writes) + list(extra)
        for d in o.deps:
            d.sig = True
        o.signal = (eng, self.phase)
        self.ops[eng].append(o)
        return o

    def dma(self, queue, fn, reads=(), writes=(), semkey=None):
        o = Op()
        o.eng = queue
        o.fn = fn
        o.dma = True
        o.sig = True
        o.idx = self.n
        self.n += 1
        o.deps = self._deps(o, reads, writes)
        for d in o.deps:
            d.sig = True
        if semkey is None:
            semkey = ("dma", id(writes[0]))
        if semkey not in self.dma_sems:
            self.dma_sems[semkey] = [self._new_sem("d%d" % len(self.dma_sems)), 0]
        ent = self.dma_sems[semkey]
        ent[1] += 16
        o.signal = (ent[0], ent[1])
        self.ops[queue].append(o)
        self.pending_dma.append(o)
        return o

    def barrier(self):
        lasts = []
        for e in self.ENGS:
            for o in reversed(self.ops[e]):
                if not o.dma:
                    lasts.append(o)
                    break
        lasts += self.pending_dma
        self.pending_dma = []
        for e in self.ENGS:
            self.op(e, lambda eng: eng.nop(), extra=[d for d in lasts])
        self.phase += 1

    def finalize(self):
        counters = {}
        for e in self.ENGS:
            for o in self.ops[e]:
                if o.dma:
                    continue
                if o.sig:
                    key = o.signal
                    if key not in self.eng_sems:
                        self.eng_sems[key] = self._new_sem("e_%s_%d" % key)
                    counters[key] = counters.get(key, 0) + 1
                    o.signal = (self.eng_sems[key], counters[key])
                else:
                    o.signal = None

    def emit(self, ename, eng):
        waited = {}
        for o in self.ops[ename]:
            best = {}
            for d in o.deps:
                sem, val = d.signal
                k = id(sem)
                if waited.get(k, 0) < val:
                    waited[k] = val
                    best[k] = (sem, val)
            ws = list(best.values())
            for sem, val in ws[1:]:
                eng.wait_ge(sem, val)
            ins = o.fn(eng)
            if ws:
                ins._wait_ge(ws[0][0], ws[0][1])
            if o.dma:
                ins.then_inc(o.signal[0], 16)
            elif o.sig:
                ins.then_inc(o.signal[0], 1)

    def run_block(self, final_waits=()):
        self.finalize()
        nc = self.nc
        sch = self
        with nc.Block() as block:
            @block.tensor
            def _(e):
                sch.emit("pe", e)

            @block.scalar
            def _(e):
                sch.emit("act", e)

            @block.vector
            def _(e):
                sch.emit("dve", e)

            @block.gpsimd
            def _(e):
                sch.emit("pool", e)

            @block.sync
            def _(e):
                sch.emit("sp", e)
                best = {}
                for o in final_waits:
                    sem, val = o.signal
                    if id(sem) not in best or best[id(sem)][1] < val:
                        best[id(sem)] = (sem, val)
                for sem, val in best.values():
                    e.wait_ge(sem, val)


class Rot:
    def __init__(self, items):
        self.items = items
        self.i = 0

    def next(self):
        it = self.items[self.i % len(self.items)]
        self.i += 1
        return it


GF = 6
GROUPS = [(0, 6), (6, 6), (12, 6), (18, 4)]
GAMMA_LOG = [float(np.log(np.float32(1.0) - np.float32(2.0) ** np.float32(-5.0 - h))) for h in range(6)]
E30 = float(np.exp(30.0))
PI = float(np.pi)


def build(n_layers=NL, do_mix=True, do_ffn=True, kinds=None):
    nc = bass.Bass("TRN2", target_bir_lowering=False)

    def din(name, shape, dt=F32):
        return nc.dram_tensor(name, shape, dt, kind="ExternalInput").ap()

    x_d = din("x", [T, D])
    mem_d = din("mem", [NMEM, D])
    pos_d = din("positions", [16, 128], I32)
    norm_mix_d = din("norm_mix", [NL, D])
    w_in_d = din("w_in", [NL, D, INW])
    w_out_d = din("w_out", [NL, D, D])
    norm_mem_d = din("norm_mem", [NL, D])
    w_kv_d = din("w_mem_kv", [NL, D, 512])
    lb_d = din("hgrn_lb_logits", [2, MIXW])
    hgn_d = din("hgrn_out_norm", [2, MIXW])
    rtn_d = din("ret_out_norm", [2, MIXW])
    norm_ffn_d = din("norm_ffn", [NL, D])
    w_fi_d = din("w_ffn_in", [NL, D, 2 * DFF])
    w_fo_d = din("w_ffn_out", [NL, DFF, D])
    norm_fin_d = din("norm_final", [1, D])
    consts_d = din("consts", [128, 1024])
    out_d = nc.dram_tensor("out", [T, D], F32, kind="ExternalOutput").ap()

    es = ExitStack()

    def sb(name, shape, dt):
        return es.enter_context(nc.sbuf_tensor(name, shape, dt))

    S = Sched(nc)

    xT = sb("xT", [128, KC, T], F32)
    XB = [Buf("x%d" % i) for i in range(16)]
    U1 = sb("U1", [128, 34816], BF16)
    U2W = 12320
    U2 = sb("U2", [128, U2W], F32)
    U2b = U2[:].bitcast(BF16)
    cst = sb("cst", [128, 1024], F32)
    identF = cst[:, 0:128]
    M1c, M2c, M4c, BDc, CAUc = (cst[:, 128 * i:128 * (i + 1)] for i in range(1, 6))
    sqc = cst[:, 768:774]
    skc = cst[:, 774:780]
    invf = cst[:, 832:896]
    identB = sb("identB", [128, 128], BF16)
    onesB = sb("onesB", [128, 128], BF16)
    vcol = sb("vcol", [128, 128], F32)
    KIND = sb("KIND", [128, 2048], F32)
    KINDB = Buf("KIND")
    xatt = sb("xatt", [128, 2304], BF16)
    KTpad = xatt[:, 0:1024].rearrange("p (h m) -> p h m", h=4)
    Vpad = xatt[:, 1024:2048].rearrange("p (t h c) -> p t h c", t=2, h=4)
    onespad = xatt[:, 2048:2304].rearrange("p (a c) -> p a c", a=2)
    XAB = Buf("xatt")
    OPB = Buf("onespad")
    CB = Buf("consts")
    VC = Buf("vcol")
    sqt = sb("sqt", [128, 2, 512], BF16)
    sq_rot = Rot([(sqt[:, i, :], Buf("sq%d" % i)) for i in range(2)])
    rstd_t = sb("rstd", [128, 2, 512], F32)
    rstd_rot = Rot([(rstd_t[:, i, :], Buf("rstd%d" % i)) for i in range(2)])

    PSA = es.enter_context(nc.psum_tensor("psa", [128, 4096], F32))
    PSAb = PSA[:].bitcast(BF16)
    PB = [Buf("ps%d" % i) for i in range(8)]
    ps_all = Rot([(PSA[:, 512 * i:512 * (i + 1)], PB[i]) for i in range(8)])

    w_in_sb = U1[:, 0:KC * INW].rearrange("p (k n) -> p k n", k=KC)
    w_out_sb = U1[:, KC * INW:KC * INW + KC * D].rearrange("p (k n) -> p k n", k=KC)
    WIN_B = [Buf("win%d" % i) for i in range(4)]
    WOUT_B = Buf("wout")
    hT = U1[:, 0:KC * T].rearrange("p (k t) -> p k t", k=KC)
    HB = [Buf("h%d" % i) for i in range(4)]
    aT = U1[:, KC * T:KC * T + GF * T].rearrange("p (f t) -> p f t", f=GF)
    AB = [[Buf("a%d_%d" % (f, b)) for b in range(4)] for f in range(GF)]
    wo_sb = U2b[:, 0:GF * D].rearrange("p (f n) -> p f n", f=GF)
    WO_B = Buf("wo")
    NWI = 3
    wi_sb = [U2b[:, GF * D + i * 2048:GF * D + (i + 1) * 2048].rearrange("p (g k n) -> p g k n", g=2, k=KC) for i in range(NWI)]
    wi_rot = Rot([(wi_sb[i], (Buf("wig%d" % i), Buf("wiu%d" % i))) for i in range(NWI)])
    st0 = GF * D + NWI * 2048
    silu_rot = Rot([(U2b[:, st0 + i * 512:st0 + (i + 1) * 512], Buf("sl%d" % i)) for i in range(4)])
    stage_rot = Rot([(U2[:, i * 1024:(i + 1) * 1024], Buf("stg%d" % i)) for i in range(2)])
    yT_rot = Rot([(U2[:, 2048 + i * 1024:2048 + (i + 1) * 1024].rearrange("p (k t) -> p k t", k=KC), Buf("yT%d" % i)) for i in range(2)])
    vrows = U2[:, 4096:4224]
    VR = Buf("vrows")

    _o = [0]

    def u2f(n):
        a = U2[:, _o[0]:_o[0] + n]
        _o[0] += n
        return a

    def u2b(n):
        a = U2b[:, 2 * _o[0]:2 * _o[0] + n]
        _o[0] += (n + 1) // 2
        return a
    hTt = u2b(1024).rearrange("p (k t) -> p k t", k=KC); HTB = Buf("hTt")
    catT = u2b(1024).rearrange("p (k t) -> p k t", k=KC); CATB = Buf("catT")
    tA = u2f(768); TAB = Buf("tA")
    tB = u2f(768); TBB = Buf("tB")
    tC = u2f(768); TCB = Buf("tC")
    tE = u2f(768); TEB = Buf("tE")
    tO = u2f(768); TOB = Buf("tO")
    tG = u2f(768); TGB = Buf("tG")
    tR = u2f(768); TRB = Buf("tR")
    Qt = u2b(768); QTB_ = Buf("Qt")
    Kt = u2b(768); KTB_ = Buf("Kt")
    Qot = u2b(768); QOTB_ = Buf("Qot")
    Kst = u2b(768); KSTB = Buf("Kst")
    Vt = u2b(768); VTB = Buf("Vt")
    QTf = u2b(768); QTFB = Buf("QTf")
    KTf = u2b(768); KTFB = Buf("KTf")
    QoTf = u2b(768); QOTFB = Buf("QoTf")
    Pm = u2b(768); PMB = Buf("Pm")
    sqo = u2b(768); SQOB = Buf("sqo")
    S32H = [Buf("S32h%d" % h) for h in range(6)]
    SBFH = [Buf("Sbfh%d" % h) for h in range(6)]
    S32 = u2f(768); S32B = Buf("S32", kids=S32H)
    Sbf = u2b(768); SBFB = Buf("Sbf", kids=SBFH)
    dec = u2f(24); DECB = Buf("dec")
    qxT = u2b(256); QXB = Buf("qxT")
    expT = u2b(1024); EXB = Buf("expT")
    rsx = u2f(256); RSXB = Buf("rsx")
    setup_base = _o[0]
    assert _o[0] <= U2W, _o[0]
    memst = [U2[:, 2048 + i * 1024:2048 + (i + 1) * 1024] for i in range(2)]
    MSB = [Buf("memst%d" % i) for i in range(2)]
    memh = U2b[:, 2 * 4096:2 * 4096 + 2048].rearrange("p (t n) -> p t n", t=2)
    MHB = Buf("memh")
    memhT = U2b[:, 2 * 5120:2 * 5120 + 2048].rearrange("p (k m) -> p k m", k=KC)
    MHTB = Buf("memhT")
    wkv = U2b[:, 2 * 6144:2 * 6144 + 4096].rearrange("p (k n) -> p k n", k=KC)
    WKVB = Buf("wkv")
    mscr = U2[:, 8192:8192 + 1024]
    MSCB = Buf("mscr")
    angt = U2[:, 9216:9216 + 1024].rearrange("p (t j) -> p t j", t=16)
    ANGB = Buf("ang")
    posr = U2[:, 10240:10240 + 128]
    posi = U2[:, 10368:10368 + 128].bitcast(I32)
    posf = U2[:, 10496:10496 + 16]
    POSB = Buf("pos")
    assert 10512 <= U2W

    R0 = PSA[:, 0:1024]; R0B = Buf("R0")
    R1 = PSA[:, 1024:2048]; R1B = Buf("R1")
    R2H = [Buf("R2h%d" % h) for h in range(6)]
    R3H = [Buf("R3h%d" % h) for h in range(6)]
    R2 = PSA[:, 2048:3072]; R2B = Buf("R2", kids=R2H)
    R3 = PSA[:, 3072:4096]; R3B = Buf("R3", kids=R3H)
    R0b = PSAb[:, 0:2048]; R1b = PSAb[:, 2048:4096]; R2b = PSAb[:, 4096:6144]; R3b = PSAb[:, 6144:8192]
    ALLPS = PB

    def rbufs(*rb):
        return list(rb)

    outs = []

    S.dma("sp", lambda e: e.dma_start(out=cst[:], in_=consts_d), writes=[CB])
    S.op("dve", lambda e: e.tensor_copy(out=identB[:], in_=identF), reads=[CB], writes=[CB])
    S.op("pool", lambda e: e.memset(onesB[:], 1.0), writes=[CB])
    S.op("pool", lambda e: e.memset(xatt[:], 0.0), writes=[XAB, OPB])
    S.op("pool", lambda e: e.memset(onespad[:, 0, 0:64], 1.0), writes=[OPB])
    S.op("pool", lambda e: e.memset(onespad[:, 1, 64:128], 1.0), writes=[OPB])
    S.op("pool", lambda e: e.memset(vrows, 0.0), writes=[VR])
    for i, (src, r0, n) in enumerate([(norm_mix_d, 0, 32), (norm_ffn_d, 32, 32), (norm_mem_d, 64, 32), (norm_fin_d, 96, 8), (hgn_d, 104, 12), (rtn_d, 116, 12)]):
        S.dma("sp", lambda e, src=src, r0=r0, n=n: e.dma_start(out=vrows[r0:r0 + n, :], in_=src.rearrange("l (k p) -> (l k) p", p=128)),
              writes=[VR], semkey="vr")
    S.op("pe", lambda e: e.transpose(out=PSA[:, 0:128], in_=vrows, identity=identF), reads=[VR, CB], writes=[PB[0]])
    S.op("dve", lambda e: e.tensor_copy(out=vcol[:], in_=PSA[:, 0:128]), reads=[PB[0]], writes=[VC])

    def col_norm_mix(l, k): return vcol[:, l * 8 + k:l * 8 + k + 1]
    def col_norm_ffn(l, k): return vcol[:, 32 + l * 8 + k:32 + l * 8 + k + 1]
    def col_norm_mem(l, k): return vcol[:, 64 + l * 8 + k:64 + l * 8 + k + 1]
    def col_norm_fin(k): return vcol[:, 96 + k:96 + k + 1]
    def col_hgn(j, h): return vcol[:, 104 + j * 6 + h:104 + j * 6 + h + 1]
    def col_rtn(j, h): return vcol[:, 116 + j * 6 + h:116 + j * 6 + h + 1]

    S.barrier()

    def load_transposed(src_d, ntile, dst, dst_bufs):
        for t in range(ntile):
            stg, stgb = stage_rot.next()
            S.dma("sp", lambda e, stg=stg, t=t: e.dma_start(out=stg, in_=src_d[t * 128:(t + 1) * 128, :]), writes=[stgb])
            for half in range(2):
                ps, pb = ps_all.next()
                for j in range(4):
                    k = half * 4 + j
                    S.op("pe", lambda e, ps=ps, stg=stg, j=j, k=k: e.transpose(out=ps[:, j * 128:(j + 1) * 128], in_=stg[:, k * 128:(k + 1) * 128], identity=identF),
                         reads=[stgb, CB], writes=[pb])
                if half == 0:
                    S.op("act", lambda e, ps=ps, half=half, t=t: e.copy(out=dst[:, half * 4:half * 4 + 4, t * 128:(t + 1) * 128],
                                                                       in_=ps.rearrange("p (k c) -> p k c", k=4)), reads=[pb], writes=[dst_bufs[t]])
                else:
                    S.op("dve", lambda e, ps=ps, half=half, t=t: e.tensor_copy(out=dst[:, half * 4:half * 4 + 4, t * 128:(t + 1) * 128],
                                                                              in_=ps.rearrange("p (k c) -> p k c", k=4)), reads=[pb], writes=[dst_bufs[t]])
    load_transposed(x_d, 16, xT, XB)

    def rms_stats_blk(src_fn, src_bufs, width, scale, psreg=None):
        ps, pb = psreg if psreg is not None else ps_all.next()
        for k in range(KC):
            sq, sqb = sq_rot.next()
            S.op("act", lambda e, sq=sq, k=k: e.activation(out=sq[:, 0:width], in_=src_fn(k), func=AF.Square), reads=src_bufs, writes=[sqb])
            S.op("pe", lambda e, ps=ps, sq=sq, k=k: e.matmul(ps[:, 0:width], lhsT=onesB[:], rhs=sq[:, 0:width], start=(k == 0), stop=(k == KC - 1)),
                 reads=[sqb, CB], writes=[pb])
        rs, rsb = rstd_rot.next()
        S.op("act", lambda e, ps=ps, rs=rs: e.activation(out=rs[:, 0:width], in_=ps[:, 0:width], func=AF.Ln, scale=scale, bias=EPS), reads=[pb], writes=[rsb])
        S.op("act", lambda e, rs=rs: e.activation(out=rs[:, 0:width], in_=rs[:, 0:width], func=AF.Exp, scale=-0.5), reads=[rsb], writes=[rsb])
        return rs, rsb

    def mm(out, lhsT, rhs, start, stop, reads, writes, **kw):
        S.op("pe", lambda e: e.matmul(out, lhsT=lhsT, rhs=rhs, start=start, stop=stop, **kw), reads=reads, writes=writes)

    def act(out, in_, func, reads, writes, **kw):
        S.op("act", lambda e: e.activation(out=out, in_=in_, func=func, **kw), reads=reads, writes=writes)

    def hv(ap):
        return ap.rearrange("p (h c) -> p h c", h=6)

    def mixer_layer(l, kind, j):
        for i in range(4):
            S.dma("pool", lambda e, i=i: e.dma_start(out=w_in_sb[:, 2 * i:2 * i + 2, :],
                                                    in_=w_in_d[l, 256 * i:256 * (i + 1), :].rearrange("(k p) n -> p k n", p=128)), writes=[WIN_B[i]])
        S.dma("pool", lambda e: e.dma_start(out=w_out_sb, in_=w_out_d[l].rearrange("(k p) n -> p k n", p=128)), writes=[WOUT_B])
        S.dma("pool", lambda e: e.dma_start(out=wkv, in_=w_kv_d[l].rearrange("(k p) n -> p k n", p=128)), writes=[WKVB])
        BIS = 99
        angf_ = angt.rearrange("p t j -> p (t j)")
        if BIS >= 1:
            S.dma("sp", lambda e: e.dma_start(out=angf_[0:1, :], in_=norm_mem_d[l:l + 1, :]), writes=[ANGB])
            for hh in range(2):
                S.op("pe", lambda e, hh=hh: e.matmul(R3[:, hh * 512:(hh + 1) * 512], lhsT=cst[0:1, 640:768], rhs=angf_[0:1, hh * 512:(hh + 1) * 512],
                                                   start=True, stop=True), reads=[ANGB, CB], writes=[R3B])
            S.op("dve", lambda e: e.tensor_copy(out=mscr, in_=R3), reads=[R3B], writes=[MSCB])
        for mt in range(2 if BIS >= 2 else 0):
            S.dma("sp", lambda e, mt=mt: e.dma_start(out=memst[mt], in_=mem_d[mt * 128:(mt + 1) * 128, :]), writes=[MSB[mt]])
            S.op("pool", lambda e, mt=mt: e.memset(posf[:, mt:mt + 1], 0.0), writes=[POSB])
            act(angf_, memst[mt], AF.Square, [MSB[mt], POSB], [ANGB, POSB], accum_out=posf[:, mt:mt + 1])
            act(posf[:, mt:mt + 1], posf[:, mt:mt + 1], AF.Sqrt, [POSB], [POSB], scale=1.0 / D, bias=EPS)
            S.op("dve", lambda e, mt=mt: e.reciprocal(out=posf[:, mt:mt + 1], in_=posf[:, mt:mt + 1]), reads=[POSB], writes=[POSB])
            S.op("dve", lambda e, mt=mt: e.scalar_tensor_tensor(out=memh[:, mt, :], in0=memst[mt], scalar=posf[:, mt:mt + 1], in1=mscr,
                                                               op0=ALU.mult, op1=ALU.mult), reads=[MSB[mt], POSB, MSCB], writes=[MHB])
            if BIS >= 3:
                for k in range(KC):
                    S.op("pe", lambda e, mt=mt, k=k: e.transpose(out=R0b[:, mt * 1024 + k * 128:mt * 1024 + (k + 1) * 128], in_=memh[:, mt, k * 128:(k + 1) * 128],
                                                                identity=identB[:]), reads=[MHB, CB], writes=[R0B])
                S.op("act", lambda e, mt=mt: e.copy(out=memhT[:, :, mt * 128:(mt + 1) * 128], in_=R0b[:, mt * 1024:(mt + 1) * 1024].rearrange("p (k c) -> p k c", k=KC)),
                     reads=[R0B], writes=[MHTB])
        for c in range(2 if BIS >= 4 else 0):
            for k in range(KC):
                mm(R1[:, c * 256:(c + 1) * 256], wkv[:, k, c * 128:(c + 1) * 128], memhT[:, k, :], k == 0, k == KC - 1, [WKVB, MHTB], [R1B])
        if BIS >= 4:
            for h in range(4):
                c, po = h // 2, 64 * (h % 2)
                S.op("act", lambda e, h=h, c=c, po=po: e.copy(out=KTpad[po:po + 64, h, :], in_=R1[po:po + 64, c * 256:(c + 1) * 256]), reads=[R1B], writes=[XAB])
        for mt in range(2 if BIS >= 5 else 0):
            for k in range(KC):
                mm(R2[:, mt * 256:(mt + 1) * 256], memhT[:, k, mt * 128:(mt + 1) * 128], wkv[:, k, 256:512], k == 0, k == KC - 1, [WKVB, MHTB], [R2B])
        for mt in range(2 if BIS >= 5 else 0):
            for h in range(4):
                S.op("dve", lambda e, mt=mt, h=h: e.tensor_copy(out=Vpad[:, mt, h, (h % 2) * 64:(h % 2) * 64 + 64], in_=R2[:, mt * 256 + h * 64:mt * 256 + (h + 1) * 64]),
                     reads=[R2B], writes=[XAB])
        if kind == "ret":
            S.dma("sp", lambda e: e.dma_start(out=posi[0:16, :], in_=pos_d), writes=[POSB])
            S.op("dve", lambda e: e.tensor_copy(out=posr[0:16, :], in_=posi[0:16, :]), reads=[POSB], writes=[POSB])
            S.op("pe", lambda e: e.transpose(out=R3[:, 0:16], in_=posr[0:16, :], identity=identF[0:16, 0:16]), reads=[POSB, CB], writes=[R3B])
            S.op("dve", lambda e: e.tensor_copy(out=posf, in_=R3[:, 0:16]), reads=[R3B], writes=[POSB])
            for t in range(16):
                S.op("dve", lambda e, t=t: e.tensor_scalar(out=angt[:, t, :], in0=invf, scalar1=posf[:, t:t + 1], scalar2=None, op0=ALU.mult), reads=[POSB, CB], writes=[ANGB])
            angf = angt.rearrange("p t j -> p (t j)")
            scr2 = U2[:, 1024:2048]
            scr2i = scr2.bitcast(I32)
            for (off, dst0) in ((0.5 * PI, 0), (0.0, 1024)):
                S.op("dve", lambda e, off=off: e.tensor_scalar(out=mscr, in0=angf, scalar1=off, scalar2=None, op0=ALU.add), reads=[ANGB], writes=[MSCB])
                S.op("dve", lambda e: e.tensor_scalar(out=scr2, in0=mscr, scalar1=1.0 / (2 * PI), scalar2=None, op0=ALU.mult), reads=[MSCB], writes=[TAB, TBB])
                S.op("dve", lambda e: e.tensor_copy(out=scr2i, in_=scr2), reads=[TAB, TBB], writes=[TAB, TBB])
                S.op("dve", lambda e: e.tensor_copy(out=scr2, in_=scr2i), reads=[TAB, TBB], writes=[TAB, TBB])
                S.op("dve", lambda e: e.scalar_tensor_tensor(out=mscr, in0=scr2, scalar=-2 * PI, in1=mscr, op0=ALU.mult, op1=ALU.add), reads=[TAB, TBB, MSCB], writes=[MSCB])
                S.op("dve", lambda e: e.tensor_scalar(out=scr2, in0=mscr, scalar1=PI, scalar2=2 * PI, op0=ALU.is_gt, op1=ALU.mult), reads=[MSCB], writes=[TAB, TBB])
                S.op("dve", lambda e: e.tensor_tensor(out=mscr, in0=mscr, in1=scr2, op=ALU.subtract), reads=[TAB, TBB, MSCB], writes=[MSCB])
                S.op("dve", lambda e: e.tensor_scalar(out=mscr, in0=mscr, scalar1=-PI, scalar2=PI, op0=ALU.max, op1=ALU.min), reads=[MSCB], writes=[MSCB])
                act(KIND[:, dst0:dst0 + 1024], mscr, AF.Sin, [MSCB], [KINDB])
        elif kind == "hgrn":
            lb_bc = KIND[:, 0:768]
            nom_bc = KIND[:, 768:1536]
            if j == 0:
                S.op("pool", lambda e: e.memset(lb_bc, 0.0), writes=[KINDB])
                S.op("pool", lambda e: e.memset(nom_bc, -1.0), writes=[KINDB])
            else:
                S.dma("sp", lambda e: e.dma_start(out=mscr[:, 0:768], in_=lb_d[0:1, :].partition_broadcast(128)), writes=[MSCB])
                S.dma("sp", lambda e: e.dma_start(out=angt.rearrange("p t j -> p (t j)")[:, 0:768], in_=lb_d[1:2, :].partition_broadcast(128)), writes=[ANGB])
                S.op("dve", lambda e: e.tensor_tensor(out=mscr[:, 0:768], in0=mscr[:, 0:768], in1=angt.rearrange("p t j -> p (t j)")[:, 0:768], op=ALU.subtract),
                     reads=[MSCB, ANGB], writes=[MSCB])
                act(mscr[:, 0:768], mscr[:, 0:768], AF.Exp, [MSCB], [MSCB])
                S.op("dve", lambda e: e.tensor_scalar(out=mscr[:, 0:768], in0=mscr[:, 0:768], scalar1=1.0, scalar2=None, op0=ALU.add), reads=[MSCB], writes=[MSCB])
                S.op("dve", lambda e: e.reciprocal(out=lb_bc, in_=mscr[:, 0:768]), reads=[MSCB], writes=[KINDB])
                S.op("dve", lambda e: e.tensor_scalar(out=nom_bc, in0=lb_bc, scalar1=-1.0, scalar2=None, op0=ALU.add), reads=[KINDB], writes=[KINDB])
        S.barrier()
        S.op("pool", lambda e: e.memset(S32, 0.0), writes=[S32B])
        S.op("pool", lambda e: e.memset(Sbf, 0.0), writes=[SBFB])

        blk_rs = {}

        def head1(t):
            ts = slice(t * 128, (t + 1) * 128)
            if t % 4 == 0:
                b0 = t
                blk_rs[t // 4] = rms_stats_blk(lambda k, b0=b0: xT[:, k, b0 * 128:(b0 + 4) * 128], XB[b0:b0 + 4], 512, 1.0 / D, psreg=(R3[:, 0:512], R3B))
            rs, rsb = blk_rs[t // 4]
            tt = t % 4
            for k in range(KC):
                S.op("dve", lambda e, k=k, ts=ts, rs=rs, tt=tt: e.scalar_tensor_tensor(out=hTt[:, k, :], in0=xT[:, k, ts], scalar=col_norm_mix(l, k),
                                                                                   in1=rs[:, tt * 128:(tt + 1) * 128], op0=ALU.mult, op1=ALU.mult),
                     reads=[XB[t], rsb, VC], writes=[HTB])
            for c in range(8):
                c0 = 2304 + c * 128
                for k in range(KC):
                    mm(R3[:, c * 128:(c + 1) * 128], w_in_sb[:, k, c0:c0 + 128], hTt[:, k, :], k == 0, k == KC - 1, [WIN_B[k // 2], HTB], [R3B])

        for t in range(16):
            ts = slice(t * 128, (t + 1) * 128)
            if t == 0:
                head1(0)
            if kind == "hgrn":
                act(tG, R3[:, 0:768], AF.Tanh, [R3B], [TGB], scale=0.5)
            else:
                act(tG, R3[:, 0:768], AF.Silu, [R3B], [TGB])
            S.op("act", lambda e: e.copy(out=qxT, in_=R3[:, 768:1024]), reads=[R3B], writes=[QXB])
            for h in range(4):
                c = h // 2
                for mt in range(2):
                    mm(R3[:, (h * 2 + mt) * 128:(h * 2 + mt + 1) * 128], KTpad[:, h, mt * 128:(mt + 1) * 128], qxT[:, c * 128:(c + 1) * 128],
                       True, True, [XAB, QXB], [R3B])
            for (R, RB_, c0) in ((R0, R0B, 0), (R1, R1B, 768), (R2, R2B, 1536)):
                for (a, n) in ((0, 512), (512, 256)):
                    for k in range(KC):
                        mm(R[:, a:a + n], hTt[:, k, :], w_in_sb[:, k, c0 + a:c0 + a + n], k == 0, k == KC - 1, [WIN_B[k // 2], HTB], [RB_])
            if kind == "hgrn":
                act(tC, R0[:, 0:768], AF.Silu, [R0B], [TCB])
            act(expT, R3[:, 0:1024], AF.Exp, [R3B], [EXB], scale=0.125)
            for c in range(2):
                i = 0
                for h in (2 * c, 2 * c + 1):
                    for mt in range(2):
                        mm(R3[:, c * 128:(c + 1) * 128], Vpad[:, mt, h, :], expT[:, (h * 2 + mt) * 128:(h * 2 + mt + 1) * 128], i == 0, i == 3, [XAB, EXB], [R3B])
                        i += 1
            for c in range(2):
                i = 0
                for h in (2 * c, 2 * c + 1):
                    for mt in range(2):
                        mm(R3[:, 256 + c * 128:256 + (c + 1) * 128], onespad[:, h % 2, :], expT[:, (h * 2 + mt) * 128:(h * 2 + mt + 1) * 128], i == 0, i == 3, [OPB, EXB], [R3B])
                        i += 1
            act(rsx, R3[:, 256:512], AF.Ln, [R3B], [RSXB])
            act(rsx, rsx, AF.Exp, [RSXB], [RSXB], scale=-1.0)
            S.op("dve", lambda e: e.tensor_tensor(out=catT[:, 6:8, :].rearrange("p c t -> p (c t)"), in0=R3[:, 0:256],
                                                  in1=rsx, op=ALU.mult), reads=[R3B, RSXB], writes=[CATB])
            nxt = (lambda t=t: head1(t + 1)) if t < 15 else (lambda: None)
            if kind == "ret":
                ret_tile(l, j, t, nxt)
            elif kind == "hgrn":
                hgrn_tile(l, j, t, nxt)
            else:
                S.op("dve", lambda e: e.memset(catT[:, 0:6, :], 0.0), writes=[CATB])
                nxt()
            for dc in range(KC):
                for k in range(KC):
                    mm(R0[:, dc * 128:(dc + 1) * 128], w_out_sb[:, k, dc * 128:(dc + 1) * 128], catT[:, k, :], k == 0, k == KC - 1, [WOUT_B, CATB], [R0B])
            S.op("dve", lambda e, ts=ts: e.tensor_tensor(out=xT[:, :, ts], in0=R0.rearrange("p (k c) -> p k c", k=KC), in1=xT[:, :, ts], op=ALU.add),
                 reads=[R0B, XB[t]], writes=[XB[t]])

    def ret_tile(l, j, t, nxt):
        cosb = KIND[:, t * 64:(t + 1) * 64].unsqueeze(1).to_broadcast([128, 6, 64])
        sinb = KIND[:, 1024 + t * 64:1024 + (t + 1) * 64].unsqueeze(1).to_broadcast([128, 6, 64])
        for (R, RB_, sc, dstT, DSTB) in ((R0, R0B, sqc, Qt, QTB_), (R1, R1B, skc, Kt, KTB_)):
            S.op("dve", lambda e, R=R, sc=sc: e.tensor_tensor(out=hv(tA), in0=hv(R[:, 0:768]), in1=sc.unsqueeze(2).to_broadcast([128, 6, 128]), op=ALU.mult),
                 reads=[RB_, CB], writes=[TAB])
            a4 = tA.rearrange("p (h two d) -> p h two d", h=6, two=2)
            c4 = tC.rearrange("p (h two d) -> p h two d", h=6, two=2)
            e4 = tE.rearrange("p (h two d) -> p h two d", h=6, two=2)
            d4 = dstT.rearrange("p (h two d) -> p h two d", h=6, two=2)
            S.op("dve", lambda e, a4=a4, c4=c4: e.tensor_tensor(out=c4[:, :, 0, :], in0=a4[:, :, 0, :], in1=cosb, op=ALU.mult), reads=[TAB, KINDB], writes=[TCB])
            S.op("dve", lambda e, a4=a4, c4=c4: e.tensor_tensor(out=c4[:, :, 1, :], in0=a4[:, :, 0, :], in1=sinb, op=ALU.mult), reads=[TAB, KINDB], writes=[TCB])
            S.op("dve", lambda e, a4=a4, e4=e4: e.tensor_tensor(out=e4[:, :, 0, :], in0=a4[:, :, 1, :], in1=sinb, op=ALU.mult), reads=[TAB, KINDB], writes=[TEB])
            S.op("dve", lambda e, a4=a4, e4=e4: e.tensor_tensor(out=e4[:, :, 1, :], in0=a4[:, :, 1, :], in1=cosb, op=ALU.mult), reads=[TAB, KINDB], writes=[TEB])
            S.op("dve", lambda e, c4=c4, e4=e4, d4=d4: e.tensor_tensor(out=d4[:, :, 0, :], in0=c4[:, :, 0, :], in1=e4[:, :, 0, :], op=ALU.subtract), reads=[TCB, TEB], writes=[DSTB])
            S.op("dve", lambda e, c4=c4, e4=e4, d4=d4: e.tensor_tensor(out=d4[:, :, 1, :], in0=c4[:, :, 1, :], in1=e4[:, :, 1, :], op=ALU.add), reads=[TCB, TEB], writes=[DSTB])
        S.op("act", lambda e: e.copy(out=Vt, in_=R2[:, 0:768]), reads=[R2B], writes=[VTB])
        for h in range(6):
            hs = slice(h * 128, (h + 1) * 128)
            S.op("pe", lambda e, hs=hs: e.transpose(out=R0b[:, hs], in_=Qt[:, hs], identity=identB[:]), reads=[QTB_, CB], writes=[R0B])
            S.op("pe", lambda e, hs=hs: e.transpose(out=R1b[:, hs], in_=Kt[:, hs], identity=identB[:]), reads=[KTB_, CB], writes=[R1B])
        S.op("act", lambda e: e.copy(out=QTf, in_=R0b[:, 0:768]), reads=[R0B], writes=[QTFB])
        S.op("dve", lambda e: e.tensor_copy(out=KTf, in_=R1b[:, 0:768]), reads=[R1B], writes=[KTFB])
        for h in range(6):
            hs = slice(h * 128, (h + 1) * 128)
            mm(R2[:, hs], KTf[:, hs], QTf[:, hs], True, True, [KTFB, QTFB], [R2B])
        for h in range(6):
            hs = slice(h * 128, (h + 1) * 128)
            ginv = float(np.exp(-128.0 * GAMMA_LOG[h]))
            S.op("dve", lambda e, hs=hs, ginv=ginv: e.scalar_tensor_tensor(out=Pm[:, hs], in0=R2[:, hs], scalar=ginv, in1=CAUc, op0=ALU.mult, op1=ALU.mult),
                 reads=[R2B, CB], writes=[PMB])
        for h in range(6):
            hs = slice(h * 128, (h + 1) * 128)
            mm(R0[:, hs], Vt[:, hs], Pm[:, hs], True, True, [VTB, PMB], [R0B])
        for h in range(6):
            hs = slice(h * 128, (h + 1) * 128)
            mm(R1[:, hs], Sbf[:, hs], QTf[:, hs], True, True, [SBFB, QTFB], [R1B])
        for h in range(6):
            hs = slice(h * 128, (h + 1) * 128)
            mm(R3[:, hs], Kt[:, hs], Vt[:, hs], True, True, [KTB_, VTB], [R3B])
        for h in range(6):
            hs = slice(h * 128, (h + 1) * 128)
            ch = float(np.exp(128.0 * GAMMA_LOG[h]))
            S.op("dve", lambda e, hs=hs, ch=ch: e.scalar_tensor_tensor(out=S32[:, hs], in0=S32[:, hs], scalar=ch, in1=R3[:, hs], op0=ALU.mult, op1=ALU.add),
                 reads=[S32B, R3B], writes=[S32B])
        S.op("act", lambda e: e.copy(out=Sbf, in_=S32), reads=[S32B], writes=[SBFB])
        S.op("act", lambda e: e.copy(out=tO, in_=R0[:, 0:768]), reads=[R0B], writes=[TOB])
        S.op("dve", lambda e: e.tensor_tensor(out=tO, in0=R1[:, 0:768], in1=tO, op=ALU.add), reads=[R1B, TOB], writes=[TOB])
        act(sqo, tO, AF.Square, [TOB], [SQOB])
        for h in range(6):
            hs = slice(h * 128, (h + 1) * 128)
            mm(R2[:, hs], onesB[:], sqo[:, hs], True, True, [SQOB, CB], [R2B])
        act(tR, R2[:, 0:768], AF.Ln, [R2B], [TRB], scale=1.0 / 128, bias=EPS)
        act(tR, tR, AF.Exp, [TRB], [TRB], scale=-0.5)
        nxt()
        for h in range(6):
            hs = slice(h * 128, (h + 1) * 128)
            S.op("dve", lambda e, hs=hs, h=h: e.scalar_tensor_tensor(out=tC[:, hs], in0=tO[:, hs], scalar=col_rtn(j, h), in1=tR[:, hs], op0=ALU.mult, op1=ALU.mult),
                 reads=[TOB, TRB, VC], writes=[TCB])
        S.op("dve", lambda e: e.tensor_tensor(out=catT[:, 0:6, :], in0=hv(tC), in1=hv(tG), op=ALU.mult), reads=[TCB, TGB], writes=[CATB])

    def hgrn_tile(l, j, t, nxt):
        lb_bc = KIND[:, 0:768]
        nom_bc = KIND[:, 768:1536]
        act(tA, R1[:, 0:768], AF.Exp, [R1B], [TAB], scale=-1.0)
        S.op("dve", lambda e: e.scalar_tensor_tensor(out=tB, in0=tA, scalar=E30, in1=lb_bc, op0=ALU.min, op1=ALU.mult), reads=[TAB, KINDB], writes=[TBB])
        act(tB, tB, AF.Ln, [TBB], [TBB], bias=1.0)
        act(tA, tA, AF.Ln, [TAB], [TAB], bias=1.0)
        S.op("dve", lambda e: e.tensor_tensor(out=tB, in0=tB, in1=tA, op=ALU.subtract), reads=[TBB, TAB], writes=[TBB])
        act(tA, tA, AF.Exp, [TAB], [TAB], scale=-1.0)
        S.op("dve", lambda e: e.scalar_tensor_tensor(out=tA, in0=tA, scalar=1.0, in1=nom_bc, op0=ALU.subtract, op1=ALU.mult), reads=[TAB, KINDB], writes=[TAB])
        S.op("act", lambda e: e.copy(out=Vt, in_=R2[:, 0:768]), reads=[R2B], writes=[VTB])
        for (R, RB_, M) in ((R0, R0B, M1c), (R1, R1B, M2c), (R2, R2B, M4c)):
            for (a, n) in ((0, 512), (512, 256)):
                mm(R[:, a:a + n], M, tB[:, a:a + n], True, True, [CB, TBB], [RB_])
        act(tE, R0[:, 0:768], AF.Exp, [R0B], [TEB])
        S.op("dve", lambda e: e.tensor_tensor(out=Qt, in0=tC, in1=tE, op=ALU.mult), reads=[TCB, TEB], writes=[QTB_])
        act(tB, R0[:, 0:768], AF.Exp, [R0B], [TBB], scale=-1.0)
        S.op("dve", lambda e: e.tensor_tensor(out=Kt, in0=tA, in1=tB, op=ALU.mult), reads=[TAB, TBB], writes=[KTB_])
        act(tE, R1[:, 0:768], AF.Exp, [R1B], [TEB])
        S.op("dve", lambda e: e.tensor_tensor(out=Qot, in0=tC, in1=tE, op=ALU.mult), reads=[TCB, TEB], writes=[QOTB_])
        for h in range(6):
            hs = slice(h * 128, (h + 1) * 128)
            S.op("pe", lambda e, hs=hs: e.transpose(out=R0[:, hs], in_=tE[:, hs], identity=identF), reads=[TEB, CB], writes=[R0B])
        S.op("dve", lambda e: e.tensor_copy(out=dec.rearrange("p (h n) -> p h n", h=6),
                                            in_=R0[:, 0:768].rearrange("p (h n c) -> p h n c", h=6, n=4)[:, :, :, 31]), reads=[R0B], writes=[DECB])
        act(tB, R2[:, 0:768], AF.Exp, [R2B], [TBB])
        S.op("dve", lambda e: e.tensor_tensor(out=Kst, in0=tA, in1=tB, op=ALU.mult), reads=[TAB, TBB], writes=[KSTB])
        for h in range(6):
            hs = slice(h * 128, (h + 1) * 128)
            S.op("pe", lambda e, hs=hs: e.transpose(out=R1b[:, hs], in_=Qt[:, hs], identity=identB[:]), reads=[QTB_, CB], writes=[R1B])
            S.op("pe", lambda e, hs=hs: e.transpose(out=R2b[:, hs], in_=Kt[:, hs], identity=identB[:]), reads=[KTB_, CB], writes=[R2B])
            S.op("pe", lambda e, hs=hs: e.transpose(out=R3b[:, hs], in_=Qot[:, hs], identity=identB[:]), reads=[QOTB_, CB], writes=[R3B])
        S.op("act", lambda e: e.copy(out=QTf, in_=R1b[:, 0:768]), reads=[R1B], writes=[QTFB])
        S.op("dve", lambda e: e.tensor_copy(out=KTf, in_=R2b[:, 0:768]), reads=[R2B], writes=[KTFB])
        S.op("act", lambda e: e.copy(out=QoTf, in_=R3b[:, 0:768]), reads=[R3B], writes=[QOTFB])
        for h in range(6):
            hs = slice(h * 128, (h + 1) * 128)
            mm(R0[:, hs], KTf[:, hs], QTf[:, hs], True, True, [KTFB, QTFB], [R0B])
        S.op("dve", lambda e: e.tensor_tensor(out=hv(Pm), in0=hv(R0[:, 0:768]), in1=BDc.unsqueeze(1).to_broadcast([128, 6, 128]), op=ALU.mult),
             reads=[R0B, CB], writes=[PMB])
        for h in range(6):
            hs = slice(h * 128, (h + 1) * 128)
            mm(R1[:, hs], Vt[:, hs], Pm[:, hs], True, True, [VTB, PMB], [R1B])
        for n in range(4):
            ns = slice(32 * n, 32 * n + 32)
            for h in range(6):
                hs = slice(h * 128, (h + 1) * 128)
                cs = slice(h * 128 + 32 * n, h * 128 + 32 * n + 32)
                mm(R2[:, cs], Sbf[:, hs], QoTf[:, cs], True, True, [SBFB, QOTFB], [R2B])
            for h in range(6):
                hs = slice(h * 128, (h + 1) * 128)
                mm(R3[:, hs], Kst[ns, hs], Vt[ns, hs], True, True, [KSTB, VTB], [R3B], tile_position=(32 * n, 0))
            for h in range(6):
                hs = slice(h * 128, (h + 1) * 128)
                S.op("dve", lambda e, hs=hs, h=h, n=n: e.scalar_tensor_tensor(out=S32[:, hs], in0=S32[:, hs], scalar=dec[:, h * 4 + n:h * 4 + n + 1], in1=R3[:, hs],
                                                                           op0=ALU.mult, op1=ALU.add), reads=[S32B, R3B, DECB], writes=[S32B])
            S.op("act", lambda e: e.copy(out=Sbf, in_=S32), reads=[S32B], writes=[SBFB])
        S.op("act", lambda e: e.copy(out=tO, in_=R1[:, 0:768]), reads=[R1B], writes=[TOB])
        S.op("dve", lambda e: e.tensor_tensor(out=tO, in0=R2[:, 0:768], in1=tO, op=ALU.add), reads=[R2B, TOB], writes=[TOB])
        act(sqo, tO, AF.Square, [TOB], [SQOB])
        for h in range(6):
            hs = slice(h * 128, (h + 1) * 128)
            mm(R0[:, 0:128], onesB[:], sqo[:, hs], h == 0, h == 5, [SQOB, CB], [R0B])
        act(tR[:, 0:128], R0[:, 0:128], AF.Ln, [R0B], [TRB], scale=4.0 / MIXW, bias=4.0 * EPS)
        act(tR[:, 0:128], tR[:, 0:128], AF.Exp, [TRB], [TRB], scale=-0.5)
        nxt()
        for h in range(6):
            hs = slice(h * 128, (h + 1) * 128)
            S.op("dve", lambda e, hs=hs, h=h: e.scalar_tensor_tensor(out=tC[:, hs], in0=tO[:, hs], scalar=col_hgn(j, h), in1=tR[:, 0:128], op0=ALU.mult, op1=ALU.mult),
                 reads=[TOB, TRB, VC], writes=[TCB])
        S.op("dve", lambda e: e.scalar_tensor_tensor(out=hv(catT[:, 0:6, :].rearrange("p h c -> p (h c)")) if False else catT[:, 0:6, :], in0=hv(tG), scalar=1.0, in1=hv(tC),
                                                     op0=ALU.add, op1=ALU.mult), reads=[TGB, TCB], writes=[CATB])

    for l in range(n_layers):
        kind = kinds[l] if kinds else ("hgrn" if l % 2 == 0 else "ret")
        j = l // 2
        if do_mix:
            S.barrier()
            mixer_layer(l, kind, j)
        if do_ffn:
            S.barrier()
            for b in range(4):
                rs, rsb = rms_stats_blk(lambda k, b=b: xT[:, k, b * 512:(b + 1) * 512], XB[4 * b:4 * b + 4], 512, 1.0 / D)
                for k in range(KC):
                    S.op("dve", lambda e, b=b, k=k, l=l, rs=rs: e.scalar_tensor_tensor(
                        out=hT[:, k, b * 512:(b + 1) * 512], in0=xT[:, k, b * 512:(b + 1) * 512], scalar=col_norm_ffn(l, k),
                        in1=rs, op0=ALU.mult, op1=ALU.mult), reads=XB[4 * b:4 * b + 4] + [rsb, VC], writes=[HB[b]])
            for (f0, nf) in GROUPS:
                S.dma("pool", lambda e, f0=f0, nf=nf, l=l: e.dma_start(
                    out=wo_sb[:, 0:nf, :], in_=w_fo_d[l, f0 * 128:(f0 + nf) * 128, :].rearrange("(f p) n -> p f n", p=128)), writes=[WO_B])
                for fi in range(nf):
                    f = f0 + fi
                    wi, wib = wi_rot.next()
                    S.dma("pool", lambda e, wi=wi, f=f, l=l: e.dma_start(
                        out=wi[:, 0], in_=w_fi_d[l, :, f * 128:(f + 1) * 128].rearrange("(k p) n -> p k n", p=128)), writes=[wib[0]])
                    S.dma("pool", lambda e, wi=wi, f=f, l=l: e.dma_start(
                        out=wi[:, 1], in_=w_fi_d[l, :, DFF + f * 128:DFF + (f + 1) * 128].rearrange("(k p) n -> p k n", p=128)), writes=[wib[1]])
                    for b in range(4):
                        pg, pgb = ps_all.next()
                        pu, pub = ps_all.next()
                        for k in range(KC):
                            S.op("pe", lambda e, pg=pg, wi=wi, k=k, b=b: e.matmul(pg, lhsT=wi[:, 0, k, :], rhs=hT[:, k, b * 512:(b + 1) * 512],
                                                                             start=(k == 0), stop=(k == KC - 1)), reads=[wib[0], HB[b]], writes=[pgb])
                        for k in range(KC):
                            S.op("pe", lambda e, pu=pu, wi=wi, k=k, b=b: e.matmul(pu, lhsT=wi[:, 1, k, :], rhs=hT[:, k, b * 512:(b + 1) * 512],
                                                                             start=(k == 0), stop=(k == KC - 1)), reads=[wib[1], HB[b]], writes=[pub])
                        sl, slb = silu_rot.next()
                        S.op("act", lambda e, sl=sl, pg=pg: e.activation(out=sl, in_=pg, func=AF.Silu), reads=[pgb], writes=[slb])
                        S.op("dve", lambda e, sl=sl, pu=pu, fi=fi, b=b: e.tensor_tensor(out=aT[:, fi, b * 512:(b + 1) * 512], in0=pu, in1=sl, op=ALU.mult),
                             reads=[pub, slb], writes=[AB[fi][b]])
                for b in range(4):
                    for dc in range(KC):
                        py, pyb = ps_all.next()
                        for fi in range(nf):
                            S.op("pe", lambda e, py=py, fi=fi, dc=dc, b=b, nf=nf: e.matmul(py, lhsT=wo_sb[:, fi, dc * 128:(dc + 1) * 128],
                                                                                      rhs=aT[:, fi, b * 512:(b + 1) * 512], start=(fi == 0), stop=(fi == nf - 1)),
                                 reads=[WO_B, AB[fi][b]], writes=[pyb])
                        S.op("dve", lambda e, py=py, dc=dc, b=b: e.tensor_tensor(out=xT[:, dc, b * 512:(b + 1) * 512], in0=py,
                                                                                in1=xT[:, dc, b * 512:(b + 1) * 512], op=ALU.add),
                             reads=[pyb] + XB[4 * b:4 * b + 4], writes=XB[4 * b:4 * b + 4])

    S.barrier()
    for b in range(4):
        rs, rsb = rms_stats_blk(lambda k, b=b: xT[:, k, b * 512:(b + 1) * 512], XB[4 * b:4 * b + 4], 512, 1.0 / D)
        for tt in range(4):
            t = 4 * b + tt
            yT, ytb = yT_rot.next()
            for k in range(KC):
                S.op("dve", lambda e, yT=yT, k=k, t=t, tt=tt, rs=rs: e.scalar_tensor_tensor(
                    out=yT[:, k, :], in0=xT[:, k, t * 128:(t + 1) * 128], scalar=col_norm_fin(k),
                    in1=rs[:, tt * 128:(tt + 1) * 128], op0=ALU.mult, op1=ALU.mult), reads=[XB[t], rsb, VC], writes=[ytb])
            stg, stgb = stage_rot.next()
            for half in range(2):
                ps, pb = ps_all.next()
                for jj in range(4):
                    k = half * 4 + jj
                    S.op("pe", lambda e, ps=ps, yT=yT, jj=jj, k=k: e.transpose(out=ps[:, jj * 128:(jj + 1) * 128], in_=yT[:, k, :], identity=identF),
                         reads=[ytb, CB], writes=[pb])
                if half == 0:
                    S.op("act", lambda e, ps=ps, stg=stg, half=half: e.copy(out=stg[:, half * 512:(half + 1) * 512], in_=ps), reads=[pb], writes=[stgb])
                else:
                    S.op("dve", lambda e, ps=ps, stg=stg, half=half: e.tensor_copy(out=stg[:, half * 512:(half + 1) * 512], in_=ps), reads=[pb], writes=[stgb])
            outs.append(S.dma("sp", lambda e, stg=stg, t=t: e.dma_start(out=out_d[t * 128:(t + 1) * 128, :], in_=stg), reads=[stgb], semkey="out"))

    S.run_block(final_waits=outs)
    S.close()
    es.close()
    return nc


def _consts():
    c = np.zeros((128, 1024), np.float64)
    c[:, 0:128] = np.eye(128)
    s = np.arange(128)[:, None]
    t = np.arange(128)[None, :]
    same = (s // 32) == (t // 32)
    cs, ct = s % 32, t % 32
    c[:, 128:256] = same * ((cs <= ct).astype(np.float64) - (cs <= 15).astype(np.float64))
    c[:, 256:384] = same * (cs <= ct)
    c[:, 384:512] = same * (cs > ct)
    c[:, 512:640] = same * (s <= t)
    c[:, 640:768] = (s <= t)
    p = np.arange(128)
    for h in range(6):
        lg = GAMMA_LOG[h]
        c[:, 768 + h] = np.exp(lg * (p + 1.0))
        c[:, 774 + h] = np.exp(lg * (127.0 - p)) * (128.0 ** -0.5)
    inv = (np.float32(10000.0) ** (-np.linspace(0.0, 1.0, 64, dtype=np.float32))).astype(np.float32)
    c[:, 832:896] = inv[None, :]
    return c.astype(np.float32)


_NC_CACHE = {}


def make_in_maps(inputs, n_cores=8):
    x = np.asarray(inputs["x"], np.float32)
    mem = np.asarray(inputs["mem"], np.float32)
    pos = np.asarray(inputs["positions"], np.int32)
    f = lambda k: np.ascontiguousarray(np.asarray(inputs[k], np.float32))
    shared = {k: f(k) for k in ("norm_mix", "w_in", "w_out", "norm_mem", "w_mem_kv", "hgrn_lb_logits", "hgrn_out_norm",
                                "norm_ffn", "w_ffn_in", "w_ffn_out")}
    shared["ret_out_norm"] = np.ascontiguousarray(np.asarray(inputs["ret_out_norm"], np.float32).reshape(2, MIXW))
    shared["norm_final"] = np.ascontiguousarray(np.asarray(inputs["norm_final"], np.float32).reshape(1, D))
    shared["consts"] = _consts()
    maps = []
    for c in range(n_cores):
        m = dict(shared)
        m["x"] = np.ascontiguousarray(x[c])
        m["mem"] = np.ascontiguousarray(mem[c])
        m["positions"] = np.ascontiguousarray(pos[c].reshape(16, 128))
        maps.append(m)
    return maps


def kernel(**inputs):
    if "full" not in _NC_CACHE:
        _NC_CACHE["full"] = build()
    nc = _NC_CACHE["full"]
    maps = make_in_maps(inputs, 8)
    res = run_bass_kernel_spmd(nc, maps, core_ids=list(range(8)))
    return np.stack([np.asarray(r["out"], np.float32) for r in res.results], axis=0)
```

```python
import numpy as np
from contextlib import ExitStack
import concourse.bass as bass
import concourse.mybir as mybir
from concourse.bass_utils import run_bass_kernel_spmd

F32 = mybir.dt.float32
BF16 = mybir.dt.bfloat16
I32 = mybir.dt.int32
AF = mybir.ActivationFunctionType
ALU = mybir.AluOpType

D = 1024
T = 2048
NL = 4
KC = 8
MIXW = 768
INW = 3328
DFF = 2816
FC = 22
NMEM = 256
EPS = 1e-6


class Buf:
    __slots__ = ("name", "w", "r", "kids")

    def __init__(self, name="", kids=()):
        self.name = name
        self.w = None
        self.r = []
        self.kids = tuple(kids)


def _expand(bufs):
    out = []
    for b in bufs:
        out.append(b)
        out.extend(b.kids)
    return out


class Op:
    __slots__ = ("eng", "fn", "deps", "sig", "signal", "dma", "idx")


class Sched:
    ENGS = ("pe", "act", "dve", "pool", "sp")

    def __init__(self, nc):
        self.nc = nc
        self.ops = {e: [] for e in self.ENGS}
        self.n = 0
        self.phase = 0
        self.dma_sems = {}
        self.eng_sems = {}
        self._sem_ctx = []
        self.pending_dma = []

    def _new_sem(self, name):
        cm = self.nc.semaphore(name)
        s = cm.__enter__()
        self._sem_ctx.append(cm)
        return s

    def close(self):
        for cm in reversed(self._sem_ctx):
            cm.__exit__(None, None, None)

    def _deps(self, op, reads, writes):
        reads = _expand(reads)
        writes = _expand(writes)
        deps = []
        for b in reads:
            if b.w is not None:
                deps.append(b.w)
        for b in writes:
            if b.w is not None:
                deps.append(b.w)
            deps.extend(b.r)
        for b in reads:
            b.r.append(op)
        for b in writes:
            b.w = op
            b.r = []
        out = []
        seen = set()
        for d in deps:
            if d is op or id(d) in seen:
                continue
            seen.add(id(d))
            if d.eng == "pe" and op.eng == "pe" and not d.dma and not op.dma:
                continue
            out.append(d)
        return out

    def op(self, eng, fn, reads=(), writes=(), extra=()):
        o = Op()
        o.eng = eng
        o.fn = fn
        o.dma = False
        o.sig = False
        o.idx = self.n
        self.n += 1
        o.deps = self._deps(o, reads, writes) + list(extra)
        for d in o.deps:
            d.sig = True
        o.signal = (eng, self.phase)
        self.ops[eng].append(o)
        return o

    def dma(self, queue, fn, reads=(), writes=(), semkey=None):
        o = Op()
        o.eng = queue
        o.fn = fn
        o.dma = True
        o.sig = True
        o.idx = self.n
        self.n += 1
        o.deps = self._deps(o, reads, writes)
        for d in o.deps:
            d.sig = True
        if semkey is None:
            semkey = ("dma", id(writes[0]))
        if semkey not in self.dma_sems:
            self.dma_sems[semkey] = [self._new_sem("d%d" % len(self.dma_sems)), 0]
        ent = self.dma_sems[semkey]
        ent[1] += 16
        o.signal = (ent[0], ent[1])
        self.ops[queue].append(o)
        self.pending_dma.append(o)
        return o

    def barrier(self):
        lasts = []
        for e in self.ENGS:
            for o in reversed(self.ops[e]):
                if not o.dma:
                    lasts.append(o)
                    break
        lasts += self.pending_dma
        self.pending_dma = []
        for e in self.ENGS:
            self.op(e, lambda eng: eng.nop(), extra=[d for d in lasts])
        self.phase += 1

    def finalize(self):
        counters = {}
        for e in self.ENGS:
            for o in self.ops[e]:
                if o.dma:
                    continue
                if o.sig:
                    key = o.signal
                    if key not in self.eng_sems:
                        self.eng_sems[key] = self._new_sem("e_%s_%d" % key)
                    counters[key] = counters.get(key, 0) + 1
                    o.signal = (self.eng_sems[key], counters[key])
                else:
                    o.signal = None

    def emit(self, ename, eng):
        waited = {}
        for o in self.ops[ename]:
            best = {}
            for d in o.deps:
                sem, val = d.signal
                k = id(sem)
                if waited.get(k, 0) < val:
                    waited[k] = val
                    best[k] = (sem, val)
            ws = list(best.values())
            for sem, val in ws[1:]:
                eng.wait_ge(sem, val)
            ins = o.fn(eng)
            if ws:
                ins._wait_ge(ws[0][0], ws[0][1])
            if o.dma:
                ins.then_inc(o.signal[0], 16)
            elif o.sig:
                ins.then_inc(o.signal[0], 1)

    def run_block(self, final_waits=()):
        self.finalize()
        nc = self.nc
        sch = self
        with nc.Block() as block:
            @block.tensor
            def _(e):
                sch.emit("pe", e)

            @block.scalar
            def _(e):
                sch.emit("act", e)

            @block.vector
            def _(e):
                sch.emit("dve", e)

            @block.gpsimd
            def _(e):
                sch.emit("pool", e)

            @block.sync
            def _(e):
                sch.emit("sp", e)
                best = {}
                for o in final_waits:
                    sem, val = o.signal
                    if id(sem) not in best or best[id(sem)][1] < val:
                        best[id(sem)] = (sem, val)
                for sem, val in best.values():
                    e.wait_ge(sem, val)


class Rot:
    def __init__(self, items):
        self.items = items
        self.i = 0

    def next(self):
        it = self.items[self.i % len(self.items)]
        self.i += 1
        return it


GF = 6
GROUPS = [(0, 6), (6, 6), (12, 6), (18, 4)]
GAMMA_LOG = [float(np.log(np.float32(1.0) - np.float32(2.0) ** np.float32(-5.0 - h))) for h in range(6)]
E30 = float(np.exp(30.0))
PI = float(np.pi)


def build(n_layers=NL, do_mix=True, do_ffn=True, kinds=None):
    nc = bass.Bass("TRN2", target_bir_lowering=False)

    def din(name, shape, dt=F32):
        return nc.dram_tensor(name, shape, dt, kind="ExternalInput").ap()

    x_d = din("x", [T, D])
    mem_d = din("mem", [NMEM, D])
    pos_d = din("positions", [16, 128], I32)
    norm_mix_d = din("norm_mix", [NL, D])
    w_in_d = din("w_in", [NL, D, INW])
    w_out_d = din("w_out", [NL, D, D])
    norm_mem_d = din("norm_mem", [NL, D])
    w_kv_d = din("w_mem_kv", [NL, D, 512])
    lb_d = din("hgrn_lb_logits", [2, MIXW])
    hgn_d = din("hgrn_out_norm", [2, MIXW])
    rtn_d = din("ret_out_norm", [2, MIXW])
    norm_ffn_d = din("norm_ffn", [NL, D])
    w_fi_d = din("w_ffn_in", [NL, D, 2 * DFF])
    w_fo_d = din("w_ffn_out", [NL, DFF, D])
    norm_fin_d = din("norm_final", [1, D])
    consts_d = din("consts", [128, 1024])
    out_d = nc.dram_tensor("out", [T, D], F32, kind="ExternalOutput").ap()

    es = ExitStack()

    def sb(name, shape, dt):
        return es.enter_context(nc.sbuf_tensor(name, shape, dt))

    S = Sched(nc)

    xT = sb("xT", [128, KC, T], F32)
    XB = [Buf("x%d" % i) for i in range(16)]
    U1 = sb("U1", [128, 34816], BF16)
    U2W = 12320
    U2 = sb("U2", [128, U2W], F32)
    U2b = U2[:].bitcast(BF16)
    cst = sb("cst", [128, 1024], F32)
    identF = cst[:, 0:128]
    M1c, M2c, M4c, BDc, CAUc = (cst[:, 128 * i:128 * (i + 1)] for i in range(1, 6))
    sqc = cst[:, 768:774]
    skc = cst[:, 774:780]
    invf = cst[:, 832:896]
    identB = sb("identB", [128, 128], BF16)
    onesB = sb("onesB", [128, 128], BF16)
    vcol = sb("vcol", [128, 128], F32)
    KIND = sb("KIND", [128, 2048], F32)
    KINDB = Buf("KIND")
    xatt = sb("xatt", [128, 2304], BF16)
    KTpad = xatt[:, 0:1024].rearrange("p (h m) -> p h m", h=4)
    Vpad = xatt[:, 1024:2048].rearrange("p (t h c) -> p t h c", t=2, h=4)
    onespad = xatt[:, 2048:2304].rearrange("p (a c) -> p a c", a=2)
    XAB = Buf("xatt")
    OPB = Buf("onespad")
    CB = Buf("consts")
    VC = Buf("vcol")
    sqt = sb("sqt", [128, 2, 512], BF16)
    sq_rot = Rot([(sqt[:, i, :], Buf("sq%d" % i)) for i in range(2)])
    rstd_t = sb("rstd", [128, 2, 512], F32)
    rstd_rot = Rot([(rstd_t[:, i, :], Buf("rstd%d" % i)) for i in range(2)])

    PSA = es.enter_context(nc.psum_tensor("psa", [128, 4096], F32))
    PSAb = PSA[:].bitcast(BF16)
    PB = [Buf("ps%d" % i) for i in range(8)]
    ps_all = Rot([(PSA[:, 512 * i:512 * (i + 1)], PB[i]) for i in range(8)])

    w_in_sb = U1[:, 0:KC * INW].rearrange("p (k n) -> p k n", k=KC)
    w_out_sb = U1[:, KC * INW:KC * INW + KC * D].rearrange("p (k n) -> p k n", k=KC)
    WIN_B = [Buf("win%d" % i) for i in range(4)]
    WOUT_B = Buf("wout")
    hT = U1[:, 0:KC * T].rearrange("p (k t) -> p k t", k=KC)
    HB = [Buf("h%d" % i) for i in range(4)]
    aT = U1[:, KC * T:KC * T + GF * T].rearrange("p (f t) -> p f t", f=GF)
    AB = [[Buf("a%d_%d" % (f, b)) for b in range(4)] for f in range(GF)]
    wo_sb = U2b[:, 0:GF * D].rearrange("p (f n) -> p f n", f=GF)
    WO_B = Buf("wo")
    NWI = 3
    wi_sb = [U2b[:, GF * D + i * 2048:GF * D + (i + 1) * 2048].rearrange("p (g k n) -> p g k n", g=2, k=KC) for i in range(NWI)]
    wi_rot = Rot([(wi_sb[i], (Buf("wig%d" % i), Buf("wiu%d" % i))) for i in range(NWI)])
    st0 = GF * D + NWI * 2048
    silu_rot = Rot([(U2b[:, st0 + i * 512:st0 + (i + 1) * 512], Buf("sl%d" % i)) for i in range(4)])
    stage_rot = Rot([(U2[:, i * 1024:(i + 1) * 1024], Buf("stg%d" % i)) for i in range(2)])
    yT_rot = Rot([(U2[:, 2048 + i * 1024:2048 + (i + 1) * 1024].rearrange("p (k t) -> p k t", k=KC), Buf("yT%d" % i)) for i in range(2)])
    vrows = U2[:, 4096:4224]
    VR = Buf("vrows")

    _o = [0]

    def u2f(n):
        a = U2[:, _o[0]:_o[0] + n]
        _o[0] += n
        return a

    def u2b(n):
        a = U2b[:, 2 * _o[0]:2 * _o[0] + n]
        _o[0] += (n + 1) // 2
        return a
    hTt = u2b(1024).rearrange("p (k t) -> p k t", k=KC); HTB = Buf("hTt")
    catTs = [u2b(1024).rearrange("p (k t) -> p k t", k=KC) for _ in range(2)]
    CATBs = [Buf("catT0"), Buf("catT1")]
    tA = u2f(768); TAB = Buf("tA")
    tB = u2f(768); TBB = Buf("tB")
    tC = u2f(768); TCB = Buf("tC")
    tE = u2f(768); TEB = Buf("tE")
    tO = u2f(768); TOB = Buf("tO")
    tG = u2f(768); TGB = Buf("tG")
    tR = u2f(768); TRB = Buf("tR")
    Qt = u2b(768); QTB_ = Buf("Qt")
    Kt = u2b(768); KTB_ = Buf("Kt")
    Qot = u2b(768); QOTB_ = Buf("Qot")
    Kst = u2b(768); KSTB = Buf("Kst")
    Vt = u2b(768); VTB = Buf("Vt")
    QTf = u2b(768); QTFB = Buf("QTf")
    KTf = u2b(768); KTFB = Buf("KTf")
    QoTf = u2b(768); QOTFB = Buf("QoTf")
    Pm = u2b(768); PMB = Buf("Pm")
    sqo = Pm; SQOB = PMB
    S32H = [Buf("S32h%d" % h) for h in range(6)]
    SBFH = [Buf("Sbfh%d" % h) for h in range(6)]
    S32 = u2f(768); S32B = Buf("S32", kids=S32H)
    Sbf = u2b(768); SBFB = Buf("Sbf", kids=SBFH)
    dec = u2f(24); DECB = Buf("dec")
    qxT = Pm[:, 0:256]; QXB = PMB
    expT = u2b(1024); EXB = Buf("expT")
    rsx = u2f(256); RSXB = Buf("rsx")
    setup_base = _o[0]
    assert _o[0] <= U2W, _o[0]
    memst = [U2[:, 2048 + i * 1024:2048 + (i + 1) * 1024] for i in range(2)]
    MSB = [Buf("memst%d" % i) for i in range(2)]
    memh = U2b[:, 2 * 4096:2 * 4096 + 2048].rearrange("p (t n) -> p t n", t=2)
    MHB = Buf("memh")
    memhT = U2b[:, 2 * 5120:2 * 5120 + 2048].rearrange("p (k m) -> p k m", k=KC)
    MHTB = Buf("memhT")
    wkv = U2b[:, 2 * 6144:2 * 6144 + 4096].rearrange("p (k n) -> p k n", k=KC)
    WKVB = Buf("wkv")
    mscr = U2[:, 8192:8192 + 1024]
    MSCB = Buf("mscr")
    angt = U2[:, 9216:9216 + 1024].rearrange("p (t j) -> p t j", t=16)
    ANGB = Buf("ang")
    posr = U2[:, 10240:10240 + 128]
    posi = U2[:, 10368:10368 + 128].bitcast(I32)
    posf = U2[:, 10496:10496 + 16]
    POSB = Buf("pos")
    assert 10512 <= U2W

    R0 = PSA[:, 0:1024]; R0B = Buf("R0")
    R1 = PSA[:, 1024:2048]; R1B = Buf("R1")
    R2H = [Buf("R2h%d" % h) for h in range(6)]
    R3H = [Buf("R3h%d" % h) for h in range(6)]
    R2 = PSA[:, 2048:3072]; R2B = Buf("R2", kids=R2H)
    R3 = PSA[:, 3072:4096]; R3B = Buf("R3", kids=R3H)
    R0b = PSAb[:, 0:2048]; R1b = PSAb[:, 2048:4096]; R2b = PSAb[:, 4096:6144]; R3b = PSAb[:, 6144:8192]
    ALLPS = PB

    def rbufs(*rb):
        return list(rb)

    outs = []

    S.dma("sp", lambda e: e.dma_start(out=cst[:], in_=consts_d), writes=[CB])
    S.op("dve", lambda e: e.tensor_copy(out=identB[:], in_=identF), reads=[CB], writes=[CB])
    S.op("pool", lambda e: e.memset(onesB[:], 1.0), writes=[CB])
    S.op("pool", lambda e: e.memset(xatt[:], 0.0), writes=[XAB, OPB])
    S.op("pool", lambda e: e.memset(onespad[:, 0, 0:64], 1.0), writes=[OPB])
    S.op("pool", lambda e: e.memset(onespad[:, 1, 64:128], 1.0), writes=[OPB])
    S.op("pool", lambda e: e.memset(vrows, 0.0), writes=[VR])
    for i, (src, r0, n) in enumerate([(norm_mix_d, 0, 32), (norm_ffn_d, 32, 32), (norm_mem_d, 64, 32), (norm_fin_d, 96, 8), (hgn_d, 104, 12), (rtn_d, 116, 12)]):
        S.dma("sp", lambda e, src=src, r0=r0, n=n: e.dma_start(out=vrows[r0:r0 + n, :], in_=src.rearrange("l (k p) -> (l k) p", p=128)),
              writes=[VR], semkey="vr")
    S.op("pe", lambda e: e.transpose(out=PSA[:, 0:128], in_=vrows, identity=identF), reads=[VR, CB], writes=[PB[0]])
    S.op("dve", lambda e: e.tensor_copy(out=vcol[:], in_=PSA[:, 0:128]), reads=[PB[0]], writes=[VC])

    def col_norm_mix(l, k): return vcol[:, l * 8 + k:l * 8 + k + 1]
    def col_norm_ffn(l, k): return vcol[:, 32 + l * 8 + k:32 + l * 8 + k + 1]
    def col_norm_mem(l, k): return vcol[:, 64 + l * 8 + k:64 + l * 8 + k + 1]
    def col_norm_fin(k): return vcol[:, 96 + k:96 + k + 1]
    def col_hgn(j, h): return vcol[:, 104 + j * 6 + h:104 + j * 6 + h + 1]
    def col_rtn(j, h): return vcol[:, 116 + j * 6 + h:116 + j * 6 + h + 1]

    S.barrier()

    def load_transposed(src_d, ntile, dst, dst_bufs):
        for t in range(ntile):
            stg, stgb = stage_rot.next()
            S.dma("sp", lambda e, stg=stg, t=t: e.dma_start(out=stg, in_=src_d[t * 128:(t + 1) * 128, :]), writes=[stgb])
            for half in range(2):
                ps, pb = ps_all.next()
                for j in range(4):
                    k = half * 4 + j
                    S.op("pe", lambda e, ps=ps, stg=stg, j=j, k=k: e.transpose(out=ps[:, j * 128:(j + 1) * 128], in_=stg[:, k * 128:(k + 1) * 128], identity=identF),
                         reads=[stgb, CB], writes=[pb])
                if half == 0:
                    S.op("act", lambda e, ps=ps, half=half, t=t: e.copy(out=dst[:, half * 4:half * 4 + 4, t * 128:(t + 1) * 128],
                                                                       in_=ps.rearrange("p (k c) -> p k c", k=4)), reads=[pb], writes=[dst_bufs[t]])
                else:
                    S.op("dve", lambda e, ps=ps, half=half, t=t: e.tensor_copy(out=dst[:, half * 4:half * 4 + 4, t * 128:(t + 1) * 128],
                                                                              in_=ps.rearrange("p (k c) -> p k c", k=4)), reads=[pb], writes=[dst_bufs[t]])
    load_transposed(x_d, 16, xT, XB)

    def rms_stats_blk(src_fn, src_bufs, width, scale, psreg=None):
        ps, pb = psreg if psreg is not None else ps_all.next()
        for k in range(KC):
            sq, sqb = sq_rot.next()
            S.op("act", lambda e, sq=sq, k=k: e.activation(out=sq[:, 0:width], in_=src_fn(k), func=AF.Square), reads=src_bufs, writes=[sqb])
            S.op("pe", lambda e, ps=ps, sq=sq, k=k: e.matmul(ps[:, 0:width], lhsT=onesB[:], rhs=sq[:, 0:width], start=(k == 0), stop=(k == KC - 1)),
                 reads=[sqb, CB], writes=[pb])
        rs, rsb = rstd_rot.next()
        S.op("act", lambda e, ps=ps, rs=rs: e.activation(out=rs[:, 0:width], in_=ps[:, 0:width], func=AF.Ln, scale=scale, bias=EPS), reads=[pb], writes=[rsb])
        S.op("act", lambda e, rs=rs: e.activation(out=rs[:, 0:width], in_=rs[:, 0:width], func=AF.Exp, scale=-0.5), reads=[rsb], writes=[rsb])
        return rs, rsb

    def mm(out, lhsT, rhs, start, stop, reads, writes, **kw):
        S.op("pe", lambda e: e.matmul(out, lhsT=lhsT, rhs=rhs, start=start, stop=stop, **kw), reads=reads, writes=writes)

    def act(out, in_, func, reads, writes, **kw):
        S.op("act", lambda e: e.activation(out=out, in_=in_, func=func, **kw), reads=reads, writes=writes)

    def hv(ap):
        return ap.rearrange("p (h c) -> p h c", h=6)

    def mixer_layer(l, kind, j):
        for i in range(4):
            S.dma("pool", lambda e, i=i: e.dma_start(out=w_in_sb[:, 2 * i:2 * i + 2, :],
                                                    in_=w_in_d[l, 256 * i:256 * (i + 1), :].rearrange("(k p) n -> p k n", p=128)), writes=[WIN_B[i]])
        S.dma("pool", lambda e: e.dma_start(out=w_out_sb, in_=w_out_d[l].rearrange("(k p) n -> p k n", p=128)), writes=[WOUT_B])
        S.dma("pool", lambda e: e.dma_start(out=wkv, in_=w_kv_d[l].rearrange("(k p) n -> p k n", p=128)), writes=[WKVB])
        BIS = 99
        angf_ = angt.rearrange("p t j -> p (t j)")
        if BIS >= 1:
            S.dma("sp", lambda e: e.dma_start(out=angf_[0:1, :], in_=norm_mem_d[l:l + 1, :]), writes=[ANGB])
            for hh in range(2):
                S.op("pe", lambda e, hh=hh: e.matmul(R3[:, hh * 512:(hh + 1) * 512], lhsT=cst[0:1, 640:768], rhs=angf_[0:1, hh * 512:(hh + 1) * 512],
                                                   start=True, stop=True), reads=[ANGB, CB], writes=[R3B])
            S.op("dve", lambda e: e.tensor_copy(out=mscr, in_=R3), reads=[R3B], writes=[MSCB])
        for mt in range(2 if BIS >= 2 else 0):
            S.dma("sp", lambda e, mt=mt: e.dma_start(out=memst[mt], in_=mem_d[mt * 128:(mt + 1) * 128, :]), writes=[MSB[mt]])
            S.op("pool", lambda e, mt=mt: e.memset(posf[:, mt:mt + 1], 0.0), writes=[POSB])
            act(angf_, memst[mt], AF.Square, [MSB[mt], POSB], [ANGB, POSB], accum_out=posf[:, mt:mt + 1])
            act(posf[:, mt:mt + 1], posf[:, mt:mt + 1], AF.Sqrt, [POSB], [POSB], scale=1.0 / D, bias=EPS)
            S.op("dve", lambda e, mt=mt: e.reciprocal(out=posf[:, mt:mt + 1], in_=posf[:, mt:mt + 1]), reads=[POSB], writes=[POSB])
            S.op("dve", lambda e, mt=mt: e.scalar_tensor_tensor(out=memh[:, mt, :], in0=memst[mt], scalar=posf[:, mt:mt + 1], in1=mscr,
                                                               op0=ALU.mult, op1=ALU.mult), reads=[MSB[mt], POSB, MSCB], writes=[MHB])
            if BIS >= 3:
                for k in range(KC):
                    S.op("pe", lambda e, mt=mt, k=k: e.transpose(out=R0b[:, mt * 1024 + k * 128:mt * 1024 + (k + 1) * 128], in_=memh[:, mt, k * 128:(k + 1) * 128],
                                                                identity=identB[:]), reads=[MHB, CB], writes=[R0B])
                S.op("act", lambda e, mt=mt: e.copy(out=memhT[:, :, mt * 128:(mt + 1) * 128], in_=R0b[:, mt * 1024:(mt + 1) * 1024].rearrange("p (k c) -> p k c", k=KC)),
                     reads=[R0B], writes=[MHTB])
        for c in range(2 if BIS >= 4 else 0):
            for k in range(KC):
                mm(R1[:, c * 256:(c + 1) * 256], wkv[:, k, c * 128:(c + 1) * 128], memhT[:, k, :], k == 0, k == KC - 1, [WKVB, MHTB], [R1B])
        if BIS >= 4:
            for h in range(4):
                c, po = h // 2, 64 * (h % 2)
                S.op("act", lambda e, h=h, c=c, po=po: e.copy(out=KTpad[po:po + 64, h, :], in_=R1[po:po + 64, c * 256:(c + 1) * 256]), reads=[R1B], writes=[XAB])
        for mt in range(2 if BIS >= 5 else 0):
            for k in range(KC):
                mm(R2[:, mt * 256:(mt + 1) * 256], memhT[:, k, mt * 128:(mt + 1) * 128], wkv[:, k, 256:512], k == 0, k == KC - 1, [WKVB, MHTB], [R2B])
        for mt in range(2 if BIS >= 5 else 0):
            for h in range(4):
                S.op("dve", lambda e, mt=mt, h=h: e.tensor_copy(out=Vpad[:, mt, h, (h % 2) * 64:(h % 2) * 64 + 64], in_=R2[:, mt * 256 + h * 64:mt * 256 + (h + 1) * 64]),
                     reads=[R2B], writes=[XAB])
        if kind == "ret":
            S.dma("sp", lambda e: e.dma_start(out=posi[0:16, :], in_=pos_d), writes=[POSB])
            S.op("dve", lambda e: e.tensor_copy(out=posr[0:16, :], in_=posi[0:16, :]), reads=[POSB], writes=[POSB])
            S.op("pe", lambda e: e.transpose(out=R3[:, 0:16], in_=posr[0:16, :], identity=identF[0:16, 0:16]), reads=[POSB, CB], writes=[R3B])
            S.op("dve", lambda e: e.tensor_copy(out=posf, in_=R3[:, 0:16]), reads=[R3B], writes=[POSB])
            for t in range(16):
                S.op("dve", lambda e, t=t: e.tensor_scalar(out=angt[:, t, :], in0=invf, scalar1=posf[:, t:t + 1], scalar2=None, op0=ALU.mult), reads=[POSB, CB], writes=[ANGB])
            angf = angt.rearrange("p t j -> p (t j)")
            scr2 = U2[:, 1024:2048]
            scr2i = scr2.bitcast(I32)
            for (off, dst0) in ((0.5 * PI, 0), (0.0, 1024)):
                S.op("dve", lambda e, off=off: e.tensor_scalar(out=mscr, in0=angf, scalar1=off, scalar2=None, op0=ALU.add), reads=[ANGB], writes=[MSCB])
                S.op("dve", lambda e: e.tensor_scalar(out=scr2, in0=mscr, scalar1=1.0 / (2 * PI), scalar2=None, op0=ALU.mult), reads=[MSCB], writes=[TAB, TBB])
                S.op("dve", lambda e: e.tensor_copy(out=scr2i, in_=scr2), reads=[TAB, TBB], writes=[TAB, TBB])
                S.op("dve", lambda e: e.tensor_copy(out=scr2, in_=scr2i), reads=[TAB, TBB], writes=[TAB, TBB])
                S.op("dve", lambda e: e.scalar_tensor_tensor(out=mscr, in0=scr2, scalar=-2 * PI, in1=mscr, op0=ALU.mult, op1=ALU.add), reads=[TAB, TBB, MSCB], writes=[MSCB])
                S.op("dve", lambda e: e.tensor_scalar(out=scr2, in0=mscr, scalar1=PI, scalar2=2 * PI, op0=ALU.is_gt, op1=ALU.mult), reads=[MSCB], writes=[TAB, TBB])
                S.op("dve", lambda e: e.tensor_tensor(out=mscr, in0=mscr, in1=scr2, op=ALU.subtract), reads=[TAB, TBB, MSCB], writes=[MSCB])
                S.op("dve", lambda e: e.tensor_scalar(out=mscr, in0=mscr, scalar1=-PI, scalar2=PI, op0=ALU.max, op1=ALU.min), reads=[MSCB], writes=[MSCB])
                act(KIND[:, dst0:dst0 + 1024], mscr, AF.Sin, [MSCB], [KINDB])
        elif kind == "hgrn":
            lb_bc = KIND[:, 0:768]
            nom_bc = KIND[:, 768:1536]
            if j == 0:
                S.op("pool", lambda e: e.memset(lb_bc, 0.0), writes=[KINDB])
                S.op("pool", lambda e: e.memset(nom_bc, -1.0), writes=[KINDB])
            else:
                S.dma("sp", lambda e: e.dma_start(out=mscr[:, 0:768], in_=lb_d[0:1, :].partition_broadcast(128)), writes=[MSCB])
                S.dma("sp", lambda e: e.dma_start(out=angt.rearrange("p t j -> p (t j)")[:, 0:768], in_=lb_d[1:2, :].partition_broadcast(128)), writes=[ANGB])
                S.op("dve", lambda e: e.tensor_tensor(out=mscr[:, 0:768], in0=mscr[:, 0:768], in1=angt.rearrange("p t j -> p (t j)")[:, 0:768], op=ALU.subtract),
                     reads=[MSCB, ANGB], writes=[MSCB])
                act(mscr[:, 0:768], mscr[:, 0:768], AF.Exp, [MSCB], [MSCB])
                S.op("dve", lambda e: e.tensor_scalar(out=mscr[:, 0:768], in0=mscr[:, 0:768], scalar1=1.0, scalar2=None, op0=ALU.add), reads=[MSCB], writes=[MSCB])
                S.op("dve", lambda e: e.reciprocal(out=lb_bc, in_=mscr[:, 0:768]), reads=[MSCB], writes=[KINDB])
                S.op("dve", lambda e: e.tensor_scalar(out=nom_bc, in0=lb_bc, scalar1=-1.0, scalar2=None, op0=ALU.add), reads=[KINDB], writes=[KINDB])
        S.barrier()
        S.op("pool", lambda e: e.memset(S32, 0.0), writes=[S32B])
        S.op("pool", lambda e: e.memset(Sbf, 0.0), writes=[SBFB])

        blk_rs = {}

        def head1(t):
            ts = slice(t * 128, (t + 1) * 128)
            if t % 4 == 0:
                b0 = t
                blk_rs[t // 4] = rms_stats_blk(lambda k, b0=b0: xT[:, k, b0 * 128:(b0 + 4) * 128], XB[b0:b0 + 4], 512, 1.0 / D, psreg=(R3[:, 0:512], R3B))
            rs, rsb = blk_rs[t // 4]
            tt = t % 4
            for k in range(KC):
                S.op("dve", lambda e, k=k, ts=ts, rs=rs, tt=tt: e.scalar_tensor_tensor(out=hTt[:, k, :], in0=xT[:, k, ts], scalar=col_norm_mix(l, k),
                                                                                   in1=rs[:, tt * 128:(tt + 1) * 128], op0=ALU.mult, op1=ALU.mult),
                     reads=[XB[t], rsb, VC], writes=[HTB])
            for c in range(8):
                c0 = 2304 + c * 128
                for k in range(KC):
                    mm(R3[:, c * 128:(c + 1) * 128], w_in_sb[:, k, c0:c0 + 128], hTt[:, k, :], k == 0, k == KC - 1, [WIN_B[k // 2], HTB], [R3B])

        for t in range(16):
            ts = slice(t * 128, (t + 1) * 128)
            if t == 0:
                head1(0)
            if kind == "hgrn":
                act(tG, R3[:, 0:768], AF.Tanh, [R3B], [TGB], scale=0.5)
            else:
                act(tG, R3[:, 0:768], AF.Silu, [R3B], [TGB])
            S.op("act", lambda e: e.copy(out=qxT, in_=R3[:, 768:1024]), reads=[R3B], writes=[QXB])
            for h in range(4):
                c = h // 2
                for mt in range(2):
                    mm(R3[:, (h * 2 + mt) * 128:(h * 2 + mt + 1) * 128], KTpad[:, h, mt * 128:(mt + 1) * 128], qxT[:, c * 128:(c + 1) * 128],
                       True, True, [XAB, QXB], [R3B])
            for (R, RB_, c0) in ((R0, R0B, 0), (R1, R1B, 768), (R2, R2B, 1536)):
                for (a, n) in ((0, 512), (512, 256)):
                    for k in range(KC):
                        mm(R[:, a:a + n], hTt[:, k, :], w_in_sb[:, k, c0 + a:c0 + a + n], k == 0, k == KC - 1, [WIN_B[k // 2], HTB], [RB_])
            if kind == "hgrn":
                act(tC, R0[:, 0:768], AF.Silu, [R0B], [TCB])
            act(expT, R3[:, 0:1024], AF.Exp, [R3B], [EXB], scale=0.125)
            for c in range(2):
                i = 0
                for h in (2 * c, 2 * c + 1):
                    for mt in range(2):
                        mm(R3[:, c * 128:(c + 1) * 128], Vpad[:, mt, h, :], expT[:, (h * 2 + mt) * 128:(h * 2 + mt + 1) * 128], i == 0, i == 3, [XAB, EXB], [R3B])
                        i += 1
            for c in range(2):
                i = 0
                for h in (2 * c, 2 * c + 1):
                    for mt in range(2):
                        mm(R3[:, 256 + c * 128:256 + (c + 1) * 128], onespad[:, h % 2, :], expT[:, (h * 2 + mt) * 128:(h * 2 + mt + 1) * 128], i == 0, i == 3, [OPB, EXB], [R3B])
                        i += 1
            act(rsx, R3[:, 256:512], AF.Ln, [R3B], [RSXB])
            act(rsx, rsx, AF.Exp, [RSXB], [RSXB], scale=-1.0)
            cat, catb = catTs[t % 2], CATBs[t % 2]
            prev = t - 1 if t > 0 else None
            S.op("dve", lambda e, cat=cat: e.tensor_tensor(out=cat[:, 6:8, :].rearrange("p c t -> p (c t)"), in0=R3[:, 0:256],
                                                           in1=rsx, op=ALU.mult), reads=[R3B, RSXB], writes=[catb])
            nxt = (lambda t=t: head1(t + 1)) if t < 15 else (lambda: None)
            if kind == "ret":
                ret_tile(l, j, t, nxt, cat, catb, prev)
            elif kind == "hgrn":
                hgrn_tile(l, j, t, nxt, cat, catb, prev)
            else:
                S.op("dve", lambda e, cat=cat: e.memset(cat[:, 0:6, :], 0.0), writes=[catb])
                nxt()
                w_out_mm(t, R0, R0B, range(KC))
                w_out_res(t, R0, R0B)
        if kind in ("ret", "hgrn"):
            w_out_mm(15, R0, R0B, range(KC))
            w_out_res(15, R0, R0B)

    def w_out_mm(tp, R, RB_, dcs):
        cat, catb = catTs[tp % 2], CATBs[tp % 2]
        for dc in dcs:
            for k in range(KC):
                mm(R[:, dc * 128:(dc + 1) * 128], w_out_sb[:, k, dc * 128:(dc + 1) * 128], cat[:, k, :], k == 0, k == KC - 1, [WOUT_B, catb], [RB_])

    def w_out_res(tp, R, RB_):
        ts = slice(tp * 128, (tp + 1) * 128)
        S.op("dve", lambda e, ts=ts, R=R: e.tensor_tensor(out=xT[:, :, ts], in0=R.rearrange("p (k c) -> p k c", k=KC), in1=xT[:, :, ts], op=ALU.add),
             reads=[RB_, XB[tp]], writes=[XB[tp]])

    def ret_tile(l, j, t, nxt, cat, catb, prev):
        cosb = KIND[:, t * 64:(t + 1) * 64].unsqueeze(1).to_broadcast([128, 6, 64])
        sinb = KIND[:, 1024 + t * 64:1024 + (t + 1) * 64].unsqueeze(1).to_broadcast([128, 6, 64])
        for (R, RB_, sc, dstT, DSTB) in ((R0, R0B, sqc, Qt, QTB_), (R1, R1B, skc, Kt, KTB_)):
            S.op("dve", lambda e, R=R, sc=sc: e.tensor_tensor(out=hv(tA), in0=hv(R[:, 0:768]), in1=sc.unsqueeze(2).to_broadcast([128, 6, 128]), op=ALU.mult),
                 reads=[RB_, CB], writes=[TAB])
            if R is R0 and prev is not None:
                w_out_mm(prev, R0, R0B, range(KC))
            a4 = tA.rearrange("p (h two d) -> p h two d", h=6, two=2)
            c4 = tC.rearrange("p (h two d) -> p h two d", h=6, two=2)
            e4 = tE.rearrange("p (h two d) -> p h two d", h=6, two=2)
            d4 = dstT.rearrange("p (h two d) -> p h two d", h=6, two=2)
            S.op("dve", lambda e, a4=a4, c4=c4: e.tensor_tensor(out=c4[:, :, 0, :], in0=a4[:, :, 0, :], in1=cosb, op=ALU.mult), reads=[TAB, KINDB], writes=[TCB])
            S.op("dve", lambda e, a4=a4, c4=c4: e.tensor_tensor(out=c4[:, :, 1, :], in0=a4[:, :, 0, :], in1=sinb, op=ALU.mult), reads=[TAB, KINDB], writes=[TCB])
            S.op("dve", lambda e, a4=a4, e4=e4: e.tensor_tensor(out=e4[:, :, 0, :], in0=a4[:, :, 1, :], in1=sinb, op=ALU.mult), reads=[TAB, KINDB], writes=[TEB])
            S.op("dve", lambda e, a4=a4, e4=e4: e.tensor_tensor(out=e4[:, :, 1, :], in0=a4[:, :, 1, :], in1=cosb, op=ALU.mult), reads=[TAB, KINDB], writes=[TEB])
            S.op("dve", lambda e, c4=c4, e4=e4, d4=d4: e.tensor_tensor(out=d4[:, :, 0, :], in0=c4[:, :, 0, :], in1=e4[:, :, 0, :], op=ALU.subtract), reads=[TCB, TEB], writes=[DSTB])
            S.op("dve", lambda e, c4=c4, e4=e4, d4=d4: e.tensor_tensor(out=d4[:, :, 1, :], in0=c4[:, :, 1, :], in1=e4[:, :, 1, :], op=ALU.add), reads=[TCB, TEB], writes=[DSTB])
        S.op("act", lambda e: e.copy(out=Vt, in_=R2[:, 0:768]), reads=[R2B], writes=[VTB])
        if prev is not None:
            w_out_res(prev, R0, R0B)
        for h in range(6):
            hs = slice(h * 128, (h + 1) * 128)
            S.op("pe", lambda e, hs=hs: e.transpose(out=R0b[:, hs], in_=Qt[:, hs], identity=identB[:]), reads=[QTB_, CB], writes=[R0B])
            S.op("pe", lambda e, hs=hs: e.transpose(out=R1b[:, hs], in_=Kt[:, hs], identity=identB[:]), reads=[KTB_, CB], writes=[R1B])
        S.op("act", lambda e: e.copy(out=QTf, in_=R0b[:, 0:768]), reads=[R0B], writes=[QTFB])
        S.op("dve", lambda e: e.tensor_copy(out=KTf, in_=R1b[:, 0:768]), reads=[R1B], writes=[KTFB])
        for h in range(6):
            hs = slice(h * 128, (h + 1) * 128)
            mm(R2[:, hs], KTf[:, hs], QTf[:, hs], True, True, [KTFB, QTFB], [R2B])
        for h in range(6):
            hs = slice(h * 128, (h + 1) * 128)
            ginv = float(np.exp(-128.0 * GAMMA_LOG[h]))
            S.op("dve", lambda e, hs=hs, ginv=ginv: e.scalar_tensor_tensor(out=Pm[:, hs], in0=R2[:, hs], scalar=ginv, in1=CAUc, op0=ALU.mult, op1=ALU.mult),
                 reads=[R2B, CB], writes=[PMB])
        for h in range(6):
            hs = slice(h * 128, (h + 1) * 128)
            mm(R0[:, hs], Vt[:, hs], Pm[:, hs], True, True, [VTB, PMB], [R0B])
        for h in range(6):
            hs = slice(h * 128, (h + 1) * 128)
            mm(R1[:, hs], Sbf[:, hs], QTf[:, hs], True, True, [SBFB, QTFB], [R1B])
        for h in range(6):
            hs = slice(h * 128, (h + 1) * 128)
            mm(R3[:, hs], Kt[:, hs], Vt[:, hs], True, True, [KTB_, VTB], [R3B])
        for h in range(6):
            hs = slice(h * 128, (h + 1) * 128)
            ch = float(np.exp(128.0 * GAMMA_LOG[h]))
            S.op("dve", lambda e, hs=hs, ch=ch: e.scalar_tensor_tensor(out=S32[:, hs], in0=S32[:, hs], scalar=ch, in1=R3[:, hs], op0=ALU.mult, op1=ALU.add),
                 reads=[S32B, R3B], writes=[S32B])
        S.op("act", lambda e: e.copy(out=Sbf, in_=S32), reads=[S32B], writes=[SBFB])
        S.op("act", lambda e: e.copy(out=tO, in_=R0[:, 0:768]), reads=[R0B], writes=[TOB])
        S.op("dve", lambda e: e.tensor_tensor(out=tO, in0=R1[:, 0:768], in1=tO, op=ALU.add), reads=[R1B, TOB], writes=[TOB])
        act(sqo, tO, AF.Square, [TOB], [SQOB])
        for h in range(6):
            hs = slice(h * 128, (h + 1) * 128)
            mm(R2[:, hs], onesB[:], sqo[:, hs], True, True, [SQOB, CB], [R2B])
        act(tR, R2[:, 0:768], AF.Ln, [R2B], [TRB], scale=1.0 / 128, bias=EPS)
        act(tR, tR, AF.Exp, [TRB], [TRB], scale=-0.5)
        nxt()
        for h in range(6):
            hs = slice(h * 128, (h + 1) * 128)
            S.op("dve", lambda e, hs=hs, h=h: e.scalar_tensor_tensor(out=tC[:, hs], in0=tO[:, hs], scalar=col_rtn(j, h), in1=tR[:, hs], op0=ALU.mult, op1=ALU.mult),
                 reads=[TOB, TRB, VC], writes=[TCB])
        S.op("dve", lambda e, cat=cat: e.tensor_tensor(out=cat[:, 0:6, :], in0=hv(tC), in1=hv(tG), op=ALU.mult), reads=[TCB, TGB], writes=[catb])

    def hgrn_tile(l, j, t, nxt, cat, catb, prev):
        lb_bc = KIND[:, 0:768]
        nom_bc = KIND[:, 768:1536]
        act(tA, R1[:, 0:768], AF.Exp, [R1B], [TAB], scale=-1.0)
        S.op("dve", lambda e: e.scalar_tensor_tensor(out=tB, in0=tA, scalar=E30, in1=lb_bc, op0=ALU.min, op1=ALU.mult), reads=[TAB, KINDB], writes=[TBB])
        act(tB, tB, AF.Ln, [TBB], [TBB], bias=1.0)
        act(tA, tA, AF.Ln, [TAB], [TAB], bias=1.0)
        S.op("dve", lambda e: e.tensor_tensor(out=tB, in0=tB, in1=tA, op=ALU.subtract), reads=[TBB, TAB], writes=[TBB])
        act(tA, tA, AF.Exp, [TAB], [TAB], scale=-1.0)
        S.op("dve", lambda e: e.scalar_tensor_tensor(out=tA, in0=tA, scalar=1.0, in1=nom_bc, op0=ALU.subtract, op1=ALU.mult), reads=[TAB, KINDB], writes=[TAB])
        S.op("act", lambda e: e.copy(out=Vt, in_=R2[:, 0:768]), reads=[R2B], writes=[VTB])
        for (R, RB_, M) in ((R0, R0B, M1c), (R1, R1B, M2c), (R2, R2B, M4c)):
            for (a, n) in ((0, 512), (512, 256)):
                mm(R[:, a:a + n], M, tB[:, a:a + n], True, True, [CB, TBB], [RB_])
        act(tE, R0[:, 0:768], AF.Exp, [R0B], [TEB])
        S.op("dve", lambda e: e.tensor_tensor(out=Qt, in0=tC, in1=tE, op=ALU.mult), reads=[TCB, TEB], writes=[QTB_])
        act(tB, R0[:, 0:768], AF.Exp, [R0B], [TBB], scale=-1.0)
        S.op("dve", lambda e: e.tensor_tensor(out=Kt, in0=tA, in1=tB, op=ALU.mult), reads=[TAB, TBB], writes=[KTB_])
        act(tE, R1[:, 0:768], AF.Exp, [R1B], [TEB])
        S.op("dve", lambda e: e.tensor_tensor(out=Qot, in0=tC, in1=tE, op=ALU.mult), reads=[TCB, TEB], writes=[QOTB_])
        for h in range(6):
            hs = slice(h * 128, (h + 1) * 128)
            S.op("pe", lambda e, hs=hs: e.transpose(out=R0[:, hs], in_=tE[:, hs], identity=identF), reads=[TEB, CB], writes=[R0B])
        S.op("dve", lambda e: e.tensor_copy(out=dec.rearrange("p (h n) -> p h n", h=6),
                                            in_=R0[:, 0:768].rearrange("p (h n c) -> p h n c", h=6, n=4)[:, :, :, 31]), reads=[R0B], writes=[DECB])
        act(tB, R2[:, 0:768], AF.Exp, [R2B], [TBB])
        S.op("dve", lambda e: e.tensor_tensor(out=Kst, in0=tA, in1=tB, op=ALU.mult), reads=[TAB, TBB], writes=[KSTB])
        for h in range(6):
            hs = slice(h * 128, (h + 1) * 128)
            S.op("pe", lambda e, hs=hs: e.transpose(out=R1b[:, hs], in_=Qt[:, hs], identity=identB[:]), reads=[QTB_, CB], writes=[R1B])
            S.op("pe", lambda e, hs=hs: e.transpose(out=R2b[:, hs], in_=Kt[:, hs], identity=identB[:]), reads=[KTB_, CB], writes=[R2B])
            S.op("pe", lambda e, hs=hs: e.transpose(out=R3b[:, hs], in_=Qot[:, hs], identity=identB[:]), reads=[QOTB_, CB], writes=[R3B])
        S.op("act", lambda e: e.copy(out=QTf, in_=R1b[:, 0:768]), reads=[R1B], writes=[QTFB])
        S.op("dve", lambda e: e.tensor_copy(out=KTf, in_=R2b[:, 0:768]), reads=[R2B], writes=[KTFB])
        S.op("act", lambda e: e.copy(out=QoTf, in_=R3b[:, 0:768]), reads=[R3B], writes=[QOTFB])
        for h in range(6):
            hs = slice(h * 128, (h + 1) * 128)
            mm(R0[:, hs], KTf[:, hs], QTf[:, hs], True, True, [KTFB, QTFB], [R0B])
        S.op("dve", lambda e: e.tensor_tensor(out=hv(Pm), in0=hv(R0[:, 0:768]), in1=BDc.unsqueeze(1).to_broadcast([128, 6, 128]), op=ALU.mult),
             reads=[R0B, CB], writes=[PMB])
        for h in range(6):
            hs = slice(h * 128, (h + 1) * 128)
            mm(R1[:, hs], Vt[:, hs], Pm[:, hs], True, True, [VTB, PMB], [R1B])
        for n in range(4):
            ns = slice(32 * n, 32 * n + 32)
            for h in range(6):
                hs = slice(h * 128, (h + 1) * 128)
                cs = slice(h * 128 + 32 * n, h * 128 + 32 * n + 32)
                mm(R2[:, cs], Sbf[:, hs], QoTf[:, cs], True, True, [SBFB, QOTFB], [R2B])
            for h in range(6):
                hs = slice(h * 128, (h + 1) * 128)
                mm(R3[:, hs], Kst[ns, hs], Vt[ns, hs], True, True, [KSTB, VTB], [R3B], tile_position=(32 * n, 0))
            if prev is not None:
                w_out_mm(prev, R0, R0B, (2 * n, 2 * n + 1))
            for h in range(6):
                hs = slice(h * 128, (h + 1) * 128)
                S.op("dve", lambda e, hs=hs, h=h, n=n: e.scalar_tensor_tensor(out=S32[:, hs], in0=S32[:, hs], scalar=dec[:, h * 4 + n:h * 4 + n + 1], in1=R3[:, hs],
                                                                           op0=ALU.mult, op1=ALU.add), reads=[S32B, R3B, DECB], writes=[S32B])
            S.op("act", lambda e: e.copy(out=Sbf, in_=S32), reads=[S32B], writes=[SBFB])
        if prev is not None:
            w_out_res(prev, R0, R0B)
        S.op("act", lambda e: e.copy(out=tO, in_=R1[:, 0:768]), reads=[R1B], writes=[TOB])
        S.op("dve", lambda e: e.tensor_tensor(out=tO, in0=R2[:, 0:768], in1=tO, op=ALU.add), reads=[R2B, TOB], writes=[TOB])
        act(sqo, tO, AF.Square, [TOB], [SQOB])
        for h in range(6):
            hs = slice(h * 128, (h + 1) * 128)
            mm(R0[:, 0:128], onesB[:], sqo[:, hs], h == 0, h == 5, [SQOB, CB], [R0B])
        act(tR[:, 0:128], R0[:, 0:128], AF.Ln, [R0B], [TRB], scale=4.0 / MIXW, bias=4.0 * EPS)
        act(tR[:, 0:128], tR[:, 0:128], AF.Exp, [TRB], [TRB], scale=-0.5)
        nxt()
        for h in range(6):
            hs = slice(h * 128, (h + 1) * 128)
            S.op("dve", lambda e, hs=hs, h=h: e.scalar_tensor_tensor(out=tC[:, hs], in0=tO[:, hs], scalar=col_hgn(j, h), in1=tR[:, 0:128], op0=ALU.mult, op1=ALU.mult),
                 reads=[TOB, TRB, VC], writes=[TCB])
        S.op("dve", lambda e, cat=cat: e.scalar_tensor_tensor(out=cat[:, 0:6, :], in0=hv(tG), scalar=1.0, in1=hv(tC),
                                                              op0=ALU.add, op1=ALU.mult), reads=[TGB, TCB], writes=[catb])

    for l in range(n_layers):
        kind = kinds[l] if kinds else ("hgrn" if l % 2 == 0 else "ret")
        j = l // 2
        if do_mix:
            S.barrier()
            mixer_layer(l, kind, j)
        if do_ffn:
            S.barrier()
            for b in range(4):
                rs, rsb = rms_stats_blk(lambda k, b=b: xT[:, k, b * 512:(b + 1) * 512], XB[4 * b:4 * b + 4], 512, 1.0 / D)
                for k in range(KC):
                    S.op("dve", lambda e, b=b, k=k, l=l, rs=rs: e.scalar_tensor_tensor(
                        out=hT[:, k, b * 512:(b + 1) * 512], in0=xT[:, k, b * 512:(b + 1) * 512], scalar=col_norm_ffn(l, k),
                        in1=rs, op0=ALU.mult, op1=ALU.mult), reads=XB[4 * b:4 * b + 4] + [rsb, VC], writes=[HB[b]])
            for (f0, nf) in GROUPS:
                S.dma("pool", lambda e, f0=f0, nf=nf, l=l: e.dma_start(
                    out=wo_sb[:, 0:nf, :], in_=w_fo_d[l, f0 * 128:(f0 + nf) * 128, :].rearrange("(f p) n -> p f n", p=128)), writes=[WO_B])
                for fi in range(nf):
                    f = f0 + fi
                    wi, wib = wi_rot.next()
                    S.dma("pool", lambda e, wi=wi, f=f, l=l: e.dma_start(
                        out=wi[:, 0], in_=w_fi_d[l, :, f * 128:(f + 1) * 128].rearrange("(k p) n -> p k n", p=128)), writes=[wib[0]])
                    S.dma("pool", lambda e, wi=wi, f=f, l=l: e.dma_start(
                        out=wi[:, 1], in_=w_fi_d[l, :, DFF + f * 128:DFF + (f + 1) * 128].rearrange("(k p) n -> p k n", p=128)), writes=[wib[1]])
                    for b in range(4):
                        pg, pgb = ps_all.next()
                        pu, pub = ps_all.next()
                        for k in range(KC):
                            S.op("pe", lambda e, pg=pg, wi=wi, k=k, b=b: e.matmul(pg, lhsT=wi[:, 0, k, :], rhs=hT[:, k, b * 512:(b + 1) * 512],
                                                                             start=(k == 0), stop=(k == KC - 1)), reads=[wib[0], HB[b]], writes=[pgb])
                        for k in range(KC):
                            S.op("pe", lambda e, pu=pu, wi=wi, k=k, b=b: e.matmul(pu, lhsT=wi[:, 1, k, :], rhs=hT[:, k, b * 512:(b + 1) * 512],
                                                                             start=(k == 0), stop=(k == KC - 1)), reads=[wib[1], HB[b]], writes=[pub])
                        sl, slb = silu_rot.next()
                        S.op("act", lambda e, sl=sl, pg=pg: e.activation(out=sl, in_=pg, func=AF.Silu), reads=[pgb], writes=[slb])
                        S.op("dve", lambda e, sl=sl, pu=pu, fi=fi, b=b: e.tensor_tensor(out=aT[:, fi, b * 512:(b + 1) * 512], in0=pu, in1=sl, op=ALU.mult),
                             reads=[pub, slb], writes=[AB[fi][b]])
                for b in range(4):
                    for dc in range(KC):
                        py, pyb = ps_all.next()
                        for fi in range(nf):
                            S.op("pe", lambda e, py=py, fi=fi, dc=dc, b=b, nf=nf: e.matmul(py, lhsT=wo_sb[:, fi, dc * 128:(dc + 1) * 128],
                                                                                      rhs=aT[:, fi, b * 512:(b + 1) * 512], start=(fi == 0), stop=(fi == nf - 1)),
                                 reads=[WO_B, AB[fi][b]], writes=[pyb])
                        S.op("dve", lambda e, py=py, dc=dc, b=b: e.tensor_tensor(out=xT[:, dc, b * 512:(b + 1) * 512], in0=py,
                                                                                in1=xT[:, dc, b * 512:(b + 1) * 512], op=ALU.add),
                             reads=[pyb] + XB[4 * b:4 * b + 4], writes=XB[4 * b:4 * b + 4])

    S.barrier()
    for b in range(4):
        rs, rsb = rms_stats_blk(lambda k, b=b: xT[:, k, b * 512:(b + 1) * 512], XB[4 * b:4 * b + 4], 512, 1.0 / D)
        for tt in range(4):
            t = 4 * b + tt
            yT, ytb = yT_rot.next()
            for k in range(KC):
                S.op("dve", lambda e, yT=yT, k=k, t=t, tt=tt, rs=rs: e.scalar_tensor_tensor(
                    out=yT[:, k, :], in0=xT[:, k, t * 128:(t + 1) * 128], scalar=col_norm_fin(k),
                    in1=rs[:, tt * 128:(tt + 1) * 128], op0=ALU.mult, op1=ALU.mult), reads=[XB[t], rsb, VC], writes=[ytb])
            stg, stgb = stage_rot.next()
            for half in range(2):
                ps, pb = ps_all.next()
                for jj in range(4):
                    k = half * 4 + jj
                    S.op("pe", lambda e, ps=ps, yT=yT, jj=jj, k=k: e.transpose(out=ps[:, jj * 128:(jj + 1) * 128], in_=yT[:, k, :], identity=identF),
                         reads=[ytb, CB], writes=[pb])
                if half == 0:
                    S.op("act", lambda e, ps=ps, stg=stg, half=half: e.copy(out=stg[:, half * 512:(half + 1) * 512], in_=ps), reads=[pb], writes=[stgb])
                else:
                    S.op("dve", lambda e, ps=ps, stg=stg, half=half: e.tensor_copy(out=stg[:, half * 512:(half + 1) * 512], in_=ps), reads=[pb], writes=[stgb])
            outs.append(S.dma("sp", lambda e, stg=stg, t=t: e.dma_start(out=out_d[t * 128:(t + 1) * 128, :], in_=stg), reads=[stgb], semkey="out"))

    S.run_block(final_waits=outs)
    S.close()
    es.close()
    return nc


def _consts():
    c = np.zeros((128, 1024), np.float64)
    c[:, 0:128] = np.eye(128)
    s = np.arange(128)[:, None]
    t = np.arange(128)[None, :]
    same = (s // 32) == (t // 32)
    cs, ct = s % 32, t % 32
    c[:, 128:256] = same * ((cs <= ct).astype(np.float64) - (cs <= 15).astype(np.float64))
    c[:, 256:384] = same * (cs <= ct)
    c[:, 384:512] = same * (cs > ct)
    c[:, 512:640] = same * (s <= t)
    c[:, 640:768] = (s <= t)
    p = np.arange(128)
    for h in range(6):
        lg = GAMMA_LOG[h]
        c[:, 768 + h] = np.exp(lg * (p + 1.0))
        c[:, 774 + h] = np.exp(lg * (127.0 - p)) * (128.0 ** -0.5)
    inv = (np.float32(10000.0) ** (-np.linspace(0.0, 1.0, 64, dtype=np.float32))).astype(np.float32)
    c[:, 832:896] = inv[None, :]
    return c.astype(np.float32)


_NC_CACHE = {}


def make_in_maps(inputs, n_cores=8):
    x = np.asarray(inputs["x"], np.float32)
    mem = np.asarray(inputs["mem"], np.float32)
    pos = np.asarray(inputs["positions"], np.int32)
    f = lambda k: np.ascontiguousarray(np.asarray(inputs[k], np.float32))
    shared = {k: f(k) for k in ("norm_mix", "w_in", "w_out", "norm_mem", "w_mem_kv", "hgrn_lb_logits", "hgrn_out_norm",
                                "norm_ffn", "w_ffn_in", "w_ffn_out")}
    shared["ret_out_norm"] = np.ascontiguousarray(np.asarray(inputs["ret_out_norm"], np.float32).reshape(2, MIXW))
    shared["norm_final"] = np.ascontiguousarray(np.asarray(inputs["norm_final"], np.float32).reshape(1, D))
    shared["consts"] = _consts()
    maps = []
    for c in range(n_cores):
        m = dict(shared)
        m["x"] = np.ascontiguousarray(x[c])
        m["mem"] = np.ascontiguousarray(mem[c])
        m["positions"] = np.ascontiguousarray(pos[c].reshape(16, 128))
        maps.append(m)
    return maps


def kernel(**inputs):
    if "full" not in _NC_CACHE:
        _NC_CACHE["full"] = build()
    nc = _NC_CACHE["full"]
    maps = make_in_maps(inputs, 8)
    res = run_bass_kernel_spmd(nc, maps, core_ids=list(range(8)))
    return np.stack([np.asarray(r["out"], np.float32) for r in res.results], axis=0)
```

```python
import numpy as np
from contextlib import ExitStack
import concourse.bass as bass
import concourse.mybir as mybir
from concourse.bass_utils import run_bass_kernel_spmd

F32 = mybir.dt.float32
BF16 = mybir.dt.bfloat16
I32 = mybir.dt.int32
AF = mybir.ActivationFunctionType
ALU = mybir.AluOpType

D = 1024
T = 2048
NL = 4
KC = 8
MIXW = 768
INW = 3328
DFF = 2816
FC = 22
NMEM = 256
EPS = 1e-6


class Buf:
    __slots__ = ("name", "w", "r", "kids")

    def __init__(self, name="", kids=()):
        self.name = name
        self.w = None
        self.r = []
        self.kids = tuple(kids)


def _expand(bufs):
    out = []
    for b in bufs:
        out.append(b)
        out.extend(b.kids)
    return out


class Op:
    __slots__ = ("eng", "fn", "deps", "sig", "signal", "dma", "idx")


class Sched:
    ENGS = ("pe", "act", "dve", "pool", "sp")

    def __init__(self, nc):
        self.nc = nc
        self.ops = {e: [] for e in self.ENGS}
        self.n = 0
        self.phase = 0
        self.dma_sems = {}
        self.eng_sems = {}
        self._sem_ctx = []
        self.pending_dma = []

    def _new_sem(self, name):
        cm = self.nc.semaphore(name)
        s = cm.__enter__()
        self._sem_ctx.append(cm)
        return s

    def close(self):
        for cm in reversed(self._sem_ctx):
            cm.__exit__(None, None, None)

    def _deps(self, op, reads, writes):
        reads = _expand(reads)
        writes = _expand(writes)
        deps = []
        for b in reads:
            if b.w is not None:
                deps.append(b.w)
        for b in writes:
            if b.w is not None:
                deps.append(b.w)
            deps.extend(b.r)
        for b in reads:
            b.r.append(op)
        for b in writes:
            b.w = op
            b.r = []
        out = []
        seen = set()
        for d in deps:
            if d is op or id(d) in seen:
                continue
            seen.add(id(d))
            if d.eng == "pe" and op.eng == "pe" and not d.dma and not op.dma:
                continue
            out.append(d)
        return out

    def op(self, eng, fn, reads=(), writes=(), extra=()):
        o = Op()
        o.eng = eng
        o.fn = fn
        o.dma = False
        o.sig = False
        o.idx = self.n
        self.n += 1
        o.deps = self._deps(o, reads, writes) + list(extra)
        for d in o.deps:
            d.sig = True
        o.signal = (eng, self.phase)
        self.ops[eng].append(o)
        return o

    def dma(self, queue, fn, reads=(), writes=(), semkey=None):
        o = Op()
        o.eng = queue
        o.fn = fn
        o.dma = True
        o.sig = True
        o.idx = self.n
        self.n += 1
        o.deps = self._deps(o, reads, writes)
        for d in o.deps:
            d.sig = True
        if semkey is None:
            semkey = ("dma", id(writes[0]))
        if semkey not in self.dma_sems:
            self.dma_sems[semkey] = [self._new_sem("d%d" % len(self.dma_sems)), 0]
        ent = self.dma_sems[semkey]
        ent[1] += 16
        o.signal = (ent[0], ent[1])
        self.ops[queue].append(o)
        self.pending_dma.append(o)
        return o

    def barrier(self):
        lasts = []
        for e in self.ENGS:
            for o in reversed(self.ops[e]):
                if not o.dma:
                    lasts.append(o)
                    break
        lasts += self.pending_dma
        self.pending_dma = []
        for e in self.ENGS:
            self.op(e, lambda eng: eng.nop(), extra=[d for d in lasts])
        self.phase += 1

    def finalize(self):
        counters = {}
        for e in self.ENGS:
            for o in self.ops[e]:
                if o.dma:
                    continue
                if o.sig:
                    key = o.signal
                    if key not in self.eng_sems:
                        self.eng_sems[key] = self._new_sem("e_%s_%d" % key)
                    counters[key] = counters.get(key, 0) + 1
                    o.signal = (self.eng_sems[key], counters[key])
                else:
                    o.signal = None

    def emit(self, ename, eng):
        waited = {}
        for o in self.ops[ename]:
            best = {}
            for d in o.deps:
                sem, val = d.signal
                k = id(sem)
                if waited.get(k, 0) < val:
                    waited[k] = val
                    best[k] = (sem, val)
            ws = list(best.values())
            for sem, val in ws[1:]:
                eng.wait_ge(sem, val)
            ins = o.fn(eng)
            if ws:
                ins._wait_ge(ws[0][0], ws[0][1])
            if o.dma:
                ins.then_inc(o.signal[0], 16)
            elif o.sig:
                ins.then_inc(o.signal[0], 1)

    def run_block(self, final_waits=()):
        self.finalize()
        nc = self.nc
        sch = self
        with nc.Block() as block:
            @block.tensor
            def _(e):
                sch.emit("pe", e)

            @block.scalar
            def _(e):
                sch.emit("act", e)

            @block.vector
            def _(e):
                sch.emit("dve", e)

            @block.gpsimd
            def _(e):
                sch.emit("pool", e)

            @block.sync
            def _(e):
                sch.emit("sp", e)
                best = {}
                for o in final_waits:
                    sem, val = o.signal
                    if id(sem) not in best or best[id(sem)][1] < val:
                        best[id(sem)] = (sem, val)
                for sem, val in best.values():
                    e.wait_ge(sem, val)


class Rot:
    def __init__(self, items):
        self.items = items
        self.i = 0

    def next(self):
        it = self.items[self.i % len(self.items)]
        self.i += 1
        return it


GF = 6
GROUPS = [(0, 6), (6, 6), (12, 6), (18, 4)]
GAMMA_LOG = [float(np.log(np.float32(1.0) - np.float32(2.0) ** np.float32(-5.0 - h))) for h in range(6)]
E30 = float(np.exp(30.0))
PI = float(np.pi)


def build(n_layers=NL, do_mix=True, do_ffn=True, kinds=None):
    nc = bass.Bass("TRN2", target_bir_lowering=False)

    def din(name, shape, dt=F32):
        return nc.dram_tensor(name, shape, dt, kind="ExternalInput").ap()

    x_d = din("x", [T, D])
    mem_d = din("mem", [NMEM, D])
    pos_d = din("positions", [16, 128], I32)
    norm_mix_d = din("norm_mix", [NL, D])
    w_in_d = din("w_in", [NL, D, INW])
    w_out_d = din("w_out", [NL, D, D])
    norm_mem_d = din("norm_mem", [NL, D])
    w_kv_d = din("w_mem_kv", [NL, D, 512])
    lb_d = din("hgrn_lb_logits", [2, MIXW])
    hgn_d = din("hgrn_out_norm", [2, MIXW])
    rtn_d = din("ret_out_norm", [2, MIXW])
    norm_ffn_d = din("norm_ffn", [NL, D])
    w_fi_d = din("w_ffn_in", [NL, D, 2 * DFF])
    w_fo_d = din("w_ffn_out", [NL, DFF, D])
    norm_fin_d = din("norm_final", [1, D])
    consts_d = din("consts", [128, 1024])
    out_d = nc.dram_tensor("out", [T, D], F32, kind="ExternalOutput").ap()

    es = ExitStack()

    def sb(name, shape, dt):
        return es.enter_context(nc.sbuf_tensor(name, shape, dt))

    S = Sched(nc)

    xT = sb("xT", [128, KC, T], F32)
    XB = [Buf("x%d" % i) for i in range(16)]
    U1 = sb("U1", [128, 34816], BF16)
    U2W = 12320
    U2 = sb("U2", [128, U2W], F32)
    U2b = U2[:].bitcast(BF16)
    cst = sb("cst", [128, 1024], F32)
    identF = cst[:, 0:128]
    M1c, M2c, M4c, BDc, CAUc = (cst[:, 128 * i:128 * (i + 1)] for i in range(1, 6))
    sqc = cst[:, 768:774]
    skc = cst[:, 774:780]
    invf = cst[:, 832:896]
    identB = sb("identB", [128, 128], BF16)
    onesB = sb("onesB", [128, 128], BF16)
    vcol = sb("vcol", [128, 128], F32)
    KIND = sb("KIND", [128, 2048], F32)
    KINDB = Buf("KIND")
    xatt = sb("xatt", [128, 2304], BF16)
    KTpad = xatt[:, 0:1024].rearrange("p (h m) -> p h m", h=4)
    Vpad = xatt[:, 1024:2048].rearrange("p (t h c) -> p t h c", t=2, h=4)
    onespad = xatt[:, 2048:2304].rearrange("p (a c) -> p a c", a=2)
    XAB = Buf("xatt")
    OPB = Buf("onespad")
    CB = Buf("consts")
    VC = Buf("vcol")
    sqt = sb("sqt", [128, 2, 512], BF16)
    sq_rot = Rot([(sqt[:, i, :], Buf("sq%d" % i)) for i in range(2)])
    rstd_t = sb("rstd", [128, 2, 512], F32)
    rstd_rot = Rot([(rstd_t[:, i, :], Buf("rstd%d" % i)) for i in range(2)])

    PSA = es.enter_context(nc.psum_tensor("psa", [128, 4096], F32))
    PSAb = PSA[:].bitcast(BF16)
    PB = [Buf("ps%d" % i) for i in range(8)]
    ps_all = Rot([(PSA[:, 512 * i:512 * (i + 1)], PB[i]) for i in range(8)])

    w_in_sb = U1[:, 0:KC * INW].rearrange("p (k n) -> p k n", k=KC)
    w_out_sb = U1[:, KC * INW:KC * INW + KC * D].rearrange("p (k n) -> p k n", k=KC)
    WIN_B = [Buf("win%d" % i) for i in range(4)]
    WOUT_B = Buf("wout")
    hT = U1[:, 0:KC * T].rearrange("p (k t) -> p k t", k=KC)
    HB = [Buf("h%d" % i) for i in range(4)]
    aT = U1[:, KC * T:KC * T + GF * T].rearrange("p (f t) -> p f t", f=GF)
    AB = [[Buf("a%d_%d" % (f, b)) for b in range(4)] for f in range(GF)]
    wo_sb = U2b[:, 0:GF * D].rearrange("p (f n) -> p f n", f=GF)
    WO_B = Buf("wo")
    NWI = 3
    wi_sb = [U2b[:, GF * D + i * 2048:GF * D + (i + 1) * 2048].rearrange("p (g k n) -> p g k n", g=2, k=KC) for i in range(NWI)]
    wi_rot = Rot([(wi_sb[i], (Buf("wig%d" % i), Buf("wiu%d" % i))) for i in range(NWI)])
    st0 = GF * D + NWI * 2048
    silu_rot = Rot([(U2b[:, st0 + i * 512:st0 + (i + 1) * 512], Buf("sl%d" % i)) for i in range(4)])
    stage_rot = Rot([(U2[:, i * 1024:(i + 1) * 1024], Buf("stg%d" % i)) for i in range(2)])
    yT_rot = Rot([(U2[:, 2048 + i * 1024:2048 + (i + 1) * 1024].rearrange("p (k t) -> p k t", k=KC), Buf("yT%d" % i)) for i in range(2)])
    vrows = U2[:, 4096:4224]
    VR = Buf("vrows")

    _o = [0]

    def u2f(n):
        a = U2[:, _o[0]:_o[0] + n]
        _o[0] += n
        return a

    def u2b(n):
        a = U2b[:, 2 * _o[0]:2 * _o[0] + n]
        _o[0] += (n + 1) // 2
        return a
    hTt = u2b(1024).rearrange("p (k t) -> p k t", k=KC); HTB = Buf("hTt")
    catTs = [u2b(1024).rearrange("p (k t) -> p k t", k=KC) for _ in range(2)]
    CATBs = [Buf("catT0"), Buf("catT1")]
    tA = u2f(768); TAB = Buf("tA")
    tB = u2f(768); TBB = Buf("tB")
    tC = u2f(768); TCB = Buf("tC")
    tE = u2f(768); TEB = Buf("tE")
    tO = u2f(768); TOB = Buf("tO")
    tG = u2f(768); TGB = Buf("tG")
    tR = u2f(768); TRB = Buf("tR")
    Qt = u2b(768); QTB_ = Buf("Qt")
    Kt = u2b(768); KTB_ = Buf("Kt")
    Qot = u2b(768); QOTB_ = Buf("Qot")
    Kst = u2b(768); KSTB = Buf("Kst")
    Vt = u2b(768); VTB = Buf("Vt")
    QTf = u2b(768); QTFB = Buf("QTf")
    KTf = u2b(768); KTFB = Buf("KTf")
    QoTf = u2b(768); QOTFB = Buf("QoTf")
    Pm = u2b(768); PMB = Buf("Pm")
    sqo = Pm; SQOB = PMB
    S32H = [Buf("S32h%d" % h) for h in range(6)]
    SBFH = [Buf("Sbfh%d" % h) for h in range(6)]
    S32 = u2f(768); S32B = Buf("S32", kids=S32H)
    Sbf = u2b(768); SBFB = Buf("Sbf", kids=SBFH)
    dec = u2f(24); DECB = Buf("dec")
    qxT = Pm[:, 0:256]; QXB = PMB
    expT = u2b(1024); EXB = Buf("expT")
    rsx = u2f(256); RSXB = Buf("rsx")
    setup_base = _o[0]
    assert _o[0] <= U2W, _o[0]
    memst = [U2[:, 2048 + i * 1024:2048 + (i + 1) * 1024] for i in range(2)]
    MSB = [Buf("memst%d" % i) for i in range(2)]
    memh = U2b[:, 2 * 4096:2 * 4096 + 2048].rearrange("p (t n) -> p t n", t=2)
    MHB = Buf("memh")
    memhT = U2b[:, 2 * 5120:2 * 5120 + 2048].rearrange("p (k m) -> p k m", k=KC)
    MHTB = Buf("memhT")
    wkv = U2b[:, 2 * 6144:2 * 6144 + 4096].rearrange("p (k n) -> p k n", k=KC)
    WKVB = Buf("wkv")
    mscr = U2[:, 8192:8192 + 1024]
    MSCB = Buf("mscr")
    angt = U2[:, 9216:9216 + 1024].rearrange("p (t j) -> p t j", t=16)
    ANGB = Buf("ang")
    posr = U2[:, 10240:10240 + 128]
    posi = U2[:, 10368:10368 + 128].bitcast(I32)
    posf = U2[:, 10496:10496 + 16]
    POSB = Buf("pos")
    assert 10512 <= U2W

    R0 = PSA[:, 0:1024]; R0B = Buf("R0")
    R1 = PSA[:, 1024:2048]; R1B = Buf("R1")
    R2H = [Buf("R2h%d" % h) for h in range(6)]
    R3H = [Buf("R3h%d" % h) for h in range(6)]
    R2 = PSA[:, 2048:3072]; R2B = Buf("R2", kids=R2H)
    R3 = PSA[:, 3072:4096]; R3B = Buf("R3", kids=R3H)
    R0b = PSAb[:, 0:2048]; R1b = PSAb[:, 2048:4096]; R2b = PSAb[:, 4096:6144]; R3b = PSAb[:, 6144:8192]
    ALLPS = PB

    def rbufs(*rb):
        return list(rb)

    outs = []

    S.dma("sp", lambda e: e.dma_start(out=cst[:], in_=consts_d), writes=[CB])
    S.op("dve", lambda e: e.tensor_copy(out=identB[:], in_=identF), reads=[CB], writes=[CB])
    S.op("pool", lambda e: e.memset(onesB[:], 1.0), writes=[CB])
    S.op("pool", lambda e: e.memset(xatt[:], 0.0), writes=[XAB, OPB])
    S.op("pool", lambda e: e.memset(onespad[:, 0, 0:64], 1.0), writes=[OPB])
    S.op("pool", lambda e: e.memset(onespad[:, 1, 64:128], 1.0), writes=[OPB])
    S.op("pool", lambda e: e.memset(vrows, 0.0), writes=[VR])
    for i, (src, r0, n) in enumerate([(norm_mix_d, 0, 32), (norm_ffn_d, 32, 32), (norm_mem_d, 64, 32), (norm_fin_d, 96, 8), (hgn_d, 104, 12), (rtn_d, 116, 12)]):
        S.dma("sp", lambda e, src=src, r0=r0, n=n: e.dma_start(out=vrows[r0:r0 + n, :], in_=src.rearrange("l (k p) -> (l k) p", p=128)),
              writes=[VR], semkey="vr")
    S.op("pe", lambda e: e.transpose(out=PSA[:, 0:128], in_=vrows, identity=identF), reads=[VR, CB], writes=[PB[0]])
    S.op("dve", lambda e: e.tensor_copy(out=vcol[:], in_=PSA[:, 0:128]), reads=[PB[0]], writes=[VC])

    def col_norm_mix(l, k): return vcol[:, l * 8 + k:l * 8 + k + 1]
    def col_norm_ffn(l, k): return vcol[:, 32 + l * 8 + k:32 + l * 8 + k + 1]
    def col_norm_mem(l, k): return vcol[:, 64 + l * 8 + k:64 + l * 8 + k + 1]
    def col_norm_fin(k): return vcol[:, 96 + k:96 + k + 1]
    def col_hgn(j, h): return vcol[:, 104 + j * 6 + h:104 + j * 6 + h + 1]
    def col_rtn(j, h): return vcol[:, 116 + j * 6 + h:116 + j * 6 + h + 1]

    S.barrier()

    def load_transposed(src_d, ntile, dst, dst_bufs):
        for t in range(ntile):
            stg, stgb = stage_rot.next()
            S.dma("sp", lambda e, stg=stg, t=t: e.dma_start(out=stg, in_=src_d[t * 128:(t + 1) * 128, :]), writes=[stgb])
            for half in range(2):
                ps, pb = ps_all.next()
                for j in range(4):
                    k = half * 4 + j
                    S.op("pe", lambda e, ps=ps, stg=stg, j=j, k=k: e.transpose(out=ps[:, j * 128:(j + 1) * 128], in_=stg[:, k * 128:(k + 1) * 128], identity=identF),
                         reads=[stgb, CB], writes=[pb])
                if half == 0:
                    S.op("act", lambda e, ps=ps, half=half, t=t: e.copy(out=dst[:, half * 4:half * 4 + 4, t * 128:(t + 1) * 128],
                                                                       in_=ps.rearrange("p (k c) -> p k c", k=4)), reads=[pb], writes=[dst_bufs[t]])
                else:
                    S.op("dve", lambda e, ps=ps, half=half, t=t: e.tensor_copy(out=dst[:, half * 4:half * 4 + 4, t * 128:(t + 1) * 128],
                                                                              in_=ps.rearrange("p (k c) -> p k c", k=4)), reads=[pb], writes=[dst_bufs[t]])

    def rms_stats_blk(src_fn, src_bufs, width, scale, psreg=None):
        ps, pb = psreg if psreg is not None else ps_all.next()
        for k in range(KC):
            sq, sqb = sq_rot.next()
            S.op("act", lambda e, sq=sq, k=k: e.activation(out=sq[:, 0:width], in_=src_fn(k), func=AF.Square), reads=src_bufs, writes=[sqb])
            S.op("pe", lambda e, ps=ps, sq=sq, k=k: e.matmul(ps[:, 0:width], lhsT=onesB[:], rhs=sq[:, 0:width], start=(k == 0), stop=(k == KC - 1)),
                 reads=[sqb, CB], writes=[pb])
        rs, rsb = rstd_rot.next()
        S.op("act", lambda e, ps=ps, rs=rs: e.activation(out=rs[:, 0:width], in_=ps[:, 0:width], func=AF.Ln, scale=scale, bias=EPS), reads=[pb], writes=[rsb])
        S.op("act", lambda e, rs=rs: e.activation(out=rs[:, 0:width], in_=rs[:, 0:width], func=AF.Exp, scale=-0.5), reads=[rsb], writes=[rsb])
        return rs, rsb

    def mm(out, lhsT, rhs, start, stop, reads, writes, **kw):
        S.op("pe", lambda e: e.matmul(out, lhsT=lhsT, rhs=rhs, start=start, stop=stop, **kw), reads=reads, writes=writes)

    def act(out, in_, func, reads, writes, **kw):
        S.op("act", lambda e: e.activation(out=out, in_=in_, func=func, **kw), reads=reads, writes=writes)

    def hv(ap):
        return ap.rearrange("p (h c) -> p h c", h=6)

    weights_issued = set()
    prefetched = set()

    def issue_mixer_weights(l):
        if l in weights_issued:
            return
        weights_issued.add(l)
        S.dma("pool", lambda e: e.dma_start(out=wkv, in_=w_kv_d[l].rearrange("(k p) n -> p k n", p=128)), writes=[WKVB])
        for i in range(4):
            if (l, i) in prefetched:
                continue
            S.dma("pool", lambda e, i=i: e.dma_start(out=w_in_sb[:, 2 * i:2 * i + 2, :],
                                                    in_=w_in_d[l, 256 * i:256 * (i + 1), :].rearrange("(k p) n -> p k n", p=128)), writes=[WIN_B[i]])
        S.dma("pool", lambda e: e.dma_start(out=w_out_sb, in_=w_out_d[l].rearrange("(k p) n -> p k n", p=128)), writes=[WOUT_B])

    def mixer_layer(l, kind, j):
        issue_mixer_weights(l)
        if kind == "ret":
            S.dma("sp", lambda e: e.dma_start(out=posi[0:16, :], in_=pos_d), writes=[POSB])
            S.op("dve", lambda e: e.tensor_copy(out=posr[0:16, :], in_=posi[0:16, :]), reads=[POSB], writes=[POSB])
            S.op("pe", lambda e: e.transpose(out=R3[:, 0:16], in_=posr[0:16, :], identity=identF[0:16, 0:16]), reads=[POSB, CB], writes=[R3B])
            S.op("dve", lambda e: e.tensor_copy(out=posf, in_=R3[:, 0:16]), reads=[R3B], writes=[POSB])
            for t in range(16):
                S.op("dve", lambda e, t=t: e.tensor_scalar(out=angt[:, t, :], in0=invf, scalar1=posf[:, t:t + 1], scalar2=None, op0=ALU.mult), reads=[POSB, CB], writes=[ANGB])
            angf = angt.rearrange("p t j -> p (t j)")
            scr2 = U2[:, 1024:2048]
            scr2i = scr2.bitcast(I32)
            for (off, dst0) in ((0.5 * PI, 0), (0.0, 1024)):
                S.op("dve", lambda e, off=off: e.tensor_scalar(out=mscr, in0=angf, scalar1=off, scalar2=None, op0=ALU.add), reads=[ANGB], writes=[MSCB])
                S.op("dve", lambda e: e.tensor_scalar(out=scr2, in0=mscr, scalar1=1.0 / (2 * PI), scalar2=None, op0=ALU.mult), reads=[MSCB], writes=[TAB, TBB])
                S.op("dve", lambda e: e.tensor_copy(out=scr2i, in_=scr2), reads=[TAB, TBB], writes=[TAB, TBB])
                S.op("dve", lambda e: e.tensor_copy(out=scr2, in_=scr2i), reads=[TAB, TBB], writes=[TAB, TBB])
                S.op("dve", lambda e: e.scalar_tensor_tensor(out=mscr, in0=scr2, scalar=-2 * PI, in1=mscr, op0=ALU.mult, op1=ALU.add), reads=[TAB, TBB, MSCB], writes=[MSCB])
                S.op("dve", lambda e: e.tensor_scalar(out=scr2, in0=mscr, scalar1=PI, scalar2=2 * PI, op0=ALU.is_gt, op1=ALU.mult), reads=[MSCB], writes=[TAB, TBB])
                S.op("dve", lambda e: e.tensor_tensor(out=mscr, in0=mscr, in1=scr2, op=ALU.subtract), reads=[TAB, TBB, MSCB], writes=[MSCB])
                S.op("dve", lambda e: e.tensor_scalar(out=mscr, in0=mscr, scalar1=-PI, scalar2=PI, op0=ALU.max, op1=ALU.min), reads=[MSCB], writes=[MSCB])
                act(KIND[:, dst0:dst0 + 1024], mscr, AF.Sin, [MSCB], [KINDB])
        elif kind == "hgrn":
            lb_bc = KIND[:, 0:768]
            nom_bc = KIND[:, 768:1536]
            if j == 0:
                S.op("pool", lambda e: e.memset(lb_bc, 0.0), writes=[KINDB])
                S.op("pool", lambda e: e.memset(nom_bc, -1.0), writes=[KINDB])
            else:
                S.dma("sp", lambda e: e.dma_start(out=mscr[:, 0:768], in_=lb_d[0:1, :].partition_broadcast(128)), writes=[MSCB])
                S.dma("sp", lambda e: e.dma_start(out=angt.rearrange("p t j -> p (t j)")[:, 0:768], in_=lb_d[1:2, :].partition_broadcast(128)), writes=[ANGB])
                S.op("dve", lambda e: e.tensor_tensor(out=mscr[:, 0:768], in0=mscr[:, 0:768], in1=angt.rearrange("p t j -> p (t j)")[:, 0:768], op=ALU.subtract),
                     reads=[MSCB, ANGB], writes=[MSCB])
                act(mscr[:, 0:768], mscr[:, 0:768], AF.Exp, [MSCB], [MSCB])
                S.op("dve", lambda e: e.tensor_scalar(out=mscr[:, 0:768], in0=mscr[:, 0:768], scalar1=1.0, scalar2=None, op0=ALU.add), reads=[MSCB], writes=[MSCB])
                S.op("dve", lambda e: e.reciprocal(out=lb_bc, in_=mscr[:, 0:768]), reads=[MSCB], writes=[KINDB])
                S.op("dve", lambda e: e.tensor_scalar(out=nom_bc, in0=lb_bc, scalar1=-1.0, scalar2=None, op0=ALU.add), reads=[KINDB], writes=[KINDB])
        BIS = 99
        angf_ = angt.rearrange("p t j -> p (t j)")
        if BIS >= 1:
            S.dma("sp", lambda e: e.dma_start(out=angf_[0:1, :], in_=norm_mem_d[l:l + 1, :]), writes=[ANGB])
            for hh in range(2):
                S.op("pe", lambda e, hh=hh: e.matmul(R3[:, hh * 512:(hh + 1) * 512], lhsT=cst[0:1, 640:768], rhs=angf_[0:1, hh * 512:(hh + 1) * 512],
                                                   start=True, stop=True), reads=[ANGB, CB], writes=[R3B])
            S.op("dve", lambda e: e.tensor_copy(out=mscr, in_=R3), reads=[R3B], writes=[MSCB])
        for mt in range(2 if BIS >= 2 else 0):
            S.dma("sp", lambda e, mt=mt: e.dma_start(out=memst[mt], in_=mem_d[mt * 128:(mt + 1) * 128, :]), writes=[MSB[mt]])
            S.op("pool", lambda e, mt=mt: e.memset(posf[:, mt:mt + 1], 0.0), writes=[POSB])
            act(angf_, memst[mt], AF.Square, [MSB[mt], POSB], [ANGB, POSB], accum_out=posf[:, mt:mt + 1])
            act(posf[:, mt:mt + 1], posf[:, mt:mt + 1], AF.Sqrt, [POSB], [POSB], scale=1.0 / D, bias=EPS)
            S.op("dve", lambda e, mt=mt: e.reciprocal(out=posf[:, mt:mt + 1], in_=posf[:, mt:mt + 1]), reads=[POSB], writes=[POSB])
            S.op("dve", lambda e, mt=mt: e.scalar_tensor_tensor(out=memh[:, mt, :], in0=memst[mt], scalar=posf[:, mt:mt + 1], in1=mscr,
                                                               op0=ALU.mult, op1=ALU.mult), reads=[MSB[mt], POSB, MSCB], writes=[MHB])
            if BIS >= 3:
                for k in range(KC):
                    S.op("pe", lambda e, mt=mt, k=k: e.transpose(out=R0b[:, mt * 1024 + k * 128:mt * 1024 + (k + 1) * 128], in_=memh[:, mt, k * 128:(k + 1) * 128],
                                                                identity=identB[:]), reads=[MHB, CB], writes=[R0B])
                S.op("act", lambda e, mt=mt: e.copy(out=memhT[:, :, mt * 128:(mt + 1) * 128], in_=R0b[:, mt * 1024:(mt + 1) * 1024].rearrange("p (k c) -> p k c", k=KC)),
                     reads=[R0B], writes=[MHTB])
        for c in range(2 if BIS >= 4 else 0):
            for k in range(KC):
                mm(R1[:, c * 256:(c + 1) * 256], wkv[:, k, c * 128:(c + 1) * 128], memhT[:, k, :], k == 0, k == KC - 1, [WKVB, MHTB], [R1B])
        if BIS >= 4:
            for h in range(4):
                c, po = h // 2, 64 * (h % 2)
                S.op("act", lambda e, h=h, c=c, po=po: e.copy(out=KTpad[po:po + 64, h, :], in_=R1[po:po + 64, c * 256:(c + 1) * 256]), reads=[R1B], writes=[XAB])
        for mt in range(2 if BIS >= 5 else 0):
            for k in range(KC):
                mm(R2[:, mt * 256:(mt + 1) * 256], memhT[:, k, mt * 128:(mt + 1) * 128], wkv[:, k, 256:512], k == 0, k == KC - 1, [WKVB, MHTB], [R2B])
        for mt in range(2 if BIS >= 5 else 0):
            for h in range(4):
                S.op("dve", lambda e, mt=mt, h=h: e.tensor_copy(out=Vpad[:, mt, h, (h % 2) * 64:(h % 2) * 64 + 64], in_=R2[:, mt * 256 + h * 64:mt * 256 + (h + 1) * 64]),
                     reads=[R2B], writes=[XAB])
        S.barrier()
        S.op("pool", lambda e: e.memset(S32, 0.0), writes=[S32B])
        S.op("pool", lambda e: e.memset(Sbf, 0.0), writes=[SBFB])

        blk_rs = {}

        def head1(t):
            ts = slice(t * 128, (t + 1) * 128)
            if t % 4 == 0:
                b0 = t
                blk_rs[t // 4] = rms_stats_blk(lambda k, b0=b0: xT[:, k, b0 * 128:(b0 + 4) * 128], XB[b0:b0 + 4], 512, 1.0 / D, psreg=(R3[:, 0:512], R3B))
            rs, rsb = blk_rs[t // 4]
            tt = t % 4
            for k in range(KC):
                S.op("dve", lambda e, k=k, ts=ts, rs=rs, tt=tt: e.scalar_tensor_tensor(out=hTt[:, k, :], in0=xT[:, k, ts], scalar=col_norm_mix(l, k),
                                                                                   in1=rs[:, tt * 128:(tt + 1) * 128], op0=ALU.mult, op1=ALU.mult),
                     reads=[XB[t], rsb, VC], writes=[HTB])
            for c in range(8):
                c0 = 2304 + c * 128
                for k in range(KC):
                    mm(R3[:, c * 128:(c + 1) * 128], w_in_sb[:, k, c0:c0 + 128], hTt[:, k, :], k == 0, k == KC - 1, [WIN_B[k // 2], HTB], [R3B])

        for t in range(16):
            ts = slice(t * 128, (t + 1) * 128)
            if t == 0:
                head1(0)
            if kind == "hgrn":
                act(tG, R3[:, 0:768], AF.Tanh, [R3B], [TGB], scale=0.5)
            else:
                act(tG, R3[:, 0:768], AF.Silu, [R3B], [TGB])
            S.op("act", lambda e: e.copy(out=qxT, in_=R3[:, 768:1024]), reads=[R3B], writes=[QXB])
            for h in range(4):
                c = h // 2
                for mt in range(2):
                    mm(R3[:, (h * 2 + mt) * 128:(h * 2 + mt + 1) * 128], KTpad[:, h, mt * 128:(mt + 1) * 128], qxT[:, c * 128:(c + 1) * 128],
                       True, True, [XAB, QXB], [R3B])
            for (R, RB_, c0) in ((R0, R0B, 0), (R1, R1B, 768), (R2, R2B, 1536)):
                for (a, n) in ((0, 512), (512, 256)):
                    for k in range(KC):
                        mm(R[:, a:a + n], hTt[:, k, :], w_in_sb[:, k, c0 + a:c0 + a + n], k == 0, k == KC - 1, [WIN_B[k // 2], HTB], [RB_])
            if kind == "hgrn":
                act(tC, R0[:, 0:768], AF.Silu, [R0B], [TCB])
            act(expT, R3[:, 0:1024], AF.Exp, [R3B], [EXB], scale=0.125)
            for c in range(2):
                i = 0
                for h in (2 * c, 2 * c + 1):
                    for mt in range(2):
                        mm(R3[:, c * 128:(c + 1) * 128], Vpad[:, mt, h, :], expT[:, (h * 2 + mt) * 128:(h * 2 + mt + 1) * 128], i == 0, i == 3, [XAB, EXB], [R3B])
                        i += 1
            for c in range(2):
                i = 0
                for h in (2 * c, 2 * c + 1):
                    for mt in range(2):
                        mm(R3[:, 256 + c * 128:256 + (c + 1) * 128], onespad[:, h % 2, :], expT[:, (h * 2 + mt) * 128:(h * 2 + mt + 1) * 128], i == 0, i == 3, [OPB, EXB], [R3B])
                        i += 1
            act(rsx, R3[:, 256:512], AF.Ln, [R3B], [RSXB])
            act(rsx, rsx, AF.Exp, [RSXB], [RSXB], scale=-1.0)
            cat, catb = catTs[t % 2], CATBs[t % 2]
            prev = t - 1 if t > 0 else None
            S.op("dve", lambda e, cat=cat: e.tensor_tensor(out=cat[:, 6:8, :].rearrange("p c t -> p (c t)"), in0=R3[:, 0:256],
                                                           in1=rsx, op=ALU.mult), reads=[R3B, RSXB], writes=[catb])
            nxt = (lambda t=t: head1(t + 1)) if t < 15 else (lambda: None)
            if kind == "ret":
                ret_tile(l, j, t, nxt, cat, catb, prev)
            elif kind == "hgrn":
                hgrn_tile(l, j, t, nxt, cat, catb, prev)
            else:
                S.op("dve", lambda e, cat=cat: e.memset(cat[:, 0:6, :], 0.0), writes=[catb])
                nxt()
                w_out_mm(t, R0, R0B, range(KC))
                w_out_res(t, R0, R0B)
        if kind in ("ret", "hgrn"):
            w_out_mm(15, R0, R0B, range(KC))
            w_out_res(15, R0, R0B)

    def w_out_mm(tp, R, RB_, dcs):
        cat, catb = catTs[tp % 2], CATBs[tp % 2]
        for dc in dcs:
            for k in range(KC):
                mm(R[:, dc * 128:(dc + 1) * 128], w_out_sb[:, k, dc * 128:(dc + 1) * 128], cat[:, k, :], k == 0, k == KC - 1, [WOUT_B, catb], [RB_])

    def w_out_res(tp, R, RB_):
        ts = slice(tp * 128, (tp + 1) * 128)
        S.op("dve", lambda e, ts=ts, R=R: e.tensor_tensor(out=xT[:, :, ts], in0=R.rearrange("p (k c) -> p k c", k=KC), in1=xT[:, :, ts], op=ALU.add),
             reads=[RB_, XB[tp]], writes=[XB[tp]])

    def ret_tile(l, j, t, nxt, cat, catb, prev):
        cosb = KIND[:, t * 64:(t + 1) * 64].unsqueeze(1).to_broadcast([128, 6, 64])
        sinb = KIND[:, 1024 + t * 64:1024 + (t + 1) * 64].unsqueeze(1).to_broadcast([128, 6, 64])
        for (R, RB_, sc, dstT, DSTB) in ((R0, R0B, sqc, Qt, QTB_), (R1, R1B, skc, Kt, KTB_)):
            S.op("dve", lambda e, R=R, sc=sc: e.tensor_tensor(out=hv(tA), in0=hv(R[:, 0:768]), in1=sc.unsqueeze(2).to_broadcast([128, 6, 128]), op=ALU.mult),
                 reads=[RB_, CB], writes=[TAB])
            if R is R0 and prev is not None:
                w_out_mm(prev, R0, R0B, range(KC))
            a4 = tA.rearrange("p (h two d) -> p h two d", h=6, two=2)
            c4 = tC.rearrange("p (h two d) -> p h two d", h=6, two=2)
            e4 = tE.rearrange("p (h two d) -> p h two d", h=6, two=2)
            d4 = dstT.rearrange("p (h two d) -> p h two d", h=6, two=2)
            S.op("dve", lambda e, a4=a4, c4=c4: e.tensor_tensor(out=c4[:, :, 0, :], in0=a4[:, :, 0, :], in1=cosb, op=ALU.mult), reads=[TAB, KINDB], writes=[TCB])
            S.op("dve", lambda e, a4=a4, c4=c4: e.tensor_tensor(out=c4[:, :, 1, :], in0=a4[:, :, 0, :], in1=sinb, op=ALU.mult), reads=[TAB, KINDB], writes=[TCB])
            S.op("dve", lambda e, a4=a4, e4=e4: e.tensor_tensor(out=e4[:, :, 0, :], in0=a4[:, :, 1, :], in1=sinb, op=ALU.mult), reads=[TAB, KINDB], writes=[TEB])
            S.op("dve", lambda e, a4=a4, e4=e4: e.tensor_tensor(out=e4[:, :, 1, :], in0=a4[:, :, 1, :], in1=cosb, op=ALU.mult), reads=[TAB, KINDB], writes=[TEB])
            S.op("dve", lambda e, c4=c4, e4=e4, d4=d4: e.tensor_tensor(out=d4[:, :, 0, :], in0=c4[:, :, 0, :], in1=e4[:, :, 0, :], op=ALU.subtract), reads=[TCB, TEB], writes=[DSTB])
            S.op("dve", lambda e, c4=c4, e4=e4, d4=d4: e.tensor_tensor(out=d4[:, :, 1, :], in0=c4[:, :, 1, :], in1=e4[:, :, 1, :], op=ALU.add), reads=[TCB, TEB], writes=[DSTB])
        S.op("act", lambda e: e.copy(out=Vt, in_=R2[:, 0:768]), reads=[R2B], writes=[VTB])
        if prev is not None:
            w_out_res(prev, R0, R0B)
        for h in range(6):
            hs = slice(h * 128, (h + 1) * 128)
            S.op("pe", lambda e, hs=hs: e.transpose(out=R0b[:, hs], in_=Qt[:, hs], identity=identB[:]), reads=[QTB_, CB], writes=[R0B])
            S.op("pe", lambda e, hs=hs: e.transpose(out=R1b[:, hs], in_=Kt[:, hs], identity=identB[:]), reads=[KTB_, CB], writes=[R1B])
        S.op("act", lambda e: e.copy(out=QTf, in_=R0b[:, 0:768]), reads=[R0B], writes=[QTFB])
        S.op("dve", lambda e: e.tensor_copy(out=KTf, in_=R1b[:, 0:768]), reads=[R1B], writes=[KTFB])
        for h in range(6):
            hs = slice(h * 128, (h + 1) * 128)
            mm(R2[:, hs], KTf[:, hs], QTf[:, hs], True, True, [KTFB, QTFB], [R2B])
        for h in range(6):
            hs = slice(h * 128, (h + 1) * 128)
            ginv = float(np.exp(-128.0 * GAMMA_LOG[h]))
            S.op("dve", lambda e, hs=hs, ginv=ginv: e.scalar_tensor_tensor(out=Pm[:, hs], in0=R2[:, hs], scalar=ginv, in1=CAUc, op0=ALU.mult, op1=ALU.mult),
                 reads=[R2B, CB], writes=[PMB])
        for h in range(6):
            hs = slice(h * 128, (h + 1) * 128)
            mm(R0[:, hs], Vt[:, hs], Pm[:, hs], True, True, [VTB, PMB], [R0B])
        for h in range(6):
            hs = slice(h * 128, (h + 1) * 128)
            mm(R1[:, hs], Sbf[:, hs], QTf[:, hs], True, True, [SBFB, QTFB], [R1B])
        for h in range(6):
            hs = slice(h * 128, (h + 1) * 128)
            mm(R3[:, hs], Kt[:, hs], Vt[:, hs], True, True, [KTB_, VTB], [R3B])
        for h in range(6):
            hs = slice(h * 128, (h + 1) * 128)
            ch = float(np.exp(128.0 * GAMMA_LOG[h]))
            S.op("dve", lambda e, hs=hs, ch=ch: e.scalar_tensor_tensor(out=S32[:, hs], in0=S32[:, hs], scalar=ch, in1=R3[:, hs], op0=ALU.mult, op1=ALU.add),
                 reads=[S32B, R3B], writes=[S32B])
        S.op("act", lambda e: e.copy(out=Sbf, in_=S32), reads=[S32B], writes=[SBFB])
        S.op("act", lambda e: e.copy(out=tO, in_=R0[:, 0:768]), reads=[R0B], writes=[TOB])
        S.op("dve", lambda e: e.tensor_tensor(out=tO, in0=R1[:, 0:768], in1=tO, op=ALU.add), reads=[R1B, TOB], writes=[TOB])
        act(sqo, tO, AF.Square, [TOB], [SQOB])
        for h in range(6):
            hs = slice(h * 128, (h + 1) * 128)
            mm(R2[:, hs], onesB[:], sqo[:, hs], True, True, [SQOB, CB], [R2B])
        act(tR, R2[:, 0:768], AF.Ln, [R2B], [TRB], scale=1.0 / 128, bias=EPS)
        act(tR, tR, AF.Exp, [TRB], [TRB], scale=-0.5)
        nxt()
        for h in range(6):
            hs = slice(h * 128, (h + 1) * 128)
            S.op("dve", lambda e, hs=hs, h=h: e.scalar_tensor_tensor(out=tC[:, hs], in0=tO[:, hs], scalar=col_rtn(j, h), in1=tR[:, hs], op0=ALU.mult, op1=ALU.mult),
                 reads=[TOB, TRB, VC], writes=[TCB])
        S.op("dve", lambda e, cat=cat: e.tensor_tensor(out=cat[:, 0:6, :], in0=hv(tC), in1=hv(tG), op=ALU.mult), reads=[TCB, TGB], writes=[catb])

    def hgrn_tile(l, j, t, nxt, cat, catb, prev):
        lb_bc = KIND[:, 0:768]
        nom_bc = KIND[:, 768:1536]
        act(tA, R1[:, 0:768], AF.Exp, [R1B], [TAB], scale=-1.0)
        S.op("dve", lambda e: e.scalar_tensor_tensor(out=tB, in0=tA, scalar=E30, in1=lb_bc, op0=ALU.min, op1=ALU.mult), reads=[TAB, KINDB], writes=[TBB])
        act(tB, tB, AF.Ln, [TBB], [TBB], bias=1.0)
        act(tA, tA, AF.Ln, [TAB], [TAB], bias=1.0)
        S.op("dve", lambda e: e.tensor_tensor(out=tB, in0=tB, in1=tA, op=ALU.subtract), reads=[TBB, TAB], writes=[TBB])
        act(tA, tA, AF.Exp, [TAB], [TAB], scale=-1.0)
        S.op("dve", lambda e: e.scalar_tensor_tensor(out=tA, in0=tA, scalar=1.0, in1=nom_bc, op0=ALU.subtract, op1=ALU.mult), reads=[TAB, KINDB], writes=[TAB])
        S.op("act", lambda e: e.copy(out=Vt, in_=R2[:, 0:768]), reads=[R2B], writes=[VTB])
        for (R, RB_, M) in ((R0, R0B, M1c), (R1, R1B, M2c), (R2, R2B, M4c)):
            for (a, n) in ((0, 512), (512, 256)):
                mm(R[:, a:a + n], M, tB[:, a:a + n], True, True, [CB, TBB], [RB_])
        act(tE, R0[:, 0:768], AF.Exp, [R0B], [TEB])
        S.op("dve", lambda e: e.tensor_tensor(out=Qt, in0=tC, in1=tE, op=ALU.mult), reads=[TCB, TEB], writes=[QTB_])
        act(tB, R0[:, 0:768], AF.Exp, [R0B], [TBB], scale=-1.0)
        S.op("dve", lambda e: e.tensor_tensor(out=Kt, in0=tA, in1=tB, op=ALU.mult), reads=[TAB, TBB], writes=[KTB_])
        act(tE, R1[:, 0:768], AF.Exp, [R1B], [TEB])
        S.op("dve", lambda e: e.tensor_tensor(out=Qot, in0=tC, in1=tE, op=ALU.mult), reads=[TCB, TEB], writes=[QOTB_])
        for h in range(6):
            hs = slice(h * 128, (h + 1) * 128)
            S.op("pe", lambda e, hs=hs: e.transpose(out=R0[:, hs], in_=tE[:, hs], identity=identF), reads=[TEB, CB], writes=[R0B])
        S.op("dve", lambda e: e.tensor_copy(out=dec.rearrange("p (h n) -> p h n", h=6),
                                            in_=R0[:, 0:768].rearrange("p (h n c) -> p h n c", h=6, n=4)[:, :, :, 31]), reads=[R0B], writes=[DECB])
        act(tB, R2[:, 0:768], AF.Exp, [R2B], [TBB])
        S.op("dve", lambda e: e.tensor_tensor(out=Kst, in0=tA, in1=tB, op=ALU.mult), reads=[TAB, TBB], writes=[KSTB])
        for h in range(6):
            hs = slice(h * 128, (h + 1) * 128)
            S.op("pe", lambda e, hs=hs: e.transpose(out=R1b[:, hs], in_=Qt[:, hs], identity=identB[:]), reads=[QTB_, CB], writes=[R1B])
            S.op("pe", lambda e, hs=hs: e.transpose(out=R2b[:, hs], in_=Kt[:, hs], identity=identB[:]), reads=[KTB_, CB], writes=[R2B])
            S.op("pe", lambda e, hs=hs: e.transpose(out=R3b[:, hs], in_=Qot[:, hs], identity=identB[:]), reads=[QOTB_, CB], writes=[R3B])
        S.op("act", lambda e: e.copy(out=QTf, in_=R1b[:, 0:768]), reads=[R1B], writes=[QTFB])
        S.op("dve", lambda e: e.tensor_copy(out=KTf, in_=R2b[:, 0:768]), reads=[R2B], writes=[KTFB])
        S.op("act", lambda e: e.copy(out=QoTf, in_=R3b[:, 0:768]), reads=[R3B], writes=[QOTFB])
        for h in range(6):
            hs = slice(h * 128, (h + 1) * 128)
            mm(R0[:, hs], KTf[:, hs], QTf[:, hs], True, True, [KTFB, QTFB], [R0B])
        S.op("dve", lambda e: e.tensor_tensor(out=hv(Pm), in0=hv(R0[:, 0:768]), in1=BDc.unsqueeze(1).to_broadcast([128, 6, 128]), op=ALU.mult),
             reads=[R0B, CB], writes=[PMB])
        for h in range(6):
            hs = slice(h * 128, (h + 1) * 128)
            mm(R1[:, hs], Vt[:, hs], Pm[:, hs], True, True, [VTB, PMB], [R1B])
        for n in range(4):
            ns = slice(32 * n, 32 * n + 32)
            for h in range(6):
                hs = slice(h * 128, (h + 1) * 128)
                cs = slice(h * 128 + 32 * n, h * 128 + 32 * n + 32)
                mm(R2[:, cs], Sbf[:, hs], QoTf[:, cs], True, True, [SBFB, QOTFB], [R2B])
            for h in range(6):
                hs = slice(h * 128, (h + 1) * 128)
                mm(R3[:, hs], Kst[ns, hs], Vt[ns, hs], True, True, [KSTB, VTB], [R3B], tile_position=(32 * n, 0))
            if prev is not None:
                w_out_mm(prev, R0, R0B, (2 * n, 2 * n + 1))
            for h in range(6):
                hs = slice(h * 128, (h + 1) * 128)
                S.op("dve", lambda e, hs=hs, h=h, n=n: e.scalar_tensor_tensor(out=S32[:, hs], in0=S32[:, hs], scalar=dec[:, h * 4 + n:h * 4 + n + 1], in1=R3[:, hs],
                                                                           op0=ALU.mult, op1=ALU.add), reads=[S32B, R3B, DECB], writes=[S32B])
            S.op("act", lambda e: e.copy(out=Sbf, in_=S32), reads=[S32B], writes=[SBFB])
        if prev is not None:
            w_out_res(prev, R0, R0B)
        S.op("act", lambda e: e.copy(out=tO, in_=R1[:, 0:768]), reads=[R1B], writes=[TOB])
        S.op("dve", lambda e: e.tensor_tensor(out=tO, in0=R2[:, 0:768], in1=tO, op=ALU.add), reads=[R2B, TOB], writes=[TOB])
        act(sqo, tO, AF.Square, [TOB], [SQOB])
        for h in range(6):
            hs = slice(h * 128, (h + 1) * 128)
            mm(R0[:, 0:128], onesB[:], sqo[:, hs], h == 0, h == 5, [SQOB, CB], [R0B])
        act(tR[:, 0:128], R0[:, 0:128], AF.Ln, [R0B], [TRB], scale=4.0 / MIXW, bias=4.0 * EPS)
        act(tR[:, 0:128], tR[:, 0:128], AF.Exp, [TRB], [TRB], scale=-0.5)
        nxt()
        for h in range(6):
            hs = slice(h * 128, (h + 1) * 128)
            S.op("dve", lambda e, hs=hs, h=h: e.scalar_tensor_tensor(out=tC[:, hs], in0=tO[:, hs], scalar=col_hgn(j, h), in1=tR[:, 0:128], op0=ALU.mult, op1=ALU.mult),
                 reads=[TOB, TRB, VC], writes=[TCB])
        S.op("dve", lambda e, cat=cat: e.scalar_tensor_tensor(out=cat[:, 0:6, :], in0=hv(tG), scalar=1.0, in1=hv(tC),
                                                              op0=ALU.add, op1=ALU.mult), reads=[TGB, TCB], writes=[catb])

    if do_mix and n_layers > 0:
        issue_mixer_weights(0)
    load_transposed(x_d, 16, xT, XB)

    for l in range(n_layers):
        kind = kinds[l] if kinds else ("hgrn" if l % 2 == 0 else "ret")
        j = l // 2
        if do_mix:
            S.barrier()
            mixer_layer(l, kind, j)
        if do_ffn:
            S.barrier()
            for b in range(4):
                rs, rsb = rms_stats_blk(lambda k, b=b: xT[:, k, b * 512:(b + 1) * 512], XB[4 * b:4 * b + 4], 512, 1.0 / D)
                for k in range(KC):
                    S.op("dve", lambda e, b=b, k=k, l=l, rs=rs: e.scalar_tensor_tensor(
                        out=hT[:, k, b * 512:(b + 1) * 512], in0=xT[:, k, b * 512:(b + 1) * 512], scalar=col_norm_ffn(l, k),
                        in1=rs, op0=ALU.mult, op1=ALU.mult), reads=XB[4 * b:4 * b + 4] + [rsb, VC], writes=[HB[b]])
            for (f0, nf) in GROUPS:
                S.dma("pool", lambda e, f0=f0, nf=nf, l=l: e.dma_start(
                    out=wo_sb[:, 0:nf, :], in_=w_fo_d[l, f0 * 128:(f0 + nf) * 128, :].rearrange("(f p) n -> p f n", p=128)), writes=[WO_B])
                for fi in range(nf):
                    f = f0 + fi
                    wi, wib = wi_rot.next()
                    S.dma("pool", lambda e, wi=wi, f=f, l=l: e.dma_start(
                        out=wi[:, 0], in_=w_fi_d[l, :, f * 128:(f + 1) * 128].rearrange("(k p) n -> p k n", p=128)), writes=[wib[0]])
                    S.dma("pool", lambda e, wi=wi, f=f, l=l: e.dma_start(
                        out=wi[:, 1], in_=w_fi_d[l, :, DFF + f * 128:DFF + (f + 1) * 128].rearrange("(k p) n -> p k n", p=128)), writes=[wib[1]])
                    for b in range(4):
                        pg, pgb = ps_all.next()
                        pu, pub = ps_all.next()
                        for k in range(KC):
                            S.op("pe", lambda e, pg=pg, wi=wi, k=k, b=b: e.matmul(pg, lhsT=wi[:, 0, k, :], rhs=hT[:, k, b * 512:(b + 1) * 512],
                                                                             start=(k == 0), stop=(k == KC - 1)), reads=[wib[0], HB[b]], writes=[pgb])
                        for k in range(KC):
                            S.op("pe", lambda e, pu=pu, wi=wi, k=k, b=b: e.matmul(pu, lhsT=wi[:, 1, k, :], rhs=hT[:, k, b * 512:(b + 1) * 512],
                                                                             start=(k == 0), stop=(k == KC - 1)), reads=[wib[1], HB[b]], writes=[pub])
                        sl, slb = silu_rot.next()
                        S.op("act", lambda e, sl=sl, pg=pg: e.activation(out=sl, in_=pg, func=AF.Silu), reads=[pgb], writes=[slb])
                        S.op("dve", lambda e, sl=sl, pu=pu, fi=fi, b=b: e.tensor_tensor(out=aT[:, fi, b * 512:(b + 1) * 512], in0=pu, in1=sl, op=ALU.mult),
                             reads=[pub, slb], writes=[AB[fi][b]])
                if (f0, nf) == GROUPS[-1] and do_mix and l + 1 < n_layers:
                    for i in range(2):
                        S.dma("pool", lambda e, i=i, l=l: e.dma_start(out=w_in_sb[:, 2 * i:2 * i + 2, :],
                                                                in_=w_in_d[l + 1, 256 * i:256 * (i + 1), :].rearrange("(k p) n -> p k n", p=128)),
                              writes=[WIN_B[i]] + HB)
                        prefetched.add((l + 1, i))
                for b in range(4):
                    for dc in range(KC):
                        py, pyb = ps_all.next()
                        for fi in range(nf):
                            S.op("pe", lambda e, py=py, fi=fi, dc=dc, b=b, nf=nf: e.matmul(py, lhsT=wo_sb[:, fi, dc * 128:(dc + 1) * 128],
                                                                                      rhs=aT[:, fi, b * 512:(b + 1) * 512], start=(fi == 0), stop=(fi == nf - 1)),
                                 reads=[WO_B, AB[fi][b]], writes=[pyb])
                        S.op("dve", lambda e, py=py, dc=dc, b=b: e.tensor_tensor(out=xT[:, dc, b * 512:(b + 1) * 512], in0=py,
                                                                                in1=xT[:, dc, b * 512:(b + 1) * 512], op=ALU.add),
                             reads=[pyb] + XB[4 * b:4 * b + 4], writes=XB[4 * b:4 * b + 4])

    S.barrier()
    for b in range(4):
        rs, rsb = rms_stats_blk(lambda k, b=b: xT[:, k, b * 512:(b + 1) * 512], XB[4 * b:4 * b + 4], 512, 1.0 / D)
        for tt in range(4):
            t = 4 * b + tt
            yT, ytb = yT_rot.next()
            for k in range(KC):
                S.op("dve", lambda e, yT=yT, k=k, t=t, tt=tt, rs=rs: e.scalar_tensor_tensor(
                    out=yT[:, k, :], in0=xT[:, k, t * 128:(t + 1) * 128], scalar=col_norm_fin(k),
                    in1=rs[:, tt * 128:(tt + 1) * 128], op0=ALU.mult, op1=ALU.mult), reads=[XB[t], rsb, VC], writes=[ytb])
            stg, stgb = stage_rot.next()
            for half in range(2):
                ps, pb = ps_all.next()
                for jj in range(4):
                    k = half * 4 + jj
                    S.op("pe", lambda e, ps=ps, yT=yT, jj=jj, k=k: e.transpose(out=ps[:, jj * 128:(jj + 1) * 128], in_=yT[:, k, :], identity=identF),
                         reads=[ytb, CB], writes=[pb])
                if half == 0:
                    S.op("act", lambda e, ps=ps, stg=stg, half=half: e.copy(out=stg[:, half * 512:(half + 1) * 512], in_=ps), reads=[pb], writes=[stgb])
                else:
                    S.op("dve", lambda e, ps=ps, stg=stg, half=half: e.tensor_copy(out=stg[:, half * 512:(half + 1) * 512], in_=ps), reads=[pb], writes=[stgb])
            outs.append(S.dma("sp", lambda e, stg=stg, t=t: e.dma_start(out=out_d[t * 128:(t + 1) * 128, :], in_=stg), reads=[stgb], semkey="out"))

    S.run_block(final_waits=outs)
    S.close()
    es.close()
    return nc


def _consts():
    c = np.zeros((128, 1024), np.float64)
    c[:, 0:128] = np.eye(128)
    s = np.arange(128)[:, None]
    t = np.arange(128)[None, :]
    same = (s // 32) == (t // 32)
    cs, ct = s % 32, t % 32
    c[:, 128:256] = same * ((cs <= ct).astype(np.float64) - (cs <= 15).astype(np.float64))
    c[:, 256:384] = same * (cs <= ct)
    c[:, 384:512] = same * (cs > ct)
    c[:, 512:640] = same * (s <= t)
    c[:, 640:768] = (s <= t)
    p = np.arange(128)
    for h in range(6):
        lg = GAMMA_LOG[h]
        c[:, 768 + h] = np.exp(lg * (p + 1.0))
        c[:, 774 + h] = np.exp(lg * (127.0 - p)) * (128.0 ** -0.5)
    inv = (np.float32(10000.0) ** (-np.linspace(0.0, 1.0, 64, dtype=np.float32))).astype(np.float32)
    c[:, 832:896] = inv[None, :]
    return c.astype(np.float32)


_NC_CACHE = {}


def make_in_maps(inputs, n_cores=8):
    x = np.asarray(inputs["x"], np.float32)
    mem = np.asarray(inputs["mem"], np.float32)
    pos = np.asarray(inputs["positions"], np.int32)
    f = lambda k: np.ascontiguousarray(np.asarray(inputs[k], np.float32))
    shared = {k: f(k) for k in ("norm_mix", "w_in", "w_out", "norm_mem", "w_mem_kv", "hgrn_lb_logits", "hgrn_out_norm",
                                "norm_ffn", "w_ffn_in", "w_ffn_out")}
    shared["ret_out_norm"] = np.ascontiguousarray(np.asarray(inputs["ret_out_norm"], np.float32).reshape(2, MIXW))
    shared["norm_final"] = np.ascontiguousarray(np.asarray(inputs["norm_final"], np.float32).reshape(1, D))
    shared["consts"] = _consts()
    maps = []
    for c in range(n_cores):
        m = dict(shared)
        m["x"] = np.ascontiguousarray(x[c])
        m["mem"] = np.ascontiguousarray(mem[c])
        m["positions"] = np.ascontiguousarray(pos[c].reshape(16, 128))
        maps.append(m)
    return maps


def kernel(**inputs):
    if "full" not in _NC_CACHE:
        _NC_CACHE["full"] = build()
    nc = _NC_CACHE["full"]
    maps = make_in_maps(inputs, 8)
    res = run_bass_kernel_spmd(nc, maps, core_ids=list(range(8)))
    return np.stack([np.asarray(r["out"], np.float32) for r in res.results], axis=0)
```

```python
import numpy as np
from contextlib import ExitStack
import concourse.bass as bass
import concourse.mybir as mybir
from concourse.bass_utils import run_bass_kernel_spmd

F32 = mybir.dt.float32
BF16 = mybir.dt.bfloat16
I32 = mybir.dt.int32
AF = mybir.ActivationFunctionType
ALU = mybir.AluOpType

D = 1024
T = 2048
NL = 4
KC = 8
MIXW = 768
INW = 3328
DFF = 2816
FC = 22
NMEM = 256
EPS = 1e-6


class Buf:
    __slots__ = ("name", "w", "r", "kids")

    def __init__(self, name="", kids=()):
        self.name = name
        self.w = None
        self.r = []
        self.kids = tuple(kids)


def _expand(bufs):
    out = []
    for b in bufs:
        out.append(b)
        out.extend(b.kids)
    return out


class Op:
    __slots__ = ("eng", "fn", "deps", "sig", "signal", "dma", "idx")


class Sched:
    ENGS = ("pe", "act", "dve", "pool", "sp")

    def __init__(self, nc):
        self.nc = nc
        self.ops = {e: [] for e in self.ENGS}
        self.n = 0
        self.phase = 0
        self.dma_sems = {}
        self.eng_sems = {}
        self._sem_ctx = []
        self.pending_dma = []

    def _new_sem(self, name):
        cm = self.nc.semaphore(name)
        s = cm.__enter__()
        self._sem_ctx.append(cm)
        return s

    def close(self):
        for cm in reversed(self._sem_ctx):
            cm.__exit__(None, None, None)

    def _deps(self, op, reads, writes):
        reads = _expand(reads)
        writes = _expand(writes)
        deps = []
        for b in reads:
            if b.w is not None:
                deps.append(b.w)
        for b in writes:
            if b.w is not None:
                deps.append(b.w)
            deps.extend(b.r)
        for b in reads:
            b.r.append(op)
        for b in writes:
            b.w = op
            b.r = []
        out = []
        seen = set()
        for d in deps:
            if d is op or id(d) in seen:
                continue
            seen.add(id(d))
            if d.eng == "pe" and op.eng == "pe" and not d.dma and not op.dma:
                continue
            out.append(d)
        return out

    def op(self, eng, fn, reads=(), writes=(), extra=()):
        o = Op()
        o.eng = eng
        o.fn = fn
        o.dma = False
        o.sig = False
        o.idx = self.n
        self.n += 1
        o.deps = self._deps(o, reads, writes) + list(extra)
        for d in o.deps:
            d.sig = True
        o.signal = (eng, self.phase)
        self.ops[eng].append(o)
        return o

    def dma(self, queue, fn, reads=(), writes=(), semkey=None):
        o = Op()
        o.eng = queue
        o.fn = fn
        o.dma = True
        o.sig = True
        o.idx = self.n
        self.n += 1
        o.deps = self._deps(o, reads, writes)
        for d in o.deps:
            d.sig = True
        if semkey is None:
            semkey = ("dma", id(writes[0]))
        if semkey not in self.dma_sems:
            self.dma_sems[semkey] = [self._new_sem("d%d" % len(self.dma_sems)), 0]
        ent = self.dma_sems[semkey]
        ent[1] += 16
        o.signal = (ent[0], ent[1])
        self.ops[queue].append(o)
        self.pending_dma.append(o)
        return o

    def barrier(self):
        lasts = []
        for e in self.ENGS:
            for o in reversed(self.ops[e]):
                if not o.dma:
                    lasts.append(o)
                    break
        lasts += self.pending_dma
        self.pending_dma = []
        for e in self.ENGS:
            self.op(e, lambda eng: eng.nop(), extra=[d for d in lasts])
        self.phase += 1

    def finalize(self):
        counters = {}
        for e in self.ENGS:
            for o in self.ops[e]:
                if o.dma:
                    continue
                if o.sig:
                    key = o.signal
                    if key not in self.eng_sems:
                        self.eng_sems[key] = self._new_sem("e_%s_%d" % key)
                    counters[key] = counters.get(key, 0) + 1
                    o.signal = (self.eng_sems[key], counters[key])
                else:
                    o.signal = None

    def emit(self, ename, eng):
        waited = {}
        for o in self.ops[ename]:
            best = {}
            for d in o.deps:
                sem, val = d.signal
                k = id(sem)
                if waited.get(k, 0) < val:
                    waited[k] = val
                    best[k] = (sem, val)
            ws = list(best.values())
            for sem, val in ws[1:]:
                eng.wait_ge(sem, val)
            ins = o.fn(eng)
            if ws:
                ins._wait_ge(ws[0][0], ws[0][1])
            if o.dma:
                ins.then_inc(o.signal[0], 16)
            elif o.sig:
                ins.then_inc(o.signal[0], 1)

    def run_block(self, final_waits=()):
        self.finalize()
        nc = self.nc
        sch = self
        with nc.Block() as block:
            @block.tensor
            def _(e):
                sch.emit("pe", e)

            @block.scalar
            def _(e):
                sch.emit("act", e)

            @block.vector
            def _(e):
                sch.emit("dve", e)

            @block.gpsimd
            def _(e):
                sch.emit("pool", e)

            @block.sync
            def _(e):
                sch.emit("sp", e)
                best = {}
                for o in final_waits:
                    sem, val = o.signal
                    if id(sem) not in best or best[id(sem)][1] < val:
                        best[id(sem)] = (sem, val)
                for sem, val in best.values():
                    e.wait_ge(sem, val)


class Rot:
    def __init__(self, items):
        self.items = items
        self.i = 0

    def next(self):
        it = self.items[self.i % len(self.items)]
        self.i += 1
        return it


GF = 6
GROUPS = [(0, 6), (6, 6), (12, 6), (18, 4)]
GAMMA_LOG = [float(np.log(np.float32(1.0) - np.float32(2.0) ** np.float32(-5.0 - h))) for h in range(6)]
E30 = float(np.exp(30.0))
PI = float(np.pi)


def build(n_layers=NL, do_mix=True, do_ffn=True, kinds=None):
    nc = bass.Bass("TRN2", target_bir_lowering=False)

    def din(name, shape, dt=F32):
        return nc.dram_tensor(name, shape, dt, kind="ExternalInput").ap()

    x_d = din("x", [T, D])
    mem_d = din("mem", [NMEM, D])
    pos_d = din("positions", [16, 128], I32)
    norm_mix_d = din("norm_mix", [NL, D])
    w_in_d = din("w_in", [NL, D, INW])
    w_out_d = din("w_out", [NL, D, D])
    norm_mem_d = din("norm_mem", [NL, D])
    w_kv_d = din("w_mem_kv", [NL, D, 512])
    lb_d = din("hgrn_lb_logits", [2, MIXW])
    hgn_d = din("hgrn_out_norm", [2, MIXW])
    rtn_d = din("ret_out_norm", [2, MIXW])
    norm_ffn_d = din("norm_ffn", [NL, D])
    w_fi_d = din("w_ffn_in", [NL, D, 2 * DFF])
    w_fo_d = din("w_ffn_out", [NL, DFF, D])
    norm_fin_d = din("norm_final", [1, D])
    consts_d = din("consts", [128, 1024])
    out_d = nc.dram_tensor("out", [T, D], F32, kind="ExternalOutput").ap()

    es = ExitStack()

    def sb(name, shape, dt):
        return es.enter_context(nc.sbuf_tensor(name, shape, dt))

    S = Sched(nc)

    xT = sb("xT", [128, KC, T], F32)
    XB = [Buf("x%d" % i) for i in range(16)]
    U1 = sb("U1", [128, 34816], BF16)
    U2W = 12320
    U2 = sb("U2", [128, U2W], F32)
    U2b = U2[:].bitcast(BF16)
    cst = sb("cst", [128, 1024], F32)
    identF = cst[:, 0:128]
    M1c, M2c, M4c, BDc, CAUc = (cst[:, 128 * i:128 * (i + 1)] for i in range(1, 6))
    sqc = cst[:, 768:774]
    skc = cst[:, 774:780]
    invf = cst[:, 832:896]
    identB = sb("identB", [128, 128], BF16)
    onesB = sb("onesB", [128, 128], BF16)
    vcol = sb("vcol", [128, 128], F32)
    KIND = sb("KIND", [128, 2048], F32)
    KINDB = Buf("KIND")
    xatt = sb("xatt", [128, 2304], BF16)
    KTpad = xatt[:, 0:1024].rearrange("p (h m) -> p h m", h=4)
    Vpad = xatt[:, 1024:2048].rearrange("p (t h c) -> p t h c", t=2, h=4)
    onespad = xatt[:, 2048:2304].rearrange("p (a c) -> p a c", a=2)
    XAB = Buf("xatt")
    OPB = Buf("onespad")
    CB = Buf("consts")
    VC = Buf("vcol")
    sqt = sb("sqt", [128, 2, 512], BF16)
    sq_rot = Rot([(sqt[:, i, :], Buf("sq%d" % i)) for i in range(2)])
    rstd_t = sb("rstd", [128, 2, 512], F32)
    rstd_rot = Rot([(rstd_t[:, i, :], Buf("rstd%d" % i)) for i in range(2)])

    PSA = es.enter_context(nc.psum_tensor("psa", [128, 4096], F32))
    PSAb = PSA[:].bitcast(BF16)
    PB = [Buf("ps%d" % i) for i in range(8)]
    ps_all = Rot([(PSA[:, 512 * i:512 * (i + 1)], PB[i]) for i in range(8)])

    w_in_sb = U1[:, 0:KC * INW].rearrange("p (k n) -> p k n", k=KC)
    w_out_sb = U1[:, KC * INW:KC * INW + KC * D].rearrange("p (k n) -> p k n", k=KC)
    WIN_B = [Buf("win%d" % i) for i in range(4)]
    WOUT_B = Buf("wout")
    hT = U1[:, 0:KC * T].rearrange("p (k t) -> p k t", k=KC)
    HB = [Buf("h%d" % i) for i in range(4)]
    aT = U1[:, KC * T:KC * T + GF * T].rearrange("p (f t) -> p f t", f=GF)
    AB = [[Buf("a%d_%d" % (f, b)) for b in range(4)] for f in range(GF)]
    wo_sb = U2b[:, 0:GF * D].rearrange("p (f n) -> p f n", f=GF)
    WO_B = Buf("wo")
    NWI = 3
    wi_sb = [U2b[:, GF * D + i * 2048:GF * D + (i + 1) * 2048].rearrange("p (g k n) -> p g k n", g=2, k=KC) for i in range(NWI)]
    wi_rot = Rot([(wi_sb[i], (Buf("wig%d" % i), Buf("wiu%d" % i))) for i in range(NWI)])
    st0 = GF * D + NWI * 2048
    silu_rot = Rot([(U2b[:, st0 + i * 512:st0 + (i + 1) * 512], Buf("sl%d" % i)) for i in range(4)])
    stage_rot = Rot([(U2[:, i * 1024:(i + 1) * 1024], Buf("stg%d" % i)) for i in range(2)])
    yT_rot = Rot([(U2[:, 2048 + i * 1024:2048 + (i + 1) * 1024].rearrange("p (k t) -> p k t", k=KC), Buf("yT%d" % i)) for i in range(2)])
    vrows = U2[:, 4096:4224]
    VR = Buf("vrows")

    _o = [0]

    def u2f(n):
        a = U2[:, _o[0]:_o[0] + n]
        _o[0] += n
        return a

    def u2b(n):
        a = U2b[:, 2 * _o[0]:2 * _o[0] + n]
        _o[0] += (n + 1) // 2
        return a
    hTt = u2b(1024).rearrange("p (k t) -> p k t", k=KC); HTB = Buf("hTt")
    catTs = [u2b(1024).rearrange("p (k t) -> p k t", k=KC) for _ in range(2)]
    CATBs = [Buf("catT0"), Buf("catT1")]
    tA = u2f(768); TAB = Buf("tA")
    tB = u2f(768); TBB = Buf("tB")
    tC = u2f(768); TCB = Buf("tC")
    tE = u2f(768); TEB = Buf("tE")
    tO = u2f(768); TOB = Buf("tO")
    tG = u2f(768); TGB = Buf("tG")
    tR = u2f(768); TRB = Buf("tR")
    Qt = u2b(768); QTB_ = Buf("Qt")
    Kt = u2b(768); KTB_ = Buf("Kt")
    Qot = u2b(768); QOTB_ = Buf("Qot")
    Kst = u2b(768); KSTB = Buf("Kst")
    Vt = u2b(768); VTB = Buf("Vt")
    QTf = u2b(768); QTFB = Buf("QTf")
    KTf = u2b(768); KTFB = Buf("KTf")
    QoTf = u2b(768); QOTFB = Buf("QoTf")
    Pm = u2b(768); PMB = Buf("Pm")
    sqo = Pm; SQOB = PMB
    S32H = [Buf("S32h%d" % h) for h in range(6)]
    SBFH = [Buf("Sbfh%d" % h) for h in range(6)]
    S32 = u2f(768); S32B = Buf("S32", kids=S32H)
    Sbf = u2b(768); SBFB = Buf("Sbf", kids=SBFH)
    dec = u2f(24); DECB = Buf("dec")
    qxT = Pm[:, 0:256]; QXB = PMB
    expT = u2b(1024); EXB = Buf("expT")
    rsx = u2f(256); RSXB = Buf("rsx")
    setup_base = _o[0]
    assert _o[0] <= U2W, _o[0]
    memst = [U2[:, 2048 + i * 1024:2048 + (i + 1) * 1024] for i in range(2)]
    MSB = [Buf("memst%d" % i) for i in range(2)]
    memh = U2b[:, 2 * 4096:2 * 4096 + 2048].rearrange("p (t n) -> p t n", t=2)
    MHB = Buf("memh")
    memhT = U2b[:, 2 * 5120:2 * 5120 + 2048].rearrange("p (k m) -> p k m", k=KC)
    MHTB = Buf("memhT")
    wkv = U2b[:, 2 * 6144:2 * 6144 + 4096].rearrange("p (k n) -> p k n", k=KC)
    WKVB = Buf("wkv")
    mscr = U2[:, 8192:8192 + 1024]
    MSCB = Buf("mscr")
    angt = U2[:, 9216:9216 + 1024].rearrange("p (t j) -> p t j", t=16)
    ANGB = Buf("ang")
    posr = U2[:, 10240:10240 + 128]
    posi = U2[:, 10368:10368 + 128].bitcast(I32)
    posf = U2[:, 10496:10496 + 16]
    POSB = Buf("pos")
    assert 10512 <= U2W

    R0 = PSA[:, 0:1024]; R0B = Buf("R0")
    R1 = PSA[:, 1024:2048]; R1B = Buf("R1")
    R2H = [Buf("R2h%d" % h) for h in range(6)]
    R3H = [Buf("R3h%d" % h) for h in range(6)]
    R2 = PSA[:, 2048:3072]; R2B = Buf("R2", kids=R2H)
    R3 = PSA[:, 3072:4096]; R3B = Buf("R3", kids=R3H)
    R0b = PSAb[:, 0:2048]; R1b = PSAb[:, 2048:4096]; R2b = PSAb[:, 4096:6144]; R3b = PSAb[:, 6144:8192]
    ALLPS = PB

    def rbufs(*rb):
        return list(rb)

    outs = []

    S.dma("sp", lambda e: e.dma_start(out=cst[:], in_=consts_d), writes=[CB])
    S.op("dve", lambda e: e.tensor_copy(out=identB[:], in_=identF), reads=[CB], writes=[CB])
    S.op("pool", lambda e: e.memset(onesB[:], 1.0), writes=[CB])
    S.op("pool", lambda e: e.memset(xatt[:], 0.0), writes=[XAB, OPB])
    S.op("pool", lambda e: e.memset(onespad[:, 0, 0:64], 1.0), writes=[OPB])
    S.op("pool", lambda e: e.memset(onespad[:, 1, 64:128], 1.0), writes=[OPB])
    S.op("pool", lambda e: e.memset(vrows, 0.0), writes=[VR])
    for i, (src, r0, n) in enumerate([(norm_mix_d, 0, 32), (norm_ffn_d, 32, 32), (norm_mem_d, 64, 32), (norm_fin_d, 96, 8), (hgn_d, 104, 12), (rtn_d, 116, 12)]):
        S.dma("sp", lambda e, src=src, r0=r0, n=n: e.dma_start(out=vrows[r0:r0 + n, :], in_=src.rearrange("l (k p) -> (l k) p", p=128)),
              writes=[VR], semkey="vr")
    S.op("pe", lambda e: e.transpose(out=PSA[:, 0:128], in_=vrows, identity=identF), reads=[VR, CB], writes=[PB[0]])
    S.op("dve", lambda e: e.tensor_copy(out=vcol[:], in_=PSA[:, 0:128]), reads=[PB[0]], writes=[VC])

    def col_norm_mix(l, k): return vcol[:, l * 8 + k:l * 8 + k + 1]
    def col_norm_ffn(l, k): return vcol[:, 32 + l * 8 + k:32 + l * 8 + k + 1]
    def col_norm_mem(l, k): return vcol[:, 64 + l * 8 + k:64 + l * 8 + k + 1]
    def col_norm_fin(k): return vcol[:, 96 + k:96 + k + 1]
    def col_hgn(j, h): return vcol[:, 104 + j * 6 + h:104 + j * 6 + h + 1]
    def col_rtn(j, h): return vcol[:, 116 + j * 6 + h:116 + j * 6 + h + 1]

    S.barrier()

    def load_transposed(src_d, ntile, dst, dst_bufs):
        for t in range(ntile):
            stg, stgb = stage_rot.next()
            S.dma("sp", lambda e, stg=stg, t=t: e.dma_start(out=stg, in_=src_d[t * 128:(t + 1) * 128, :]), writes=[stgb])
            for half in range(2):
                ps, pb = ps_all.next()
                for j in range(4):
                    k = half * 4 + j
                    S.op("pe", lambda e, ps=ps, stg=stg, j=j, k=k: e.transpose(out=ps[:, j * 128:(j + 1) * 128], in_=stg[:, k * 128:(k + 1) * 128], identity=identF),
                         reads=[stgb, CB], writes=[pb])
                if half == 0:
                    S.op("act", lambda e, ps=ps, half=half, t=t: e.copy(out=dst[:, half * 4:half * 4 + 4, t * 128:(t + 1) * 128],
                                                                       in_=ps.rearrange("p (k c) -> p k c", k=4)), reads=[pb], writes=[dst_bufs[t]])
                else:
                    S.op("dve", lambda e, ps=ps, half=half, t=t: e.tensor_copy(out=dst[:, half * 4:half * 4 + 4, t * 128:(t + 1) * 128],
                                                                              in_=ps.rearrange("p (k c) -> p k c", k=4)), reads=[pb], writes=[dst_bufs[t]])

    def rms_stats_blk(src_fn, src_bufs, width, scale, psreg=None):
        ps, pb = psreg if psreg is not None else ps_all.next()
        for k in range(KC):
            sq, sqb = sq_rot.next()
            S.op("act", lambda e, sq=sq, k=k: e.activation(out=sq[:, 0:width], in_=src_fn(k), func=AF.Square), reads=src_bufs, writes=[sqb])
            S.op("pe", lambda e, ps=ps, sq=sq, k=k: e.matmul(ps[:, 0:width], lhsT=onesB[:], rhs=sq[:, 0:width], start=(k == 0), stop=(k == KC - 1)),
                 reads=[sqb, CB], writes=[pb])
        rs, rsb = rstd_rot.next()
        S.op("act", lambda e, ps=ps, rs=rs: e.activation(out=rs[:, 0:width], in_=ps[:, 0:width], func=AF.Ln, scale=scale, bias=EPS), reads=[pb], writes=[rsb])
        S.op("act", lambda e, rs=rs: e.activation(out=rs[:, 0:width], in_=rs[:, 0:width], func=AF.Exp, scale=-0.5), reads=[rsb], writes=[rsb])
        return rs, rsb

    def mm(out, lhsT, rhs, start, stop, reads, writes, **kw):
        S.op("pe", lambda e: e.matmul(out, lhsT=lhsT, rhs=rhs, start=start, stop=stop, **kw), reads=reads, writes=writes)

    def act(out, in_, func, reads, writes, **kw):
        S.op("act", lambda e: e.activation(out=out, in_=in_, func=func, **kw), reads=reads, writes=writes)

    def hv(ap):
        return ap.rearrange("p (h c) -> p h c", h=6)

    weights_issued = set()
    prefetched = set()

    def issue_mixer_weights(l):
        if l in weights_issued:
            return
        weights_issued.add(l)
        S.dma("pool", lambda e: e.dma_start(out=wkv, in_=w_kv_d[l].rearrange("(k p) n -> p k n", p=128)), writes=[WKVB])
        for i in range(4):
            if (l, i) in prefetched:
                continue
            S.dma("pool", lambda e, i=i: e.dma_start(out=w_in_sb[:, 2 * i:2 * i + 2, :],
                                                    in_=w_in_d[l, 256 * i:256 * (i + 1), :].rearrange("(k p) n -> p k n", p=128)), writes=[WIN_B[i]])
        S.dma("pool", lambda e: e.dma_start(out=w_out_sb, in_=w_out_d[l].rearrange("(k p) n -> p k n", p=128)), writes=[WOUT_B])

    def mixer_layer(l, kind, j):
        issue_mixer_weights(l)
        if kind == "ret":
            S.dma("sp", lambda e: e.dma_start(out=posi[0:16, :], in_=pos_d), writes=[POSB])
            S.op("dve", lambda e: e.tensor_copy(out=posr[0:16, :], in_=posi[0:16, :]), reads=[POSB], writes=[POSB])
            S.op("pe", lambda e: e.transpose(out=R3[:, 0:16], in_=posr[0:16, :], identity=identF[0:16, 0:16]), reads=[POSB, CB], writes=[R3B])
            S.op("dve", lambda e: e.tensor_copy(out=posf, in_=R3[:, 0:16]), reads=[R3B], writes=[POSB])
            for t in range(16):
                S.op("dve", lambda e, t=t: e.tensor_scalar(out=angt[:, t, :], in0=invf, scalar1=posf[:, t:t + 1], scalar2=None, op0=ALU.mult), reads=[POSB, CB], writes=[ANGB])
            angf = angt.rearrange("p t j -> p (t j)")
            scr2 = U2[:, 1024:2048]
            scr2i = scr2.bitcast(I32)
            for (off, dst0) in ((0.5 * PI, 0), (0.0, 1024)):
                S.op("dve", lambda e, off=off: e.tensor_scalar(out=mscr, in0=angf, scalar1=off, scalar2=None, op0=ALU.add), reads=[ANGB], writes=[MSCB])
                S.op("dve", lambda e: e.tensor_scalar(out=scr2, in0=mscr, scalar1=1.0 / (2 * PI), scalar2=None, op0=ALU.mult), reads=[MSCB], writes=[TAB, TBB])
                S.op("dve", lambda e: e.tensor_copy(out=scr2i, in_=scr2), reads=[TAB, TBB], writes=[TAB, TBB])
                S.op("dve", lambda e: e.tensor_copy(out=scr2, in_=scr2i), reads=[TAB, TBB], writes=[TAB, TBB])
                S.op("dve", lambda e: e.scalar_tensor_tensor(out=mscr, in0=scr2, scalar=-2 * PI, in1=mscr, op0=ALU.mult, op1=ALU.add), reads=[TAB, TBB, MSCB], writes=[MSCB])
                S.op("dve", lambda e: e.tensor_scalar(out=scr2, in0=mscr, scalar1=PI, scalar2=2 * PI, op0=ALU.is_gt, op1=ALU.mult), reads=[MSCB], writes=[TAB, TBB])
                S.op("dve", lambda e: e.tensor_tensor(out=mscr, in0=mscr, in1=scr2, op=ALU.subtract), reads=[TAB, TBB, MSCB], writes=[MSCB])
                S.op("dve", lambda e: e.tensor_scalar(out=mscr, in0=mscr, scalar1=-PI, scalar2=PI, op0=ALU.max, op1=ALU.min), reads=[MSCB], writes=[MSCB])
                act(KIND[:, dst0:dst0 + 1024], mscr, AF.Sin, [MSCB], [KINDB])
        elif kind == "hgrn":
            lb_bc = KIND[:, 0:768]
            nom_bc = KIND[:, 768:1536]
            if j == 0:
                S.op("pool", lambda e: e.memset(lb_bc, 0.0), writes=[KINDB])
                S.op("pool", lambda e: e.memset(nom_bc, -1.0), writes=[KINDB])
            else:
                S.dma("sp", lambda e: e.dma_start(out=mscr[:, 0:768], in_=lb_d[0:1, :].partition_broadcast(128)), writes=[MSCB])
                S.dma("sp", lambda e: e.dma_start(out=angt.rearrange("p t j -> p (t j)")[:, 0:768], in_=lb_d[1:2, :].partition_broadcast(128)), writes=[ANGB])
                S.op("dve", lambda e: e.tensor_tensor(out=mscr[:, 0:768], in0=mscr[:, 0:768], in1=angt.rearrange("p t j -> p (t j)")[:, 0:768], op=ALU.subtract),
                     reads=[MSCB, ANGB], writes=[MSCB])
                act(mscr[:, 0:768], mscr[:, 0:768], AF.Exp, [MSCB], [MSCB])
                S.op("dve", lambda e: e.tensor_scalar(out=mscr[:, 0:768], in0=mscr[:, 0:768], scalar1=1.0, scalar2=None, op0=ALU.add), reads=[MSCB], writes=[MSCB])
                S.op("dve", lambda e: e.reciprocal(out=lb_bc, in_=mscr[:, 0:768]), reads=[MSCB], writes=[KINDB])
                S.op("dve", lambda e: e.tensor_scalar(out=nom_bc, in0=lb_bc, scalar1=-1.0, scalar2=None, op0=ALU.add), reads=[KINDB], writes=[KINDB])
        BIS = 99
        angf_ = angt.rearrange("p t j -> p (t j)")
        if BIS >= 1:
            S.dma("sp", lambda e: e.dma_start(out=angf_[0:1, :], in_=norm_mem_d[l:l + 1, :]), writes=[ANGB])
            for hh in range(2):
                S.op("pe", lambda e, hh=hh: e.matmul(R3[:, hh * 512:(hh + 1) * 512], lhsT=cst[0:1, 640:768], rhs=angf_[0:1, hh * 512:(hh + 1) * 512],
                                                   start=True, stop=True), reads=[ANGB, CB], writes=[R3B])
            S.op("dve", lambda e: e.tensor_copy(out=mscr, in_=R3), reads=[R3B], writes=[MSCB])
        for mt in range(2 if BIS >= 2 else 0):
            S.dma("sp", lambda e, mt=mt: e.dma_start(out=memst[mt], in_=mem_d[mt * 128:(mt + 1) * 128, :]), writes=[MSB[mt]])
            S.op("pool", lambda e, mt=mt: e.memset(posf[:, mt:mt + 1], 0.0), writes=[POSB])
            act(angf_, memst[mt], AF.Square, [MSB[mt], POSB], [ANGB, POSB], accum_out=posf[:, mt:mt + 1])
            act(posf[:, mt:mt + 1], posf[:, mt:mt + 1], AF.Sqrt, [POSB], [POSB], scale=1.0 / D, bias=EPS)
            S.op("dve", lambda e, mt=mt: e.reciprocal(out=posf[:, mt:mt + 1], in_=posf[:, mt:mt + 1]), reads=[POSB], writes=[POSB])
            S.op("dve", lambda e, mt=mt: e.scalar_tensor_tensor(out=memh[:, mt, :], in0=memst[mt], scalar=posf[:, mt:mt + 1], in1=mscr,
                                                               op0=ALU.mult, op1=ALU.mult), reads=[MSB[mt], POSB, MSCB], writes=[MHB])
            if BIS >= 3:
                for k in range(KC):
                    S.op("pe", lambda e, mt=mt, k=k: e.transpose(out=R0b[:, mt * 1024 + k * 128:mt * 1024 + (k + 1) * 128], in_=memh[:, mt, k * 128:(k + 1) * 128],
                                                                identity=identB[:]), reads=[MHB, CB], writes=[R0B])
                S.op("act", lambda e, mt=mt: e.copy(out=memhT[:, :, mt * 128:(mt + 1) * 128], in_=R0b[:, mt * 1024:(mt + 1) * 1024].rearrange("p (k c) -> p k c", k=KC)),
                     reads=[R0B], writes=[MHTB])
        for c in range(2 if BIS >= 4 else 0):
            for k in range(KC):
                mm(R1[:, c * 256:(c + 1) * 256], wkv[:, k, c * 128:(c + 1) * 128], memhT[:, k, :], k == 0, k == KC - 1, [WKVB, MHTB], [R1B])
        if BIS >= 4:
            for h in range(4):
                c, po = h // 2, 64 * (h % 2)
                S.op("act", lambda e, h=h, c=c, po=po: e.copy(out=KTpad[po:po + 64, h, :], in_=R1[po:po + 64, c * 256:(c + 1) * 256]), reads=[R1B], writes=[XAB])
        for mt in range(2 if BIS >= 5 else 0):
            for k in range(KC):
                mm(R2[:, mt * 256:(mt + 1) * 256], memhT[:, k, mt * 128:(mt + 1) * 128], wkv[:, k, 256:512], k == 0, k == KC - 1, [WKVB, MHTB], [R2B])
        for mt in range(2 if BIS >= 5 else 0):
            for h in range(4):
                S.op("dve", lambda e, mt=mt, h=h: e.tensor_copy(out=Vpad[:, mt, h, (h % 2) * 64:(h % 2) * 64 + 64], in_=R2[:, mt * 256 + h * 64:mt * 256 + (h + 1) * 64]),
                     reads=[R2B], writes=[XAB])
        S.barrier()
        S.op("pool", lambda e: e.memset(S32, 0.0), writes=[S32B])
        S.op("pool", lambda e: e.memset(Sbf, 0.0), writes=[SBFB])

        blk_rs = {}

        def head1(t):
            ts = slice(t * 128, (t + 1) * 128)
            if t % 4 == 0:
                b0 = t
                blk_rs[t // 4] = rms_stats_blk(lambda k, b0=b0: xT[:, k, b0 * 128:(b0 + 4) * 128], XB[b0:b0 + 4], 512, 1.0 / D, psreg=(R3[:, 0:512], R3B))
            rs, rsb = blk_rs[t // 4]
            tt = t % 4
            for k in range(KC):
                S.op("dve", lambda e, k=k, ts=ts, rs=rs, tt=tt: e.scalar_tensor_tensor(out=hTt[:, k, :], in0=xT[:, k, ts], scalar=col_norm_mix(l, k),
                                                                                   in1=rs[:, tt * 128:(tt + 1) * 128], op0=ALU.mult, op1=ALU.mult),
                     reads=[XB[t], rsb, VC], writes=[HTB])
            for c in range(8):
                c0 = 2304 + c * 128
                for k in range(KC):
                    mm(R3[:, c * 128:(c + 1) * 128], w_in_sb[:, k, c0:c0 + 128], hTt[:, k, :], k == 0, k == KC - 1, [WIN_B[k // 2], HTB], [R3B])

        for t in range(16):
            ts = slice(t * 128, (t + 1) * 128)
            if t == 0:
                head1(0)
            if kind == "hgrn":
                act(tG, R3[:, 0:768], AF.Tanh, [R3B], [TGB], scale=0.5)
            else:
                act(tG, R3[:, 0:768], AF.Silu, [R3B], [TGB])
            S.op("act", lambda e: e.copy(out=qxT, in_=R3[:, 768:1024]), reads=[R3B], writes=[QXB])
            for h in range(4):
                c = h // 2
                for mt in range(2):
                    mm(R3[:, (h * 2 + mt) * 128:(h * 2 + mt + 1) * 128], KTpad[:, h, mt * 128:(mt + 1) * 128], qxT[:, c * 128:(c + 1) * 128],
                       True, True, [XAB, QXB], [R3B])
            for (R, RB_, c0) in ((R0, R0B, 0), (R1, R1B, 768), (R2, R2B, 1536)):
                for (a, n) in ((0, 512), (512, 256)):
                    for k in range(KC):
                        mm(R[:, a:a + n], hTt[:, k, :], w_in_sb[:, k, c0 + a:c0 + a + n], k == 0, k == KC - 1, [WIN_B[k // 2], HTB], [RB_])
            if kind == "hgrn":
                act(tC, R0[:, 0:768], AF.Silu, [R0B], [TCB])
            act(expT, R3[:, 0:1024], AF.Exp, [R3B], [EXB], scale=0.125)
            for c in range(2):
                i = 0
                for h in (2 * c, 2 * c + 1):
                    for mt in range(2):
                        mm(R3[:, c * 128:(c + 1) * 128], Vpad[:, mt, h, :], expT[:, (h * 2 + mt) * 128:(h * 2 + mt + 1) * 128], i == 0, i == 3, [XAB, EXB], [R3B])
                        i += 1
            for c in range(2):
                i = 0
                for h in (2 * c, 2 * c + 1):
                    for mt in range(2):
                        mm(R3[:, 256 + c * 128:256 + (c + 1) * 128], onespad[:, h % 2, :], expT[:, (h * 2 + mt) * 128:(h * 2 + mt + 1) * 128], i == 0, i == 3, [OPB, EXB], [R3B])
                        i += 1
            cat, catb = catTs[t % 2], CATBs[t % 2]
            prev = t - 1 if t > 0 else None

            def xa_fin(cat=cat, catb=catb):
                act(rsx, R3[:, 256:512], AF.Ln, [R3B], [RSXB])
                act(rsx, rsx, AF.Exp, [RSXB], [RSXB], scale=-1.0)
                S.op("dve", lambda e, cat=cat: e.tensor_tensor(out=cat[:, 6:8, :].rearrange("p c t -> p (c t)"), in0=R3[:, 0:256],
                                                               in1=rsx, op=ALU.mult), reads=[R3B, RSXB], writes=[catb])
            nxt = (lambda t=t: head1(t + 1)) if t < 15 else (lambda: None)
            if kind == "ret":
                ret_tile(l, j, t, nxt, cat, catb, prev, xa_fin)
            elif kind == "hgrn":
                hgrn_tile(l, j, t, nxt, cat, catb, prev, xa_fin)
            else:
                xa_fin()
                S.op("dve", lambda e, cat=cat: e.memset(cat[:, 0:6, :], 0.0), writes=[catb])
                nxt()
                w_out_mm(t, R0, R0B, range(KC))
                w_out_res(t, R0, R0B)
        if kind in ("ret", "hgrn"):
            w_out_mm(15, R0, R0B, range(KC))
            w_out_res(15, R0, R0B)

    def w_out_mm(tp, R, RB_, dcs):
        cat, catb = catTs[tp % 2], CATBs[tp % 2]
        for dc in dcs:
            for k in range(KC):
                mm(R[:, dc * 128:(dc + 1) * 128], w_out_sb[:, k, dc * 128:(dc + 1) * 128], cat[:, k, :], k == 0, k == KC - 1, [WOUT_B, catb], [RB_])

    def w_out_res(tp, R, RB_):
        ts = slice(tp * 128, (tp + 1) * 128)
        S.op("dve", lambda e, ts=ts, R=R: e.tensor_tensor(out=xT[:, :, ts], in0=R.rearrange("p (k c) -> p k c", k=KC), in1=xT[:, :, ts], op=ALU.add),
             reads=[RB_, XB[tp]], writes=[XB[tp]])

    def ret_tile(l, j, t, nxt, cat, catb, prev, xa_fin):
        cosb = KIND[:, t * 64:(t + 1) * 64].unsqueeze(1).to_broadcast([128, 6, 64])
        sinb = KIND[:, 1024 + t * 64:1024 + (t + 1) * 64].unsqueeze(1).to_broadcast([128, 6, 64])
        for (R, RB_, sc, dstT, DSTB) in ((R0, R0B, sqc, Qt, QTB_), (R1, R1B, skc, Kt, KTB_)):
            S.op("dve", lambda e, R=R, sc=sc: e.tensor_tensor(out=hv(tA), in0=hv(R[:, 0:768]), in1=sc.unsqueeze(2).to_broadcast([128, 6, 128]), op=ALU.mult),
                 reads=[RB_, CB], writes=[TAB])
            if R is R0 and prev is not None:
                w_out_mm(prev, R0, R0B, range(KC))
            a4 = tA.rearrange("p (h two d) -> p h two d", h=6, two=2)
            c4 = tC.rearrange("p (h two d) -> p h two d", h=6, two=2)
            e4 = tE.rearrange("p (h two d) -> p h two d", h=6, two=2)
            d4 = dstT.rearrange("p (h two d) -> p h two d", h=6, two=2)
            S.op("dve", lambda e, a4=a4, c4=c4: e.tensor_tensor(out=c4[:, :, 0, :], in0=a4[:, :, 0, :], in1=cosb, op=ALU.mult), reads=[TAB, KINDB], writes=[TCB])
            S.op("dve", lambda e, a4=a4, c4=c4: e.tensor_tensor(out=c4[:, :, 1, :], in0=a4[:, :, 0, :], in1=sinb, op=ALU.mult), reads=[TAB, KINDB], writes=[TCB])
            S.op("dve", lambda e, a4=a4, e4=e4: e.tensor_tensor(out=e4[:, :, 0, :], in0=a4[:, :, 1, :], in1=sinb, op=ALU.mult), reads=[TAB, KINDB], writes=[TEB])
            S.op("dve", lambda e, a4=a4, e4=e4: e.tensor_tensor(out=e4[:, :, 1, :], in0=a4[:, :, 1, :], in1=cosb, op=ALU.mult), reads=[TAB, KINDB], writes=[TEB])
            S.op("dve", lambda e, c4=c4, e4=e4, d4=d4: e.tensor_tensor(out=d4[:, :, 0, :], in0=c4[:, :, 0, :], in1=e4[:, :, 0, :], op=ALU.subtract), reads=[TCB, TEB], writes=[DSTB])
            S.op("dve", lambda e, c4=c4, e4=e4, d4=d4: e.tensor_tensor(out=d4[:, :, 1, :], in0=c4[:, :, 1, :], in1=e4[:, :, 1, :], op=ALU.add), reads=[TCB, TEB], writes=[DSTB])
        S.op("act", lambda e: e.copy(out=Vt, in_=R2[:, 0:768]), reads=[R2B], writes=[VTB])
        xa_fin()
        if prev is not None:
            w_out_res(prev, R0, R0B)
        for h in range(6):
            hs = slice(h * 128, (h + 1) * 128)
            S.op("pe", lambda e, hs=hs: e.transpose(out=R0b[:, hs], in_=Qt[:, hs], identity=identB[:]), reads=[QTB_, CB], writes=[R0B])
            S.op("pe", lambda e, hs=hs: e.transpose(out=R1b[:, hs], in_=Kt[:, hs], identity=identB[:]), reads=[KTB_, CB], writes=[R1B])
        S.op("act", lambda e: e.copy(out=QTf, in_=R0b[:, 0:768]), reads=[R0B], writes=[QTFB])
        S.op("dve", lambda e: e.tensor_copy(out=KTf, in_=R1b[:, 0:768]), reads=[R1B], writes=[KTFB])
        for h in range(6):
            hs = slice(h * 128, (h + 1) * 128)
            mm(R2[:, hs], KTf[:, hs], QTf[:, hs], True, True, [KTFB, QTFB], [R2B])
        for h in range(6):
            hs = slice(h * 128, (h + 1) * 128)
            ginv = float(np.exp(-128.0 * GAMMA_LOG[h]))
            S.op("dve", lambda e, hs=hs, ginv=ginv: e.scalar_tensor_tensor(out=Pm[:, hs], in0=R2[:, hs], scalar=ginv, in1=CAUc, op0=ALU.mult, op1=ALU.mult),
                 reads=[R2B, CB], writes=[PMB])
        for h in range(6):
            hs = slice(h * 128, (h + 1) * 128)
            mm(R0[:, hs], Vt[:, hs], Pm[:, hs], True, True, [VTB, PMB], [R0B])
        for h in range(6):
            hs = slice(h * 128, (h + 1) * 128)
            mm(R1[:, hs], Sbf[:, hs], QTf[:, hs], True, True, [SBFB, QTFB], [R1B])
        for h in range(6):
            hs = slice(h * 128, (h + 1) * 128)
            mm(R3[:, hs], Kt[:, hs], Vt[:, hs], True, True, [KTB_, VTB], [R3B])
        for h in range(6):
            hs = slice(h * 128, (h + 1) * 128)
            ch = float(np.exp(128.0 * GAMMA_LOG[h]))
            S.op("dve", lambda e, hs=hs, ch=ch: e.scalar_tensor_tensor(out=S32[:, hs], in0=S32[:, hs], scalar=ch, in1=R3[:, hs], op0=ALU.mult, op1=ALU.add),
                 reads=[S32B, R3B], writes=[S32B])
        S.op("act", lambda e: e.copy(out=Sbf, in_=S32), reads=[S32B], writes=[SBFB])
        S.op("act", lambda e: e.copy(out=tO, in_=R0[:, 0:768]), reads=[R0B], writes=[TOB])
        S.op("dve", lambda e: e.tensor_tensor(out=tO, in0=R1[:, 0:768], in1=tO, op=ALU.add), reads=[R1B, TOB], writes=[TOB])
        act(sqo, tO, AF.Square, [TOB], [SQOB])
        for h in range(6):
            hs = slice(h * 128, (h + 1) * 128)
            mm(R2[:, hs], onesB[:], sqo[:, hs], True, True, [SQOB, CB], [R2B])
        act(tR, R2[:, 0:768], AF.Ln, [R2B], [TRB], scale=1.0 / 128, bias=EPS)
        act(tR, tR, AF.Exp, [TRB], [TRB], scale=-0.5)
        nxt()
        for h in range(6):
            hs = slice(h * 128, (h + 1) * 128)
            S.op("dve", lambda e, hs=hs, h=h: e.scalar_tensor_tensor(out=tC[:, hs], in0=tO[:, hs], scalar=col_rtn(j, h), in1=tR[:, hs], op0=ALU.mult, op1=ALU.mult),
                 reads=[TOB, TRB, VC], writes=[TCB])
        S.op("dve", lambda e, cat=cat: e.tensor_tensor(out=cat[:, 0:6, :], in0=hv(tC), in1=hv(tG), op=ALU.mult), reads=[TCB, TGB], writes=[catb])

    def hgrn_tile(l, j, t, nxt, cat, catb, prev, xa_fin):
        lb_bc = KIND[:, 0:768]
        nom_bc = KIND[:, 768:1536]
        act(tA, R1[:, 0:768], AF.Exp, [R1B], [TAB], scale=-1.0)
        S.op("dve", lambda e: e.scalar_tensor_tensor(out=tB, in0=tA, scalar=E30, in1=lb_bc, op0=ALU.min, op1=ALU.mult), reads=[TAB, KINDB], writes=[TBB])
        act(tB, tB, AF.Ln, [TBB], [TBB], bias=1.0)
        act(tA, tA, AF.Ln, [TAB], [TAB], bias=1.0)
        S.op("dve", lambda e: e.tensor_tensor(out=tB, in0=tB, in1=tA, op=ALU.subtract), reads=[TBB, TAB], writes=[TBB])
        act(tA, tA, AF.Exp, [TAB], [TAB], scale=-1.0)
        S.op("dve", lambda e: e.scalar_tensor_tensor(out=tA, in0=tA, scalar=1.0, in1=nom_bc, op0=ALU.subtract, op1=ALU.mult), reads=[TAB, KINDB], writes=[TAB])
        S.op("act", lambda e: e.copy(out=Vt, in_=R2[:, 0:768]), reads=[R2B], writes=[VTB])
        xa_fin()
        for (R, RB_, M) in ((R0, R0B, M1c), (R1, R1B, M2c), (R2, R2B, M4c)):
            for (a, n) in ((0, 512), (512, 256)):
                mm(R[:, a:a + n], M, tB[:, a:a + n], True, True, [CB, TBB], [RB_])
        act(tE, R0[:, 0:768], AF.Exp, [R0B], [TEB])
        S.op("dve", lambda e: e.tensor_tensor(out=Qt, in0=tC, in1=tE, op=ALU.mult), reads=[TCB, TEB], writes=[QTB_])
        act(tB, R0[:, 0:768], AF.Exp, [R0B], [TBB], scale=-1.0)
        S.op("dve", lambda e: e.tensor_tensor(out=Kt, in0=tA, in1=tB, op=ALU.mult), reads=[TAB, TBB], writes=[KTB_])
        act(tE, R1[:, 0:768], AF.Exp, [R1B], [TEB])
        S.op("dve", lambda e: e.tensor_tensor(out=Qot, in0=tC, in1=tE, op=ALU.mult), reads=[TCB, TEB], writes=[QOTB_])
        for h in range(6):
            hs = slice(h * 128, (h + 1) * 128)
            S.op("pe", lambda e, hs=hs: e.transpose(out=R0[:, hs], in_=tE[:, hs], identity=identF), reads=[TEB, CB], writes=[R0B])
        S.op("dve", lambda e: e.tensor_copy(out=dec.rearrange("p (h n) -> p h n", h=6),
                                            in_=R0[:, 0:768].rearrange("p (h n c) -> p h n c", h=6, n=4)[:, :, :, 31]), reads=[R0B], writes=[DECB])
        act(tB, R2[:, 0:768], AF.Exp, [R2B], [TBB])
        S.op("dve", lambda e: e.tensor_tensor(out=Kst, in0=tA, in1=tB, op=ALU.mult), reads=[TAB, TBB], writes=[KSTB])
        for h in range(6):
            hs = slice(h * 128, (h + 1) * 128)
            S.op("pe", lambda e, hs=hs: e.transpose(out=R1b[:, hs], in_=Qt[:, hs], identity=identB[:]), reads=[QTB_, CB], writes=[R1B])
            S.op("pe", lambda e, hs=hs: e.transpose(out=R2b[:, hs], in_=Kt[:, hs], identity=identB[:]), reads=[KTB_, CB], writes=[R2B])
            S.op("pe", lambda e, hs=hs: e.transpose(out=R3b[:, hs], in_=Qot[:, hs], identity=identB[:]), reads=[QOTB_, CB], writes=[R3B])
        S.op("act", lambda e: e.copy(out=QTf, in_=R1b[:, 0:768]), reads=[R1B], writes=[QTFB])
        S.op("dve", lambda e: e.tensor_copy(out=KTf, in_=R2b[:, 0:768]), reads=[R2B], writes=[KTFB])
        S.op("act", lambda e: e.copy(out=QoTf, in_=R3b[:, 0:768]), reads=[R3B], writes=[QOTFB])
        for h in range(6):
            hs = slice(h * 128, (h + 1) * 128)
            mm(R0[:, hs], KTf[:, hs], QTf[:, hs], True, True, [KTFB, QTFB], [R0B])
        S.op("dve", lambda e: e.tensor_tensor(out=hv(Pm), in0=hv(R0[:, 0:768]), in1=BDc.unsqueeze(1).to_broadcast([128, 6, 128]), op=ALU.mult),
             reads=[R0B, CB], writes=[PMB])
        for h in range(6):
            hs = slice(h * 128, (h + 1) * 128)
            mm(R1[:, hs], Vt[:, hs], Pm[:, hs], True, True, [VTB, PMB], [R1B])
        for n in range(4):
            ns = slice(32 * n, 32 * n + 32)
            for h in range(6):
                hs = slice(h * 128, (h + 1) * 128)
                cs = slice(h * 128 + 32 * n, h * 128 + 32 * n + 32)
                mm(R2[:, cs], Sbf[:, hs], QoTf[:, cs], True, True, [SBFB, QOTFB], [R2B])
            for h in range(6):
                hs = slice(h * 128, (h + 1) * 128)
                mm(R3[:, hs], Kst[ns, hs], Vt[ns, hs], True, True, [KSTB, VTB], [R3B], tile_position=(32 * n, 0))
            if prev is not None:
                w_out_mm(prev, R0, R0B, (2 * n, 2 * n + 1))
            for h in range(6):
                hs = slice(h * 128, (h + 1) * 128)
                S.op("dve", lambda e, hs=hs, h=h, n=n: e.scalar_tensor_tensor(out=S32[:, hs], in0=S32[:, hs], scalar=dec[:, h * 4 + n:h * 4 + n + 1], in1=R3[:, hs],
                                                                           op0=ALU.mult, op1=ALU.add), reads=[S32B, R3B, DECB], writes=[S32B])
            S.op("act", lambda e: e.copy(out=Sbf, in_=S32), reads=[S32B], writes=[SBFB])
        if prev is not None:
            w_out_res(prev, R0, R0B)
        S.op("act", lambda e: e.copy(out=tO, in_=R1[:, 0:768]), reads=[R1B], writes=[TOB])
        S.op("dve", lambda e: e.tensor_tensor(out=tO, in0=R2[:, 0:768], in1=tO, op=ALU.add), reads=[R2B, TOB], writes=[TOB])
        act(sqo, tO, AF.Square, [TOB], [SQOB])
        for h in range(6):
            hs = slice(h * 128, (h + 1) * 128)
            mm(R0[:, 0:128], onesB[:], sqo[:, hs], h == 0, h == 5, [SQOB, CB], [R0B])
        act(tR[:, 0:128], R0[:, 0:128], AF.Ln, [R0B], [TRB], scale=4.0 / MIXW, bias=4.0 * EPS)
        act(tR[:, 0:128], tR[:, 0:128], AF.Exp, [TRB], [TRB], scale=-0.5)
        nxt()
        for h in range(6):
            hs = slice(h * 128, (h + 1) * 128)
            S.op("dve", lambda e, hs=hs, h=h: e.scalar_tensor_tensor(out=tC[:, hs], in0=tO[:, hs], scalar=col_hgn(j, h), in1=tR[:, 0:128], op0=ALU.mult, op1=ALU.mult),
                 reads=[TOB, TRB, VC], writes=[TCB])
        S.op("dve", lambda e, cat=cat: e.scalar_tensor_tensor(out=cat[:, 0:6, :], in0=hv(tG), scalar=1.0, in1=hv(tC),
                                                              op0=ALU.add, op1=ALU.mult), reads=[TGB, TCB], writes=[catb])

    if do_mix and n_layers > 0:
        issue_mixer_weights(0)
    load_transposed(x_d, 16, xT, XB)

    for l in range(n_layers):
        kind = kinds[l] if kinds else ("hgrn" if l % 2 == 0 else "ret")
        j = l // 2
        if do_mix:
            S.barrier()
            mixer_layer(l, kind, j)
        if do_ffn:
            S.barrier()
            for b in range(4):
                rs, rsb = rms_stats_blk(lambda k, b=b: xT[:, k, b * 512:(b + 1) * 512], XB[4 * b:4 * b + 4], 512, 1.0 / D)
                for k in range(KC):
                    S.op("dve", lambda e, b=b, k=k, l=l, rs=rs: e.scalar_tensor_tensor(
                        out=hT[:, k, b * 512:(b + 1) * 512], in0=xT[:, k, b * 512:(b + 1) * 512], scalar=col_norm_ffn(l, k),
                        in1=rs, op0=ALU.mult, op1=ALU.mult), reads=XB[4 * b:4 * b + 4] + [rsb, VC], writes=[HB[b]])
            for (f0, nf) in GROUPS:
                S.dma("pool", lambda e, f0=f0, nf=nf, l=l: e.dma_start(
                    out=wo_sb[:, 0:nf, :], in_=w_fo_d[l, f0 * 128:(f0 + nf) * 128, :].rearrange("(f p) n -> p f n", p=128)), writes=[WO_B])
                for fi in range(nf):
                    f = f0 + fi
                    wi, wib = wi_rot.next()
                    S.dma("pool", lambda e, wi=wi, f=f, l=l: e.dma_start(
                        out=wi[:, 0], in_=w_fi_d[l, :, f * 128:(f + 1) * 128].rearrange("(k p) n -> p k n", p=128)), writes=[wib[0]])
                    S.dma("pool", lambda e, wi=wi, f=f, l=l: e.dma_start(
                        out=wi[:, 1], in_=w_fi_d[l, :, DFF + f * 128:DFF + (f + 1) * 128].rearrange("(k p) n -> p k n", p=128)), writes=[wib[1]])
                    for b in range(4):
                        pg, pgb = ps_all.next()
                        pu, pub = ps_all.next()
                        for k in range(KC):
                            S.op("pe", lambda e, pg=pg, wi=wi, k=k, b=b: e.matmul(pg, lhsT=wi[:, 0, k, :], rhs=hT[:, k, b * 512:(b + 1) * 512],
                                                                             start=(k == 0), stop=(k == KC - 1)), reads=[wib[0], HB[b]], writes=[pgb])
                        for k in range(KC):
                            S.op("pe", lambda e, pu=pu, wi=wi, k=k, b=b: e.matmul(pu, lhsT=wi[:, 1, k, :], rhs=hT[:, k, b * 512:(b + 1) * 512],
                                                                             start=(k == 0), stop=(k == KC - 1)), reads=[wib[1], HB[b]], writes=[pub])
                        sl, slb = silu_rot.next()
                        S.op("act", lambda e, sl=sl, pg=pg: e.activation(out=sl, in_=pg, func=AF.Silu), reads=[pgb], writes=[slb])
                        S.op("dve", lambda e, sl=sl, pu=pu, fi=fi, b=b: e.tensor_tensor(out=aT[:, fi, b * 512:(b + 1) * 512], in0=pu, in1=sl, op=ALU.mult),
                             reads=[pub, slb], writes=[AB[fi][b]])
                if (f0, nf) == GROUPS[-1] and do_mix and l + 1 < n_layers:
                    for i in range(2):
                        S.dma("pool", lambda e, i=i, l=l: e.dma_start(out=w_in_sb[:, 2 * i:2 * i + 2, :],
                                                                in_=w_in_d[l + 1, 256 * i:256 * (i + 1), :].rearrange("(k p) n -> p k n", p=128)),
                              writes=[WIN_B[i]] + HB)
                        prefetched.add((l + 1, i))
                for b in range(4):
                    for dc in range(KC):
                        py, pyb = ps_all.next()
                        for fi in range(nf):
                            S.op("pe", lambda e, py=py, fi=fi, dc=dc, b=b, nf=nf: e.matmul(py, lhsT=wo_sb[:, fi, dc * 128:(dc + 1) * 128],
                                                                                      rhs=aT[:, fi, b * 512:(b + 1) * 512], start=(fi == 0), stop=(fi == nf - 1)),
                                 reads=[WO_B, AB[fi][b]], writes=[pyb])
                        S.op("dve", lambda e, py=py, dc=dc, b=b: e.tensor_tensor(out=xT[:, dc, b * 512:(b + 1) * 512], in0=py,
                                                                                in1=xT[:, dc, b * 512:(b + 1) * 512], op=ALU.add),
                             reads=[pyb] + XB[4 * b:4 * b + 4], writes=XB[4 * b:4 * b + 4])

    S.barrier()
    for b in range(4):
        rs, rsb = rms_stats_blk(lambda k, b=b: xT[:, k, b * 512:(b + 1) * 512], XB[4 * b:4 * b + 4], 512, 1.0 / D)
        for tt in range(4):
            t = 4 * b + tt
            yT, ytb = yT_rot.next()
            for k in range(KC):
                S.op("dve", lambda e, yT=yT, k=k, t=t, tt=tt, rs=rs: e.scalar_tensor_tensor(
                    out=yT[:, k, :], in0=xT[:, k, t * 128:(t + 1) * 128], scalar=col_norm_fin(k),
                    in1=rs[:, tt * 128:(tt + 1) * 128], op0=ALU.mult, op1=ALU.mult), reads=[XB[t], rsb, VC], writes=[ytb])
            stg, stgb = stage_rot.next()
            for half in range(2):
                ps, pb = ps_all.next()
                for jj in range(4):
                    k = half * 4 + jj
                    S.op("pe", lambda e, ps=ps, yT=yT, jj=jj, k=k: e.transpose(out=ps[:, jj * 128:(jj + 1) * 128], in_=yT[:, k, :], identity=identF),
                         reads=[ytb, CB], writes=[pb])
                if half == 0:
                    S.op("act", lambda e, ps=ps, stg=stg, half=half: e.copy(out=stg[:, half * 512:(half + 1) * 512], in_=ps), reads=[pb], writes=[stgb])
                else:
                    S.op("dve", lambda e, ps=ps, stg=stg, half=half: e.tensor_copy(out=stg[:, half * 512:(half + 1) * 512], in_=ps), reads=[pb], writes=[stgb])
            outs.append(S.dma("sp", lambda e, stg=stg, t=t: e.dma_start(out=out_d[t * 128:(t + 1) * 128, :], in_=stg), reads=[stgb], semkey="out"))

    S.run_block(final_waits=outs)
    S.close()
    es.close()
    return nc


def _consts():
    c = np.zeros((128, 1024), np.float64)
    c[:, 0:128] = np.eye(128)
    s = np.arange(128)[:, None]
    t = np.arange(128)[None, :]
    same = (s // 32) == (t // 32)
    cs, ct = s % 32, t % 32
    c[:, 128:256] = same * ((cs <= ct).astype(np.float64) - (cs <= 15).astype(np.float64))
    c[:, 256:384] = same * (cs <= ct)
    c[:, 384:512] = same * (cs > ct)
    c[:, 512:640] = same * (s <= t)
    c[:, 640:768] = (s <= t)
    p = np.arange(128)
    for h in range(6):
        lg = GAMMA_LOG[h]
        c[:, 768 + h] = np.exp(lg * (p + 1.0))
        c[:, 774 + h] = np.exp(lg * (127.0 - p)) * (128.0 ** -0.5)
    inv = (np.float32(10000.0) ** (-np.linspace(0.0, 1.0, 64, dtype=np.float32))).astype(np.float32)
    c[:, 832:896] = inv[None, :]
    return c.astype(np.float32)


_NC_CACHE = {}


def make_in_maps(inputs, n_cores=8):
    x = np.asarray(inputs["x"], np.float32)
    mem = np.asarray(inputs["mem"], np.float32)
    pos = np.asarray(inputs["positions"], np.int32)
    f = lambda k: np.ascontiguousarray(np.asarray(inputs[k], np.float32))
    shared = {k: f(k) for k in ("norm_mix", "w_in", "w_out", "norm_mem", "w_mem_kv", "hgrn_lb_logits", "hgrn_out_norm",
                                "norm_ffn", "w_ffn_in", "w_ffn_out")}
    shared["ret_out_norm"] = np.ascontiguousarray(np.asarray(inputs["ret_out_norm"], np.float32).reshape(2, MIXW))
    shared["norm_final"] = np.ascontiguousarray(np.asarray(inputs["norm_final"], np.float32).reshape(1, D))
    shared["consts"] = _consts()
    maps = []
    for c in range(n_cores):
        m = dict(shared)
        m["x"] = np.ascontiguousarray(x[c])
        m["mem"] = np.ascontiguousarray(mem[c])
        m["positions"] = np.ascontiguousarray(pos[c].reshape(16, 128))
        maps.append(m)
    return maps


def kernel(**inputs):
    if "full" not in _NC_CACHE:
        _NC_CACHE["full"] = build()
    nc = _NC_CACHE["full"]
    maps = make_in_maps(inputs, 8)
    res = run_bass_kernel_spmd(nc, maps, core_ids=list(range(8)))
    return np.stack([np.asarray(r["out"], np.float32) for r in res.results], axis=0)
```

```python
import numpy as np
from contextlib import ExitStack
import concourse.bass as bass
import concourse.mybir as mybir
from concourse.bass_utils import run_bass_kernel_spmd

F32 = mybir.dt.float32
BF16 = mybir.dt.bfloat16
I32 = mybir.dt.int32
AF = mybir.ActivationFunctionType
ALU = mybir.AluOpType

D = 1024
T = 2048
NL = 4
KC = 8
MIXW = 768
INW = 3328
DFF = 2816
FC = 22
NMEM = 256
EPS = 1e-6


class Buf:
    __slots__ = ("name", "w", "r", "kids")

    def __init__(self, name="", kids=()):
        self.name = name
        self.w = None
        self.r = []
        self.kids = tuple(kids)


def _expand(bufs):
    out = []
    for b in bufs:
        out.append(b)
        out.extend(b.kids)
    return out


class Op:
    __slots__ = ("eng", "fn", "deps", "sig", "signal", "dma", "idx")


class Sched:
    ENGS = ("pe", "act", "dve", "pool", "sp")

    def __init__(self, nc):
        self.nc = nc
        self.ops = {e: [] for e in self.ENGS}
        self.n = 0
        self.phase = 0
        self.dma_sems = {}
        self.eng_sems = {}
        self._sem_ctx = []
        self.pending_dma = []

    def _new_sem(self, name):
        cm = self.nc.semaphore(name)
        s = cm.__enter__()
        self._sem_ctx.append(cm)
        return s

    def close(self):
        for cm in reversed(self._sem_ctx):
            cm.__exit__(None, None, None)

    def _deps(self, op, reads, writes):
        reads = _expand(reads)
        writes = _expand(writes)
        deps = []
        for b in reads:
            if b.w is not None:
                deps.append(b.w)
        for b in writes:
            if b.w is not None:
                deps.append(b.w)
            deps.extend(b.r)
        for b in reads:
            b.r.append(op)
        for b in writes:
            b.w = op
            b.r = []
        out = []
        seen = set()
        for d in deps:
            if d is op or id(d) in seen:
                continue
            seen.add(id(d))
            if d.eng == "pe" and op.eng == "pe" and not d.dma and not op.dma:
                continue
            out.append(d)
        return out

    def op(self, eng, fn, reads=(), writes=(), extra=()):
        o = Op()
        o.eng = eng
        o.fn = fn
        o.dma = False
        o.sig = False
        o.idx = self.n
        self.n += 1
        o.deps = self._deps(o, reads, writes) + list(extra)
        for d in o.deps:
            d.sig = True
        o.signal = (eng, self.phase)
        self.ops[eng].append(o)
        return o

    def dma(self, queue, fn, reads=(), writes=(), semkey=None):
        o = Op()
        o.eng = queue
        o.fn = fn
        o.dma = True
        o.sig = True
        o.idx = self.n
        self.n += 1
        o.deps = self._deps(o, reads, writes)
        for d in o.deps:
            d.sig = True
        if semkey is None:
            semkey = ("dma", id(writes[0]))
        if semkey not in self.dma_sems:
            self.dma_sems[semkey] = [self._new_sem("d%d" % len(self.dma_sems)), 0]
        ent = self.dma_sems[semkey]
        ent[1] += 16
        o.signal = (ent[0], ent[1])
        self.ops[queue].append(o)
        self.pending_dma.append(o)
        return o

    def barrier(self):
        lasts = []
        for e in self.ENGS:
            for o in reversed(self.ops[e]):
                if not o.dma:
                    lasts.append(o)
                    break
        lasts += self.pending_dma
        self.pending_dma = []
        for e in self.ENGS:
            self.op(e, lambda eng: eng.nop(), extra=[d for d in lasts])
        self.phase += 1

    def finalize(self):
        counters = {}
        for e in self.ENGS:
            for o in self.ops[e]:
                if o.dma:
                    continue
                if o.sig:
                    key = o.signal
                    if key not in self.eng_sems:
                        self.eng_sems[key] = self._new_sem("e_%s_%d" % key)
                    counters[key] = counters.get(key, 0) + 1
                    o.signal = (self.eng_sems[key], counters[key])
                else:
                    o.signal = None

    def emit(self, ename, eng):
        waited = {}
        for o in self.ops[ename]:
            best = {}
            for d in o.deps:
                sem, val = d.signal
                k = id(sem)
                if waited.get(k, 0) < val:
                    waited[k] = val
                    best[k] = (sem, val)
            ws = list(best.values())
            for sem, val in ws[1:]:
                eng.wait_ge(sem, val)
            ins = o.fn(eng)
            if ws:
                ins._wait_ge(ws[0][0], ws[0][1])
            if o.dma:
                ins.then_inc(o.signal[0], 16)
            elif o.sig:
                ins.then_inc(o.signal[0], 1)

    def run_block(self, final_waits=()):
        self.finalize()
        nc = self.nc
        sch = self
        with nc.Block() as block:
            @block.tensor
            def _(e):
                sch.emit("pe", e)

            @block.scalar
            def _(e):
                sch.emit("act", e)

            @block.vector
            def _(e):
                sch.emit("dve", e)

            @block.gpsimd
            def _(e):
                sch.emit("pool", e)

            @block.sync
            def _(e):
                sch.emit("sp", e)
                best = {}
                for o in final_waits:
                    sem, val = o.signal
                    if id(sem) not in best or best[id(sem)][1] < val:
                        best[id(sem)] = (sem, val)
                for sem, val in best.values():
                    e.wait_ge(sem, val)


class Rot:
    def __init__(self, items):
        self.items = items
        self.i = 0

    def next(self):
        it = self.items[self.i % len(self.items)]
        self.i += 1
        return it


GF = 6
GROUPS = [(0, 6), (6, 6), (12, 6), (18, 4)]
GAMMA_LOG = [float(np.log(np.float32(1.0) - np.float32(2.0) ** np.float32(-5.0 - h))) for h in range(6)]
E30 = float(np.exp(30.0))
PI = float(np.pi)


def build(n_layers=NL, do_mix=True, do_ffn=True, kinds=None):
    nc = bass.Bass("TRN2", target_bir_lowering=False)

    def din(name, shape, dt=F32):
        return nc.dram_tensor(name, shape, dt, kind="ExternalInput").ap()

    x_d = din("x", [T, D])
    mem_d = din("mem", [NMEM, D])
    pos_d = din("positions", [16, 128], I32)
    norm_mix_d = din("norm_mix", [NL, D])
    w_in_d = din("w_in", [NL, D, INW])
    w_out_d = din("w_out", [NL, D, D])
    norm_mem_d = din("norm_mem", [NL, D])
    w_kv_d = din("w_mem_kv", [NL, D, 512])
    lb_d = din("hgrn_lb_logits", [2, MIXW])
    hgn_d = din("hgrn_out_norm", [2, MIXW])
    rtn_d = din("ret_out_norm", [2, MIXW])
    norm_ffn_d = din("norm_ffn", [NL, D])
    w_fi_d = din("w_ffn_in", [NL, D, 2 * DFF])
    w_fo_d = din("w_ffn_out", [NL, DFF, D])
    norm_fin_d = din("norm_final", [1, D])
    consts_d = din("consts", [128, 1024])
    out_d = nc.dram_tensor("out", [T, D], F32, kind="ExternalOutput").ap()

    es = ExitStack()

    def sb(name, shape, dt):
        return es.enter_context(nc.sbuf_tensor(name, shape, dt))

    S = Sched(nc)

    xT = sb("xT", [128, KC, T], F32)
    XB = [Buf("x%d" % i) for i in range(16)]
    U1 = sb("U1", [128, 34816], BF16)
    U2W = 12320
    U2 = sb("U2", [128, U2W], F32)
    U2b = U2[:].bitcast(BF16)
    cst = sb("cst", [128, 1024], F32)
    identF = cst[:, 0:128]
    M1c, M2c, M4c, BDc, CAUc = (cst[:, 128 * i:128 * (i + 1)] for i in range(1, 6))
    sqc = cst[:, 768:774]
    skc = cst[:, 774:780]
    invf = cst[:, 832:896]
    identB = sb("identB", [128, 128], BF16)
    onesB = sb("onesB", [128, 128], BF16)
    vcol = sb("vcol", [128, 128], F32)
    KIND = sb("KIND", [128, 2048], F32)
    KINDB = Buf("KIND")
    xatt = sb("xatt", [128, 2304], BF16)
    KTpad = xatt[:, 0:1024].rearrange("p (h m) -> p h m", h=4)
    Vpad = xatt[:, 1024:2048].rearrange("p (t h c) -> p t h c", t=2, h=4)
    onespad = xatt[:, 2048:2304].rearrange("p (a c) -> p a c", a=2)
    XAB = Buf("xatt")
    OPB = Buf("onespad")
    CB = Buf("consts")
    VC = Buf("vcol")
    sqt = sb("sqt", [128, 2, 512], BF16)
    sq_rot = Rot([(sqt[:, i, :], Buf("sq%d" % i)) for i in range(2)])
    rstd_t = sb("rstd", [128, 2, 512], F32)
    rstd_rot = Rot([(rstd_t[:, i, :], Buf("rstd%d" % i)) for i in range(2)])

    PSA = es.enter_context(nc.psum_tensor("psa", [128, 4096], F32))
    PSAb = PSA[:].bitcast(BF16)
    PB = [Buf("ps%d" % i) for i in range(8)]
    ps_all = Rot([(PSA[:, 512 * i:512 * (i + 1)], PB[i]) for i in range(8)])

    w_in_sb = U1[:, 0:KC * INW].rearrange("p (k n) -> p k n", k=KC)
    w_out_sb = U1[:, KC * INW:KC * INW + KC * D].rearrange("p (k n) -> p k n", k=KC)
    WIN_B = [Buf("win%d" % i) for i in range(4)]
    WOUT_B = Buf("wout")
    hT = U1[:, 0:KC * T].rearrange("p (k t) -> p k t", k=KC)
    HB = [Buf("h%d" % i) for i in range(4)]
    aT = U1[:, KC * T:KC * T + GF * T].rearrange("p (f t) -> p f t", f=GF)
    AB = [[Buf("a%d_%d" % (f, b)) for b in range(4)] for f in range(GF)]
    wo_sb = U2b[:, 0:GF * D].rearrange("p (f n) -> p f n", f=GF)
    WO_B = Buf("wo")
    NWI = 3
    wi_sb = [U2b[:, GF * D + i * 2048:GF * D + (i + 1) * 2048].rearrange("p (g k n) -> p g k n", g=2, k=KC) for i in range(NWI)]
    wi_rot = Rot([(wi_sb[i], (Buf("wig%d" % i), Buf("wiu%d" % i))) for i in range(NWI)])
    st0 = GF * D + NWI * 2048
    silu_rot = Rot([(U2b[:, st0 + i * 512:st0 + (i + 1) * 512], Buf("sl%d" % i)) for i in range(4)])
    stage_rot = Rot([(U2[:, i * 1024:(i + 1) * 1024], Buf("stg%d" % i)) for i in range(2)])
    yT_rot = Rot([(U2[:, 2048 + i * 1024:2048 + (i + 1) * 1024].rearrange("p (k t) -> p k t", k=KC), Buf("yT%d" % i)) for i in range(2)])
    vrows = U2[:, 4096:4224]
    VR = Buf("vrows")

    _o = [0]

    def u2f(n):
        a = U2[:, _o[0]:_o[0] + n]
        _o[0] += n
        return a

    def u2b(n):
        a = U2b[:, 2 * _o[0]:2 * _o[0] + n]
        _o[0] += (n + 1) // 2
        return a
    hTt = u2b(1024).rearrange("p (k t) -> p k t", k=KC); HTB = Buf("hTt")
    catTs = [u2b(1024).rearrange("p (k t) -> p k t", k=KC) for _ in range(2)]
    CATBs = [Buf("catT0"), Buf("catT1")]
    tA = u2f(768); TAB = Buf("tA")
    tB = u2f(768); TBB = Buf("tB")
    tC = u2f(768); TCB = Buf("tC")
    tE = u2f(768); TEB = Buf("tE")
    tO = u2f(768); TOB = Buf("tO")
    tG = u2f(768); TGB = Buf("tG")
    tR = u2f(768); TRB = Buf("tR")
    Qt = u2b(768); QTB_ = Buf("Qt")
    Kt = u2b(768); KTB_ = Buf("Kt")
    Qot = u2b(768); QOTB_ = Buf("Qot")
    Kst = u2b(768); KSTB = Buf("Kst")
    Vt = u2b(768); VTB = Buf("Vt")
    QTf = u2b(768); QTFB = Buf("QTf")
    KTf = u2b(768); KTFB = Buf("KTf")
    QoTf = u2b(768); QOTFB = Buf("QoTf")
    Pm = u2b(768); PMB = Buf("Pm")
    sqo = Pm; SQOB = PMB
    S32H = [Buf("S32h%d" % h) for h in range(6)]
    SBFH = [Buf("Sbfh%d" % h) for h in range(6)]
    S32 = u2f(768); S32B = Buf("S32", kids=S32H)
    Sbf = u2b(768); SBFB = Buf("Sbf", kids=SBFH)
    dec = u2f(24); DECB = Buf("dec")
    qxT = Pm[:, 0:256]; QXB = PMB
    expT = u2b(1024); EXB = Buf("expT")
    rsx = u2f(256); RSXB = Buf("rsx")
    setup_base = _o[0]
    assert _o[0] <= U2W, _o[0]
    memst = [U2[:, 2048 + i * 1024:2048 + (i + 1) * 1024] for i in range(2)]
    MSB = [Buf("memst%d" % i) for i in range(2)]
    memh = U2b[:, 2 * 4096:2 * 4096 + 2048].rearrange("p (t n) -> p t n", t=2)
    MHB = Buf("memh")
    memhT = U2b[:, 2 * 5120:2 * 5120 + 2048].rearrange("p (k m) -> p k m", k=KC)
    MHTB = Buf("memhT")
    wkv = U2b[:, 2 * 6144:2 * 6144 + 4096].rearrange("p (k n) -> p k n", k=KC)
    WKVB = Buf("wkv")
    mscr = U2[:, 8192:8192 + 1024]
    MSCB = Buf("mscr")
    angt = U2[:, 9216:9216 + 1024].rearrange("p (t j) -> p t j", t=16)
    ANGB = Buf("ang")
    posr = U2[:, 10240:10240 + 128]
    posi = U2[:, 10368:10368 + 128].bitcast(I32)
    posf = U2[:, 10496:10496 + 16]
    POSB = Buf("pos")
    assert 10512 <= U2W

    R0 = PSA[:, 0:1024]; R0B = Buf("R0")
    R1 = PSA[:, 1024:2048]; R1B = Buf("R1")
    R2H = [Buf("R2h%d" % h) for h in range(6)]
    R3H = [Buf("R3h%d" % h) for h in range(6)]
    R2 = PSA[:, 2048:3072]; R2B = Buf("R2", kids=R2H)
    R3 = PSA[:, 3072:4096]; R3B = Buf("R3", kids=R3H)
    R0b = PSAb[:, 0:2048]; R1b = PSAb[:, 2048:4096]; R2b = PSAb[:, 4096:6144]; R3b = PSAb[:, 6144:8192]
    ALLPS = PB

    def rbufs(*rb):
        return list(rb)

    outs = []

    S.dma("sp", lambda e: e.dma_start(out=cst[:], in_=consts_d), writes=[CB])
    S.op("dve", lambda e: e.tensor_copy(out=identB[:], in_=identF), reads=[CB], writes=[CB])
    S.op("pool", lambda e: e.memset(onesB[:], 1.0), writes=[CB])
    S.op("pool", lambda e: e.memset(xatt[:], 0.0), writes=[XAB, OPB])
    S.op("pool", lambda e: e.memset(onespad[:, 0, 0:64], 1.0), writes=[OPB])
    S.op("pool", lambda e: e.memset(onespad[:, 1, 64:128], 1.0), writes=[OPB])
    S.op("pool", lambda e: e.memset(vrows, 0.0), writes=[VR])
    for i, (src, r0, n) in enumerate([(norm_mix_d, 0, 32), (norm_ffn_d, 32, 32), (norm_mem_d, 64, 32), (norm_fin_d, 96, 8), (hgn_d, 104, 12), (rtn_d, 116, 12)]):
        S.dma("sp", lambda e, src=src, r0=r0, n=n: e.dma_start(out=vrows[r0:r0 + n, :], in_=src.rearrange("l (k p) -> (l k) p", p=128)),
              writes=[VR], semkey="vr")
    S.op("pe", lambda e: e.transpose(out=PSA[:, 0:128], in_=vrows, identity=identF), reads=[VR, CB], writes=[PB[0]])
    S.op("dve", lambda e: e.tensor_copy(out=vcol[:], in_=PSA[:, 0:128]), reads=[PB[0]], writes=[VC])

    def col_norm_mix(l, k): return vcol[:, l * 8 + k:l * 8 + k + 1]
    def col_norm_ffn(l, k): return vcol[:, 32 + l * 8 + k:32 + l * 8 + k + 1]
    def col_norm_mem(l, k): return vcol[:, 64 + l * 8 + k:64 + l * 8 + k + 1]
    def col_norm_fin(k): return vcol[:, 96 + k:96 + k + 1]
    def col_hgn(j, h): return vcol[:, 104 + j * 6 + h:104 + j * 6 + h + 1]
    def col_rtn(j, h): return vcol[:, 116 + j * 6 + h:116 + j * 6 + h + 1]

    S.barrier()

    def load_transposed(src_d, ntile, dst, dst_bufs):
        for t in range(ntile):
            stg, stgb = stage_rot.next()
            S.dma("sp", lambda e, stg=stg, t=t: e.dma_start(out=stg, in_=src_d[t * 128:(t + 1) * 128, :]), writes=[stgb])
            for half in range(2):
                ps, pb = ps_all.next()
                for j in range(4):
                    k = half * 4 + j
                    S.op("pe", lambda e, ps=ps, stg=stg, j=j, k=k: e.transpose(out=ps[:, j * 128:(j + 1) * 128], in_=stg[:, k * 128:(k + 1) * 128], identity=identF),
                         reads=[stgb, CB], writes=[pb])
                if half == 0:
                    S.op("act", lambda e, ps=ps, half=half, t=t: e.copy(out=dst[:, half * 4:half * 4 + 4, t * 128:(t + 1) * 128],
                                                                       in_=ps.rearrange("p (k c) -> p k c", k=4)), reads=[pb], writes=[dst_bufs[t]])
                else:
                    S.op("dve", lambda e, ps=ps, half=half, t=t: e.tensor_copy(out=dst[:, half * 4:half * 4 + 4, t * 128:(t + 1) * 128],
                                                                              in_=ps.rearrange("p (k c) -> p k c", k=4)), reads=[pb], writes=[dst_bufs[t]])

    def rms_stats_blk(src_fn, src_bufs, width, scale, psreg=None):
        ps, pb = psreg if psreg is not None else ps_all.next()
        for k in range(KC):
            sq, sqb = sq_rot.next()
            S.op("act", lambda e, sq=sq, k=k: e.activation(out=sq[:, 0:width], in_=src_fn(k), func=AF.Square), reads=src_bufs, writes=[sqb])
            S.op("pe", lambda e, ps=ps, sq=sq, k=k: e.matmul(ps[:, 0:width], lhsT=onesB[:], rhs=sq[:, 0:width], start=(k == 0), stop=(k == KC - 1)),
                 reads=[sqb, CB], writes=[pb])
        rs, rsb = rstd_rot.next()
        S.op("act", lambda e, ps=ps, rs=rs: e.activation(out=rs[:, 0:width], in_=ps[:, 0:width], func=AF.Ln, scale=scale, bias=EPS), reads=[pb], writes=[rsb])
        S.op("act", lambda e, rs=rs: e.activation(out=rs[:, 0:width], in_=rs[:, 0:width], func=AF.Exp, scale=-0.5), reads=[rsb], writes=[rsb])
        return rs, rsb

    def mm(out, lhsT, rhs, start, stop, reads, writes, **kw):
        S.op("pe", lambda e: e.matmul(out, lhsT=lhsT, rhs=rhs, start=start, stop=stop, **kw), reads=reads, writes=writes)

    def act(out, in_, func, reads, writes, **kw):
        S.op("act", lambda e: e.activation(out=out, in_=in_, func=func, **kw), reads=reads, writes=writes)

    def hv(ap):
        return ap.rearrange("p (h c) -> p h c", h=6)

    weights_issued = set()
    prefetched = set()

    def issue_mixer_weights(l):
        if l in weights_issued:
            return
        weights_issued.add(l)
        S.dma("pool", lambda e: e.dma_start(out=wkv, in_=w_kv_d[l].rearrange("(k p) n -> p k n", p=128)), writes=[WKVB])
        for i in range(4):
            if (l, i) in prefetched:
                continue
            S.dma("pool", lambda e, i=i: e.dma_start(out=w_in_sb[:, 2 * i:2 * i + 2, :],
                                                    in_=w_in_d[l, 256 * i:256 * (i + 1), :].rearrange("(k p) n -> p k n", p=128)), writes=[WIN_B[i]])
        S.dma("pool", lambda e: e.dma_start(out=w_out_sb, in_=w_out_d[l].rearrange("(k p) n -> p k n", p=128)), writes=[WOUT_B])

    def mixer_layer(l, kind, j):
        issue_mixer_weights(l)
        if kind == "ret":
            S.dma("sp", lambda e: e.dma_start(out=posi[0:16, :], in_=pos_d), writes=[POSB])
            S.op("dve", lambda e: e.tensor_copy(out=posr[0:16, :], in_=posi[0:16, :]), reads=[POSB], writes=[POSB])
            S.op("pe", lambda e: e.transpose(out=R3[:, 0:16], in_=posr[0:16, :], identity=identF[0:16, 0:16]), reads=[POSB, CB], writes=[R3B])
            S.op("dve", lambda e: e.tensor_copy(out=posf, in_=R3[:, 0:16]), reads=[R3B], writes=[POSB])
            for t in range(16):
                S.op("dve", lambda e, t=t: e.tensor_scalar(out=angt[:, t, :], in0=invf, scalar1=posf[:, t:t + 1], scalar2=None, op0=ALU.mult), reads=[POSB, CB], writes=[ANGB])
            angf = angt.rearrange("p t j -> p (t j)")
            scr2 = U2[:, 1024:2048]
            scr2i = scr2.bitcast(I32)
            for (off, dst0) in ((0.5 * PI, 0), (0.0, 1024)):
                S.op("dve", lambda e, off=off: e.tensor_scalar(out=mscr, in0=angf, scalar1=off, scalar2=None, op0=ALU.add), reads=[ANGB], writes=[MSCB])
                S.op("dve", lambda e: e.tensor_scalar(out=scr2, in0=mscr, scalar1=1.0 / (2 * PI), scalar2=None, op0=ALU.mult), reads=[MSCB], writes=[TAB, TBB])
                S.op("dve", lambda e: e.tensor_copy(out=scr2i, in_=scr2), reads=[TAB, TBB], writes=[TAB, TBB])
                S.op("dve", lambda e: e.tensor_copy(out=scr2, in_=scr2i), reads=[TAB, TBB], writes=[TAB, TBB])
                S.op("dve", lambda e: e.scalar_tensor_tensor(out=mscr, in0=scr2, scalar=-2 * PI, in1=mscr, op0=ALU.mult, op1=ALU.add), reads=[TAB, TBB, MSCB], writes=[MSCB])
                S.op("dve", lambda e: e.tensor_scalar(out=scr2, in0=mscr, scalar1=PI, scalar2=2 * PI, op0=ALU.is_gt, op1=ALU.mult), reads=[MSCB], writes=[TAB, TBB])
                S.op("dve", lambda e: e.tensor_tensor(out=mscr, in0=mscr, in1=scr2, op=ALU.subtract), reads=[TAB, TBB, MSCB], writes=[MSCB])
                S.op("dve", lambda e: e.tensor_scalar(out=mscr, in0=mscr, scalar1=-PI, scalar2=PI, op0=ALU.max, op1=ALU.min), reads=[MSCB], writes=[MSCB])
                act(KIND[:, dst0:dst0 + 1024], mscr, AF.Sin, [MSCB], [KINDB])
        elif kind == "hgrn":
            lb_bc = KIND[:, 0:768]
            nom_bc = KIND[:, 768:1536]
            if j == 0:
                S.op("pool", lambda e: e.memset(lb_bc, 0.0), writes=[KINDB])
                S.op("pool", lambda e: e.memset(nom_bc, -1.0), writes=[KINDB])
            else:
                S.dma("sp", lambda e: e.dma_start(out=mscr[:, 0:768], in_=lb_d[0:1, :].partition_broadcast(128)), writes=[MSCB])
                S.dma("sp", lambda e: e.dma_start(out=angt.rearrange("p t j -> p (t j)")[:, 0:768], in_=lb_d[1:2, :].partition_broadcast(128)), writes=[ANGB])
                S.op("dve", lambda e: e.tensor_tensor(out=mscr[:, 0:768], in0=mscr[:, 0:768], in1=angt.rearrange("p t j -> p (t j)")[:, 0:768], op=ALU.subtract),
                     reads=[MSCB, ANGB], writes=[MSCB])
                act(mscr[:, 0:768], mscr[:, 0:768], AF.Exp, [MSCB], [MSCB])
                S.op("dve", lambda e: e.tensor_scalar(out=mscr[:, 0:768], in0=mscr[:, 0:768], scalar1=1.0, scalar2=None, op0=ALU.add), reads=[MSCB], writes=[MSCB])
                S.op("dve", lambda e: e.reciprocal(out=lb_bc, in_=mscr[:, 0:768]), reads=[MSCB], writes=[KINDB])
                S.op("dve", lambda e: e.tensor_scalar(out=nom_bc, in0=lb_bc, scalar1=-1.0, scalar2=None, op0=ALU.add), reads=[KINDB], writes=[KINDB])
        BIS = 99
        angf_ = angt.rearrange("p t j -> p (t j)")
        if BIS >= 1:
            S.dma("sp", lambda e: e.dma_start(out=angf_[0:1, :], in_=norm_mem_d[l:l + 1, :]), writes=[ANGB])
            for hh in range(2):
                S.op("pe", lambda e, hh=hh: e.matmul(R3[:, hh * 512:(hh + 1) * 512], lhsT=cst[0:1, 640:768], rhs=angf_[0:1, hh * 512:(hh + 1) * 512],
                                                   start=True, stop=True), reads=[ANGB, CB], writes=[R3B])
            S.op("dve", lambda e: e.tensor_copy(out=mscr, in_=R3), reads=[R3B], writes=[MSCB])
        for mt in range(2 if BIS >= 2 else 0):
            S.dma("sp", lambda e, mt=mt: e.dma_start(out=memst[mt], in_=mem_d[mt * 128:(mt + 1) * 128, :]), writes=[MSB[mt]])
            S.op("pool", lambda e, mt=mt: e.memset(posf[:, mt:mt + 1], 0.0), writes=[POSB])
            act(angf_, memst[mt], AF.Square, [MSB[mt], POSB], [ANGB, POSB], accum_out=posf[:, mt:mt + 1])
            act(posf[:, mt:mt + 1], posf[:, mt:mt + 1], AF.Sqrt, [POSB], [POSB], scale=1.0 / D, bias=EPS)
            S.op("dve", lambda e, mt=mt: e.reciprocal(out=posf[:, mt:mt + 1], in_=posf[:, mt:mt + 1]), reads=[POSB], writes=[POSB])
            S.op("dve", lambda e, mt=mt: e.scalar_tensor_tensor(out=memh[:, mt, :], in0=memst[mt], scalar=posf[:, mt:mt + 1], in1=mscr,
                                                               op0=ALU.mult, op1=ALU.mult), reads=[MSB[mt], POSB, MSCB], writes=[MHB])
            if BIS >= 3:
                for k in range(KC):
                    S.op("pe", lambda e, mt=mt, k=k: e.transpose(out=R0b[:, mt * 1024 + k * 128:mt * 1024 + (k + 1) * 128], in_=memh[:, mt, k * 128:(k + 1) * 128],
                                                                identity=identB[:]), reads=[MHB, CB], writes=[R0B])
                S.op("act", lambda e, mt=mt: e.copy(out=memhT[:, :, mt * 128:(mt + 1) * 128], in_=R0b[:, mt * 1024:(mt + 1) * 1024].rearrange("p (k c) -> p k c", k=KC)),
                     reads=[R0B], writes=[MHTB])
        for c in range(2 if BIS >= 4 else 0):
            for k in range(KC):
                mm(R1[:, c * 256:(c + 1) * 256], wkv[:, k, c * 128:(c + 1) * 128], memhT[:, k, :], k == 0, k == KC - 1, [WKVB, MHTB], [R1B])
        if BIS >= 4:
            for h in range(4):
                c, po = h // 2, 64 * (h % 2)
                S.op("act", lambda e, h=h, c=c, po=po: e.copy(out=KTpad[po:po + 64, h, :], in_=R1[po:po + 64, c * 256:(c + 1) * 256]), reads=[R1B], writes=[XAB])
        for mt in range(2 if BIS >= 5 else 0):
            for k in range(KC):
                mm(R2[:, mt * 256:(mt + 1) * 256], memhT[:, k, mt * 128:(mt + 1) * 128], wkv[:, k, 256:512], k == 0, k == KC - 1, [WKVB, MHTB], [R2B])
        for mt in range(2 if BIS >= 5 else 0):
            for h in range(4):
                S.op("dve", lambda e, mt=mt, h=h: e.tensor_copy(out=Vpad[:, mt, h, (h % 2) * 64:(h % 2) * 64 + 64], in_=R2[:, mt * 256 + h * 64:mt * 256 + (h + 1) * 64]),
                     reads=[R2B], writes=[XAB])
        S.barrier()
        S.op("pool", lambda e: e.memset(S32, 0.0), writes=[S32B])
        S.op("pool", lambda e: e.memset(Sbf, 0.0), writes=[SBFB])

        blk_rs = {}

        def head1(t):
            ts = slice(t * 128, (t + 1) * 128)
            if t % 4 == 0:
                b0 = t
                blk_rs[t // 4] = rms_stats_blk(lambda k, b0=b0: xT[:, k, b0 * 128:(b0 + 4) * 128], XB[b0:b0 + 4], 512, 1.0 / D, psreg=(R3[:, 0:512], R3B))
            rs, rsb = blk_rs[t // 4]
            tt = t % 4
            for k in range(KC):
                S.op("dve", lambda e, k=k, ts=ts, rs=rs, tt=tt: e.scalar_tensor_tensor(out=hTt[:, k, :], in0=xT[:, k, ts], scalar=col_norm_mix(l, k),
                                                                                   in1=rs[:, tt * 128:(tt + 1) * 128], op0=ALU.mult, op1=ALU.mult),
                     reads=[XB[t], rsb, VC], writes=[HTB])
            for c in range(8):
                c0 = 2304 + c * 128
                for k in range(KC):
                    mm(R3[:, c * 128:(c + 1) * 128], w_in_sb[:, k, c0:c0 + 128], hTt[:, k, :], k == 0, k == KC - 1, [WIN_B[k // 2], HTB], [R3B])

        for t in range(16):
            ts = slice(t * 128, (t + 1) * 128)
            if t == 0:
                head1(0)
            if kind == "hgrn":
                act(tG, R3[:, 0:768], AF.Tanh, [R3B], [TGB], scale=0.5)
            else:
                act(tG, R3[:, 0:768], AF.Silu, [R3B], [TGB])
            S.op("act", lambda e: e.copy(out=qxT, in_=R3[:, 768:1024]), reads=[R3B], writes=[QXB])
            for h in range(4):
                c = h // 2
                for mt in range(2):
                    mm(R3[:, (h * 2 + mt) * 128:(h * 2 + mt + 1) * 128], KTpad[:, h, mt * 128:(mt + 1) * 128], qxT[:, c * 128:(c + 1) * 128],
                       True, True, [XAB, QXB], [R3B])
            for (R, RB_, c0) in ((R0, R0B, 0), (R1, R1B, 768), (R2, R2B, 1536)):
                for (a, n) in ((0, 512), (512, 256)):
                    for k in range(KC):
                        mm(R[:, a:a + n], hTt[:, k, :], w_in_sb[:, k, c0 + a:c0 + a + n], k == 0, k == KC - 1, [WIN_B[k // 2], HTB], [RB_])
            if kind == "hgrn":
                act(tC, R0[:, 0:768], AF.Silu, [R0B], [TCB])
            act(expT, R3[:, 0:1024], AF.Exp, [R3B], [EXB], scale=0.125)
            for c in range(2):
                i = 0
                for h in (2 * c, 2 * c + 1):
                    for mt in range(2):
                        mm(R3[:, c * 128:(c + 1) * 128], Vpad[:, mt, h, :], expT[:, (h * 2 + mt) * 128:(h * 2 + mt + 1) * 128], i == 0, i == 3, [XAB, EXB], [R3B])
                        i += 1
            for c in range(2):
                i = 0
                for h in (2 * c, 2 * c + 1):
                    for mt in range(2):
                        mm(R3[:, 256 + c * 128:256 + (c + 1) * 128], onespad[:, h % 2, :], expT[:, (h * 2 + mt) * 128:(h * 2 + mt + 1) * 128], i == 0, i == 3, [OPB, EXB], [R3B])
                        i += 1
            cat, catb = catTs[t % 2], CATBs[t % 2]
            prev = t - 1 if t > 0 else None

            def xa_fin(cat=cat, catb=catb):
                act(rsx, R3[:, 256:512], AF.Ln, [R3B], [RSXB])
                act(rsx, rsx, AF.Exp, [RSXB], [RSXB], scale=-1.0)
                S.op("dve", lambda e, cat=cat: e.tensor_tensor(out=cat[:, 6:8, :].rearrange("p c t -> p (c t)"), in0=R3[:, 0:256],
                                                               in1=rsx, op=ALU.mult), reads=[R3B, RSXB], writes=[catb])
            nxt = (lambda t=t: head1(t + 1)) if t < 15 else (lambda: None)
            if kind == "ret":
                ret_tile(l, j, t, nxt, cat, catb, prev, xa_fin)
            elif kind == "hgrn":
                hgrn_tile(l, j, t, nxt, cat, catb, prev, xa_fin)
            else:
                xa_fin()
                S.op("dve", lambda e, cat=cat: e.memset(cat[:, 0:6, :], 0.0), writes=[catb])
                nxt()
                w_out_mm(t, R0, R0B, range(KC))
                w_out_res(t, R0, R0B)
        if kind in ("ret", "hgrn"):
            w_out_mm(15, R0, R0B, range(KC))
            w_out_res(15, R0, R0B)

    def w_out_mm(tp, R, RB_, dcs):
        cat, catb = catTs[tp % 2], CATBs[tp % 2]
        for dc in dcs:
            for k in range(KC):
                mm(R[:, dc * 128:(dc + 1) * 128], w_out_sb[:, k, dc * 128:(dc + 1) * 128], cat[:, k, :], k == 0, k == KC - 1, [WOUT_B, catb], [RB_])

    def w_out_res(tp, R, RB_):
        ts = slice(tp * 128, (tp + 1) * 128)
        S.op("dve", lambda e, ts=ts, R=R: e.tensor_tensor(out=xT[:, :, ts], in0=R.rearrange("p (k c) -> p k c", k=KC), in1=xT[:, :, ts], op=ALU.add),
             reads=[RB_, XB[tp]], writes=[XB[tp]])

    def ret_tile(l, j, t, nxt, cat, catb, prev, xa_fin):
        cosb = KIND[:, t * 64:(t + 1) * 64].unsqueeze(1).to_broadcast([128, 6, 64])
        sinb = KIND[:, 1024 + t * 64:1024 + (t + 1) * 64].unsqueeze(1).to_broadcast([128, 6, 64])
        for (R, RB_, sc, dstT, DSTB) in ((R0, R0B, sqc, Qt, QTB_), (R1, R1B, skc, Kt, KTB_)):
            S.op("dve", lambda e, R=R, sc=sc: e.tensor_tensor(out=hv(tA), in0=hv(R[:, 0:768]), in1=sc.unsqueeze(2).to_broadcast([128, 6, 128]), op=ALU.mult),
                 reads=[RB_, CB], writes=[TAB])
            if R is R0 and prev is not None:
                w_out_mm(prev, R0, R0B, range(KC))
            a4 = tA.rearrange("p (h two d) -> p h two d", h=6, two=2)
            c4 = tC.rearrange("p (h two d) -> p h two d", h=6, two=2)
            e4 = tE.rearrange("p (h two d) -> p h two d", h=6, two=2)
            d4 = dstT.rearrange("p (h two d) -> p h two d", h=6, two=2)
            S.op("dve", lambda e, a4=a4, c4=c4: e.tensor_tensor(out=c4[:, :, 0, :], in0=a4[:, :, 0, :], in1=cosb, op=ALU.mult), reads=[TAB, KINDB], writes=[TCB])
            S.op("dve", lambda e, a4=a4, c4=c4: e.tensor_tensor(out=c4[:, :, 1, :], in0=a4[:, :, 0, :], in1=sinb, op=ALU.mult), reads=[TAB, KINDB], writes=[TCB])
            S.op("dve", lambda e, a4=a4, e4=e4: e.tensor_tensor(out=e4[:, :, 0, :], in0=a4[:, :, 1, :], in1=sinb, op=ALU.mult), reads=[TAB, KINDB], writes=[TEB])
            S.op("dve", lambda e, a4=a4, e4=e4: e.tensor_tensor(out=e4[:, :, 1, :], in0=a4[:, :, 1, :], in1=cosb, op=ALU.mult), reads=[TAB, KINDB], writes=[TEB])
            S.op("dve", lambda e, c4=c4, e4=e4, d4=d4: e.tensor_tensor(out=d4[:, :, 0, :], in0=c4[:, :, 0, :], in1=e4[:, :, 0, :], op=ALU.subtract), reads=[TCB, TEB], writes=[DSTB])
            S.op("dve", lambda e, c4=c4, e4=e4, d4=d4: e.tensor_tensor(out=d4[:, :, 1, :], in0=c4[:, :, 1, :], in1=e4[:, :, 1, :], op=ALU.add), reads=[TCB, TEB], writes=[DSTB])
        S.op("act", lambda e: e.copy(out=Vt, in_=R2[:, 0:768]), reads=[R2B], writes=[VTB])
        xa_fin()
        if prev is not None:
            w_out_res(prev, R0, R0B)
        for h in range(6):
            hs = slice(h * 128, (h + 1) * 128)
            S.op("pe", lambda e, hs=hs: e.transpose(out=R0b[:, hs], in_=Qt[:, hs], identity=identB[:]), reads=[QTB_, CB], writes=[R0B])
            S.op("pe", lambda e, hs=hs: e.transpose(out=R1b[:, hs], in_=Kt[:, hs], identity=identB[:]), reads=[KTB_, CB], writes=[R1B])
        S.op("act", lambda e: e.copy(out=QTf, in_=R0b[:, 0:768]), reads=[R0B], writes=[QTFB])
        S.op("dve", lambda e: e.tensor_copy(out=KTf, in_=R1b[:, 0:768]), reads=[R1B], writes=[KTFB])
        for h in range(6):
            hs = slice(h * 128, (h + 1) * 128)
            mm(R2[:, hs], KTf[:, hs], QTf[:, hs], True, True, [KTFB, QTFB], [R2B])
        for h in range(6):
            hs = slice(h * 128, (h + 1) * 128)
            ginv = float(np.exp(-128.0 * GAMMA_LOG[h]))
            S.op("dve", lambda e, hs=hs, ginv=ginv: e.scalar_tensor_tensor(out=Pm[:, hs], in0=R2[:, hs], scalar=ginv, in1=CAUc, op0=ALU.mult, op1=ALU.mult),
                 reads=[R2B, CB], writes=[PMB])
        for h in range(6):
            hs = slice(h * 128, (h + 1) * 128)
            mm(R0[:, hs], Vt[:, hs], Pm[:, hs], True, True, [VTB, PMB], [R0B])
        for h in range(6):
            hs = slice(h * 128, (h + 1) * 128)
            mm(R1[:, hs], Sbf[:, hs], QTf[:, hs], True, True, [SBFB, QTFB], [R1B])
        for h in range(6):
            hs = slice(h * 128, (h + 1) * 128)
            mm(R3[:, hs], Kt[:, hs], Vt[:, hs], True, True, [KTB_, VTB], [R3B])
        S.op("act", lambda e: e.copy(out=tO, in_=R0[:, 0:768]), reads=[R0B], writes=[TOB])
        S.op("dve", lambda e: e.tensor_tensor(out=tO, in0=R1[:, 0:768], in1=tO, op=ALU.add), reads=[R1B, TOB], writes=[TOB])
        act(sqo, tO, AF.Square, [TOB], [SQOB])
        for h in range(6):
            hs = slice(h * 128, (h + 1) * 128)
            mm(R2[:, hs], onesB[:], sqo[:, hs], True, True, [SQOB, CB], [R2B])
        for h in range(6):
            hs = slice(h * 128, (h + 1) * 128)
            ch = float(np.exp(128.0 * GAMMA_LOG[h]))
            S.op("dve", lambda e, hs=hs, ch=ch: e.scalar_tensor_tensor(out=S32[:, hs], in0=S32[:, hs], scalar=ch, in1=R3[:, hs], op0=ALU.mult, op1=ALU.add),
                 reads=[S32B, R3B], writes=[S32B])
        act(tR, R2[:, 0:768], AF.Ln, [R2B], [TRB], scale=1.0 / 128, bias=EPS)
        act(tR, tR, AF.Exp, [TRB], [TRB], scale=-0.5)
        S.op("act", lambda e: e.copy(out=Sbf, in_=S32), reads=[S32B], writes=[SBFB])
        boundary = (t % 4 == 3)
        if not boundary:
            nxt()
        for h in range(6):
            hs = slice(h * 128, (h + 1) * 128)
            S.op("dve", lambda e, hs=hs, h=h: e.scalar_tensor_tensor(out=tC[:, hs], in0=tO[:, hs], scalar=col_rtn(j, h), in1=tR[:, hs], op0=ALU.mult, op1=ALU.mult),
                 reads=[TOB, TRB, VC], writes=[TCB])
        S.op("dve", lambda e, cat=cat: e.tensor_tensor(out=cat[:, 0:6, :], in0=hv(tC), in1=hv(tG), op=ALU.mult), reads=[TCB, TGB], writes=[catb])
        if boundary:
            nxt()

    def hgrn_tile(l, j, t, nxt, cat, catb, prev, xa_fin):
        lb_bc = KIND[:, 0:768]
        nom_bc = KIND[:, 768:1536]
        act(tA, R1[:, 0:768], AF.Exp, [R1B], [TAB], scale=-1.0)
        S.op("dve", lambda e: e.scalar_tensor_tensor(out=tB, in0=tA, scalar=E30, in1=lb_bc, op0=ALU.min, op1=ALU.mult), reads=[TAB, KINDB], writes=[TBB])
        act(tB, tB, AF.Ln, [TBB], [TBB], bias=1.0)
        act(tA, tA, AF.Ln, [TAB], [TAB], bias=1.0)
        S.op("dve", lambda e: e.tensor_tensor(out=tB, in0=tB, in1=tA, op=ALU.subtract), reads=[TBB, TAB], writes=[TBB])
        act(tA, tA, AF.Exp, [TAB], [TAB], scale=-1.0)
        S.op("dve", lambda e: e.scalar_tensor_tensor(out=tA, in0=tA, scalar=1.0, in1=nom_bc, op0=ALU.subtract, op1=ALU.mult), reads=[TAB, KINDB], writes=[TAB])
        S.op("act", lambda e: e.copy(out=Vt, in_=R2[:, 0:768]), reads=[R2B], writes=[VTB])
        xa_fin()
        for (R, RB_, M) in ((R0, R0B, M1c), (R1, R1B, M2c), (R2, R2B, M4c)):
            for (a, n) in ((0, 512), (512, 256)):
                mm(R[:, a:a + n], M, tB[:, a:a + n], True, True, [CB, TBB], [RB_])
        act(tE, R0[:, 0:768], AF.Exp, [R0B], [TEB])
        S.op("dve", lambda e: e.tensor_tensor(out=Qt, in0=tC, in1=tE, op=ALU.mult), reads=[TCB, TEB], writes=[QTB_])
        act(tB, R0[:, 0:768], AF.Exp, [R0B], [TBB], scale=-1.0)
        S.op("dve", lambda e: e.tensor_tensor(out=Kt, in0=tA, in1=tB, op=ALU.mult), reads=[TAB, TBB], writes=[KTB_])
        act(tE, R1[:, 0:768], AF.Exp, [R1B], [TEB])
        S.op("dve", lambda e: e.tensor_tensor(out=Qot, in0=tC, in1=tE, op=ALU.mult), reads=[TCB, TEB], writes=[QOTB_])
        for h in range(6):
            hs = slice(h * 128, (h + 1) * 128)
            S.op("pe", lambda e, hs=hs: e.transpose(out=R0[:, hs], in_=tE[:, hs], identity=identF), reads=[TEB, CB], writes=[R0B])
        S.op("dve", lambda e: e.tensor_copy(out=dec.rearrange("p (h n) -> p h n", h=6),
                                            in_=R0[:, 0:768].rearrange("p (h n c) -> p h n c", h=6, n=4)[:, :, :, 31]), reads=[R0B], writes=[DECB])
        act(tB, R2[:, 0:768], AF.Exp, [R2B], [TBB])
        S.op("dve", lambda e: e.tensor_tensor(out=Kst, in0=tA, in1=tB, op=ALU.mult), reads=[TAB, TBB], writes=[KSTB])
        for h in range(6):
            hs = slice(h * 128, (h + 1) * 128)
            S.op("pe", lambda e, hs=hs: e.transpose(out=R1b[:, hs], in_=Qt[:, hs], identity=identB[:]), reads=[QTB_, CB], writes=[R1B])
            S.op("pe", lambda e, hs=hs: e.transpose(out=R2b[:, hs], in_=Kt[:, hs], identity=identB[:]), reads=[KTB_, CB], writes=[R2B])
            S.op("pe", lambda e, hs=hs: e.transpose(out=R3b[:, hs], in_=Qot[:, hs], identity=identB[:]), reads=[QOTB_, CB], writes=[R3B])
        S.op("act", lambda e: e.copy(out=QTf, in_=R1b[:, 0:768]), reads=[R1B], writes=[QTFB])
        S.op("dve", lambda e: e.tensor_copy(out=KTf, in_=R2b[:, 0:768]), reads=[R2B], writes=[KTFB])
        S.op("act", lambda e: e.copy(out=QoTf, in_=R3b[:, 0:768]), reads=[R3B], writes=[QOTFB])
        for h in range(6):
            hs = slice(h * 128, (h + 1) * 128)
            mm(R0[:, hs], KTf[:, hs], QTf[:, hs], True, True, [KTFB, QTFB], [R0B])
        S.op("dve", lambda e: e.tensor_tensor(out=hv(Pm), in0=hv(R0[:, 0:768]), in1=BDc.unsqueeze(1).to_broadcast([128, 6, 128]), op=ALU.mult),
             reads=[R0B, CB], writes=[PMB])
        for h in range(6):
            hs = slice(h * 128, (h + 1) * 128)
            mm(R1[:, hs], Vt[:, hs], Pm[:, hs], True, True, [VTB, PMB], [R1B])
        for n in range(4):
            ns = slice(32 * n, 32 * n + 32)
            for h in range(6):
                hs = slice(h * 128, (h + 1) * 128)
                cs = slice(h * 128 + 32 * n, h * 128 + 32 * n + 32)
                mm(R2[:, cs], Sbf[:, hs], QoTf[:, cs], True, True, [SBFB, QOTFB], [R2B])
            for h in range(6):
                hs = slice(h * 128, (h + 1) * 128)
                mm(R3[:, hs], Kst[ns, hs], Vt[ns, hs], True, True, [KSTB, VTB], [R3B], tile_position=(32 * n, 0))
            if prev is not None:
                w_out_mm(prev, R0, R0B, (2 * n, 2 * n + 1))
            for h in range(6):
                hs = slice(h * 128, (h + 1) * 128)
                S.op("dve", lambda e, hs=hs, h=h, n=n: e.scalar_tensor_tensor(out=S32[:, hs], in0=S32[:, hs], scalar=dec[:, h * 4 + n:h * 4 + n + 1], in1=R3[:, hs],
                                                                           op0=ALU.mult, op1=ALU.add), reads=[S32B, R3B, DECB], writes=[S32B])
            S.op("act", lambda e: e.copy(out=Sbf, in_=S32), reads=[S32B], writes=[SBFB])
        if prev is not None:
            w_out_res(prev, R0, R0B)
        S.op("act", lambda e: e.copy(out=tO, in_=R1[:, 0:768]), reads=[R1B], writes=[TOB])
        S.op("dve", lambda e: e.tensor_tensor(out=tO, in0=R2[:, 0:768], in1=tO, op=ALU.add), reads=[R2B, TOB], writes=[TOB])
        act(sqo, tO, AF.Square, [TOB], [SQOB])
        for h in range(6):
            hs = slice(h * 128, (h + 1) * 128)
            mm(R0[:, 0:128], onesB[:], sqo[:, hs], h == 0, h == 5, [SQOB, CB], [R0B])
        act(tR[:, 0:128], R0[:, 0:128], AF.Ln, [R0B], [TRB], scale=4.0 / MIXW, bias=4.0 * EPS)
        act(tR[:, 0:128], tR[:, 0:128], AF.Exp, [TRB], [TRB], scale=-0.5)
        boundary = (t % 4 == 3)
        if not boundary:
            nxt()
        for h in range(6):
            hs = slice(h * 128, (h + 1) * 128)
            S.op("dve", lambda e, hs=hs, h=h: e.scalar_tensor_tensor(out=tC[:, hs], in0=tO[:, hs], scalar=col_hgn(j, h), in1=tR[:, 0:128], op0=ALU.mult, op1=ALU.mult),
                 reads=[TOB, TRB, VC], writes=[TCB])
        S.op("dve", lambda e, cat=cat: e.scalar_tensor_tensor(out=cat[:, 0:6, :], in0=hv(tG), scalar=1.0, in1=hv(tC),
                                                              op0=ALU.add, op1=ALU.mult), reads=[TGB, TCB], writes=[catb])
        if boundary:
            nxt()

    if do_mix and n_layers > 0:
        issue_mixer_weights(0)
    load_transposed(x_d, 16, xT, XB)

    for l in range(n_layers):
        kind = kinds[l] if kinds else ("hgrn" if l % 2 == 0 else "ret")
        j = l // 2
        if do_mix:
            S.barrier()
            mixer_layer(l, kind, j)
        if do_ffn:
            S.barrier()
            for b in range(4):
                rs, rsb = rms_stats_blk(lambda k, b=b: xT[:, k, b * 512:(b + 1) * 512], XB[4 * b:4 * b + 4], 512, 1.0 / D)
                for k in range(KC):
                    S.op("dve", lambda e, b=b, k=k, l=l, rs=rs: e.scalar_tensor_tensor(
                        out=hT[:, k, b * 512:(b + 1) * 512], in0=xT[:, k, b * 512:(b + 1) * 512], scalar=col_norm_ffn(l, k),
                        in1=rs, op0=ALU.mult, op1=ALU.mult), reads=XB[4 * b:4 * b + 4] + [rsb, VC], writes=[HB[b]])
            for (f0, nf) in GROUPS:
                S.dma("pool", lambda e, f0=f0, nf=nf, l=l: e.dma_start(
                    out=wo_sb[:, 0:nf, :], in_=w_fo_d[l, f0 * 128:(f0 + nf) * 128, :].rearrange("(f p) n -> p f n", p=128)), writes=[WO_B])
                for fi in range(nf):
                    f = f0 + fi
                    wi, wib = wi_rot.next()
                    S.dma("pool", lambda e, wi=wi, f=f, l=l: e.dma_start(
                        out=wi[:, 0], in_=w_fi_d[l, :, f * 128:(f + 1) * 128].rearrange("(k p) n -> p k n", p=128)), writes=[wib[0]])
                    S.dma("pool", lambda e, wi=wi, f=f, l=l: e.dma_start(
                        out=wi[:, 1], in_=w_fi_d[l, :, DFF + f * 128:DFF + (f + 1) * 128].rearrange("(k p) n -> p k n", p=128)), writes=[wib[1]])
                    for b in range(4):
                        pg, pgb = ps_all.next()
                        pu, pub = ps_all.next()
                        for k in range(KC):
                            S.op("pe", lambda e, pg=pg, wi=wi, k=k, b=b: e.matmul(pg, lhsT=wi[:, 0, k, :], rhs=hT[:, k, b * 512:(b + 1) * 512],
                                                                             start=(k == 0), stop=(k == KC - 1)), reads=[wib[0], HB[b]], writes=[pgb])
                        for k in range(KC):
                            S.op("pe", lambda e, pu=pu, wi=wi, k=k, b=b: e.matmul(pu, lhsT=wi[:, 1, k, :], rhs=hT[:, k, b * 512:(b + 1) * 512],
                                                                             start=(k == 0), stop=(k == KC - 1)), reads=[wib[1], HB[b]], writes=[pub])
                        sl, slb = silu_rot.next()
                        S.op("act", lambda e, sl=sl, pg=pg: e.activation(out=sl, in_=pg, func=AF.Silu), reads=[pgb], writes=[slb])
                        S.op("dve", lambda e, sl=sl, pu=pu, fi=fi, b=b: e.tensor_tensor(out=aT[:, fi, b * 512:(b + 1) * 512], in0=pu, in1=sl, op=ALU.mult),
                             reads=[pub, slb], writes=[AB[fi][b]])
                if (f0, nf) == GROUPS[-1] and do_mix and l + 1 < n_layers:
                    for i in range(2):
                        S.dma("pool", lambda e, i=i, l=l: e.dma_start(out=w_in_sb[:, 2 * i:2 * i + 2, :],
                                                                in_=w_in_d[l + 1, 256 * i:256 * (i + 1), :].rearrange("(k p) n -> p k n", p=128)),
                              writes=[WIN_B[i]] + HB)
                        prefetched.add((l + 1, i))
                for b in range(4):
                    for dc in range(KC):
                        py, pyb = ps_all.next()
                        for fi in range(nf):
                            S.op("pe", lambda e, py=py, fi=fi, dc=dc, b=b, nf=nf: e.matmul(py, lhsT=wo_sb[:, fi, dc * 128:(dc + 1) * 128],
                                                                                      rhs=aT[:, fi, b * 512:(b + 1) * 512], start=(fi == 0), stop=(fi == nf - 1)),
                                 reads=[WO_B, AB[fi][b]], writes=[pyb])
                        S.op("dve", lambda e, py=py, dc=dc, b=b: e.tensor_tensor(out=xT[:, dc, b * 512:(b + 1) * 512], in0=py,
                                                                                in1=xT[:, dc, b * 512:(b + 1) * 512], op=ALU.add),
                             reads=[pyb] + XB[4 * b:4 * b + 4], writes=XB[4 * b:4 * b + 4])

    S.barrier()
    for b in range(4):
        rs, rsb = rms_stats_blk(lambda k, b=b: xT[:, k, b * 512:(b + 1) * 512], XB[4 * b:4 * b + 4], 512, 1.0 / D)
        for tt in range(4):
            t = 4 * b + tt
            yT, ytb = yT_rot.next()
            for k in range(KC):
                S.op("dve", lambda e, yT=yT, k=k, t=t, tt=tt, rs=rs: e.scalar_tensor_tensor(
                    out=yT[:, k, :], in0=xT[:, k, t * 128:(t + 1) * 128], scalar=col_norm_fin(k),
                    in1=rs[:, tt * 128:(tt + 1) * 128], op0=ALU.mult, op1=ALU.mult), reads=[XB[t], rsb, VC], writes=[ytb])
            stg, stgb = stage_rot.next()
            for half in range(2):
                ps, pb = ps_all.next()
                for jj in range(4):
                    k = half * 4 + jj
                    S.op("pe", lambda e, ps=ps, yT=yT, jj=jj, k=k: e.transpose(out=ps[:, jj * 128:(jj + 1) * 128], in_=yT[:, k, :], identity=identF),
                         reads=[ytb, CB], writes=[pb])
                if half == 0:
                    S.op("act", lambda e, ps=ps, stg=stg, half=half: e.copy(out=stg[:, half * 512:(half + 1) * 512], in_=ps), reads=[pb], writes=[stgb])
                else:
                    S.op("dve", lambda e, ps=ps, stg=stg, half=half: e.tensor_copy(out=stg[:, half * 512:(half + 1) * 512], in_=ps), reads=[pb], writes=[stgb])
            outs.append(S.dma("sp", lambda e, stg=stg, t=t: e.dma_start(out=out_d[t * 128:(t + 1) * 128, :], in_=stg), reads=[stgb], semkey="out"))

    S.run_block(final_waits=outs)
    S.close()
    es.close()
    return nc


def _consts():
    c = np.zeros((128, 1024), np.float64)
    c[:, 0:128] = np.eye(128)
    s = np.arange(128)[:, None]
    t = np.arange(128)[None, :]
    same = (s // 32) == (t // 32)
    cs, ct = s % 32, t % 32
    c[:, 128:256] = same * ((cs <= ct).astype(np.float64) - (cs <= 15).astype(np.float64))
    c[:, 256:384] = same * (cs <= ct)
    c[:, 384:512] = same * (cs > ct)
    c[:, 512:640] = same * (s <= t)
    c[:, 640:768] = (s <= t)
    p = np.arange(128)
    for h in range(6):
        lg = GAMMA_LOG[h]
        c[:, 768 + h] = np.exp(lg * (p + 1.0))
        c[:, 774 + h] = np.exp(lg * (127.0 - p)) * (128.0 ** -0.5)
    inv = (np.float32(10000.0) ** (-np.linspace(0.0, 1.0, 64, dtype=np.float32))).astype(np.float32)
    c[:, 832:896] = inv[None, :]
    return c.astype(np.float32)


_NC_CACHE = {}


def make_in_maps(inputs, n_cores=8):
    x = np.asarray(inputs["x"], np.float32)
    mem = np.asarray(inputs["mem"], np.float32)
    pos = np.asarray(inputs["positions"], np.int32)
    f = lambda k: np.ascontiguousarray(np.asarray(inputs[k], np.float32))
    shared = {k: f(k) for k in ("norm_mix", "w_in", "w_out", "norm_mem", "w_mem_kv", "hgrn_lb_logits", "hgrn_out_norm",
                                "norm_ffn", "w_ffn_in", "w_ffn_out")}
    shared["ret_out_norm"] = np.ascontiguousarray(np.asarray(inputs["ret_out_norm"], np.float32).reshape(2, MIXW))
    shared["norm_final"] = np.ascontiguousarray(np.asarray(inputs["norm_final"], np.float32).reshape(1, D))
    shared["consts"] = _consts()
    maps = []
    for c in range(n_cores):
        m = dict(shared)
        m["x"] = np.ascontiguousarray(x[c])
        m["mem"] = np.ascontiguousarray(mem[c])
        m["positions"] = np.ascontiguousarray(pos[c].reshape(16, 128))
        maps.append(m)
    return maps


def kernel(**inputs):
    if "full" not in _NC_CACHE:
        _NC_CACHE["full"] = build()
    nc = _NC_CACHE["full"]
    maps = make_in_maps(inputs, 8)
    res = run_bass_kernel_spmd(nc, maps, core_ids=list(range(8)))
    return np.stack([np.asarray(r["out"], np.float32) for r in res.results], axis=0)
```

```python
import numpy as np
from contextlib import ExitStack
import concourse.bass as bass
import concourse.mybir as mybir
from concourse.bass_utils import run_bass_kernel_spmd

F32 = mybir.dt.float32
BF16 = mybir.dt.bfloat16
I32 = mybir.dt.int32
AF = mybir.ActivationFunctionType
ALU = mybir.AluOpType

D = 1024
T = 2048
NL = 4
KC = 8
MIXW = 768
INW = 3328
DFF = 2816
FC = 22
NMEM = 256
EPS = 1e-6


class Buf:
    __slots__ = ("name", "w", "r", "kids")

    def __init__(self, name="", kids=()):
        self.name = name
        self.w = None
        self.r = []
        self.kids = tuple(kids)


def _expand(bufs):
    out = []
    for b in bufs:
        out.append(b)
        out.extend(b.kids)
    return out


class Op:
    __slots__ = ("eng", "fn", "deps", "sig", "signal", "dma", "idx")


class Sched:
    ENGS = ("pe", "act", "dve", "pool", "sp")

    def __init__(self, nc):
        self.nc = nc
        self.ops = {e: [] for e in self.ENGS}
        self.n = 0
        self.phase = 0
        self.dma_sems = {}
        self.eng_sems = {}
        self._sem_ctx = []
        self.pending_dma = []

    def _new_sem(self, name):
        cm = self.nc.semaphore(name)
        s = cm.__enter__()
        self._sem_ctx.append(cm)
        return s

    def close(self):
        for cm in reversed(self._sem_ctx):
            cm.__exit__(None, None, None)

    def _deps(self, op, reads, writes):
        reads = _expand(reads)
        writes = _expand(writes)
        deps = []
        for b in reads:
            if b.w is not None:
                deps.append(b.w)
        for b in writes:
            if b.w is not None:
                deps.append(b.w)
            deps.extend(b.r)
        for b in reads:
            b.r.append(op)
        for b in writes:
            b.w = op
            b.r = []
        out = []
        seen = set()
        for d in deps:
            if d is op or id(d) in seen:
                continue
            seen.add(id(d))
            if d.eng == "pe" and op.eng == "pe" and not d.dma and not op.dma:
                continue
            out.append(d)
        return out

    def op(self, eng, fn, reads=(), writes=(), extra=()):
        o = Op()
        o.eng = eng
        o.fn = fn
        o.dma = False
        o.sig = False
        o.idx = self.n
        self.n += 1
        o.deps = self._deps(o, reads, writes) + list(extra)
        for d in o.deps:
            d.sig = True
        o.signal = (eng, self.phase)
        self.ops[eng].append(o)
        return o

    def dma(self, queue, fn, reads=(), writes=(), semkey=None):
        o = Op()
        o.eng = queue
        o.fn = fn
        o.dma = True
        o.sig = True
        o.idx = self.n
        self.n += 1
        o.deps = self._deps(o, reads, writes)
        for d in o.deps:
            d.sig = True
        if semkey is None:
            semkey = ("dma", id(writes[0]))
        if semkey not in self.dma_sems:
            self.dma_sems[semkey] = [self._new_sem("d%d" % len(self.dma_sems)), 0]
        ent = self.dma_sems[semkey]
        ent[1] += 16
        o.signal = (ent[0], ent[1])
        self.ops[queue].append(o)
        self.pending_dma.append(o)
        return o

    def barrier(self):
        lasts = []
        for e in self.ENGS:
            for o in reversed(self.ops[e]):
                if not o.dma:
                    lasts.append(o)
                    break
        lasts += self.pending_dma
        self.pending_dma = []
        for e in self.ENGS:
            self.op(e, lambda eng: eng.nop(), extra=[d for d in lasts])
        self.phase += 1

    def finalize(self):
        counters = {}
        for e in self.ENGS:
            for o in self.ops[e]:
                if o.dma:
                    continue
                if o.sig:
                    key = o.signal
                    if key not in self.eng_sems:
                        self.eng_sems[key] = self._new_sem("e_%s_%d" % key)
                    counters[key] = counters.get(key, 0) + 1
                    o.signal = (self.eng_sems[key], counters[key])
                else:
                    o.signal = None

    def emit(self, ename, eng):
        waited = {}
        for o in self.ops[ename]:
            best = {}
            for d in o.deps:
                sem, val = d.signal
                k = id(sem)
                if waited.get(k, 0) < val:
                    waited[k] = val
                    best[k] = (sem, val)
            ws = list(best.values())
            for sem, val in ws[1:]:
                eng.wait_ge(sem, val)
            ins = o.fn(eng)
            if ws:
                ins._wait_ge(ws[0][0], ws[0][1])
            if o.dma:
                ins.then_inc(o.signal[0], 16)
            elif o.sig:
                ins.then_inc(o.signal[0], 1)

    def run_block(self, final_waits=()):
        self.finalize()
        nc = self.nc
        sch = self
        with nc.Block() as block:
            @block.tensor
            def _(e):
                sch.emit("pe", e)

            @block.scalar
            def _(e):
                sch.emit("act", e)

            @block.vector
            def _(e):
                sch.emit("dve", e)

            @block.gpsimd
            def _(e):
                sch.emit("pool", e)

            @block.sync
            def _(e):
                sch.emit("sp", e)
                best = {}
                for o in final_waits:
                    sem, val = o.signal
                    if id(sem) not in best or best[id(sem)][1] < val:
                        best[id(sem)] = (sem, val)
                for sem, val in best.values():
                    e.wait_ge(sem, val)


class Rot:
    def __init__(self, items):
        self.items = items
        self.i = 0

    def next(self):
        it = self.items[self.i % len(self.items)]
        self.i += 1
        return it


GF = 6
GROUPS = [(0, 6), (6, 6), (12, 6), (18, 4)]
GAMMA_LOG = [float(np.log(np.float32(1.0) - np.float32(2.0) ** np.float32(-5.0 - h))) for h in range(6)]
E30 = float(np.exp(30.0))
PI = float(np.pi)


def build(n_layers=NL, do_mix=True, do_ffn=True, kinds=None):
    nc = bass.Bass("TRN2", target_bir_lowering=False)

    def din(name, shape, dt=F32):
        return nc.dram_tensor(name, shape, dt, kind="ExternalInput").ap()

    x_d = din("x", [T, D])
    mem_d = din("mem", [NMEM, D])
    pos_d = din("positions", [16, 128], I32)
    norm_mix_d = din("norm_mix", [NL, D])
    w_in_d = din("w_in", [NL, D, INW])
    w_out_d = din("w_out", [NL, D, D])
    norm_mem_d = din("norm_mem", [NL, D])
    w_kv_d = din("w_mem_kv", [NL, D, 512])
    lb_d = din("hgrn_lb_logits", [2, MIXW])
    hgn_d = din("hgrn_out_norm", [2, MIXW])
    rtn_d = din("ret_out_norm", [2, MIXW])
    norm_ffn_d = din("norm_ffn", [NL, D])
    w_fi_d = din("w_ffn_in", [NL, D, 2 * DFF])
    w_fo_d = din("w_ffn_out", [NL, DFF, D])
    norm_fin_d = din("norm_final", [1, D])
    consts_d = din("consts", [128, 1024])
    out_d = nc.dram_tensor("out", [T, D], F32, kind="ExternalOutput").ap()

    es = ExitStack()

    def sb(name, shape, dt):
        return es.enter_context(nc.sbuf_tensor(name, shape, dt))

    S = Sched(nc)

    xT = sb("xT", [128, KC, T], F32)
    XB = [Buf("x%d" % i) for i in range(16)]
    U1 = sb("U1", [128, 34816], BF16)
    U2W = 12320
    U2 = sb("U2", [128, U2W], F32)
    U2b = U2[:].bitcast(BF16)
    cst = sb("cst", [128, 1024], F32)
    identF = cst[:, 0:128]
    M1c, M2c, M4c, BDc, CAUc = (cst[:, 128 * i:128 * (i + 1)] for i in range(1, 6))
    sqc = cst[:, 768:774]
    skc = cst[:, 774:780]
    invf = cst[:, 832:896]
    identB = sb("identB", [128, 128], BF16)
    onesB = sb("onesB", [128, 128], BF16)
    vcol = sb("vcol", [128, 128], F32)
    KIND = sb("KIND", [128, 2048], F32)
    KINDB = Buf("KIND")
    xatt = sb("xatt", [128, 2304], BF16)
    KTpad = xatt[:, 0:1024].rearrange("p (h m) -> p h m", h=4)
    Vpad = xatt[:, 1024:2048].rearrange("p (t h c) -> p t h c", t=2, h=4)
    onespad = xatt[:, 2048:2304].rearrange("p (a c) -> p a c", a=2)
    XAB = Buf("xatt")
    OPB = Buf("onespad")
    CB = Buf("consts")
    VC = Buf("vcol")
    sqt = sb("sqt", [128, 2, 512], BF16)
    sq_rot = Rot([(sqt[:, i, :], Buf("sq%d" % i)) for i in range(2)])
    rstd_t = sb("rstd", [128, 2, 512], F32)
    rstd_rot = Rot([(rstd_t[:, i, :], Buf("rstd%d" % i)) for i in range(2)])

    PSA = es.enter_context(nc.psum_tensor("psa", [128, 4096], F32))
    PSAb = PSA[:].bitcast(BF16)
    PB = [Buf("ps%d" % i) for i in range(8)]
    ps_all = Rot([(PSA[:, 512 * i:512 * (i + 1)], PB[i]) for i in range(8)])

    w_in_sb = U1[:, 0:KC * INW].rearrange("p (k n) -> p k n", k=KC)
    w_out_sb = U1[:, KC * INW:KC * INW + KC * D].rearrange("p (k n) -> p k n", k=KC)
    WIN_B = [Buf("win%d" % i) for i in range(4)]
    WOUT_B = Buf("wout")
    hT = U1[:, 0:KC * T].rearrange("p (k t) -> p k t", k=KC)
    HB = [Buf("h%d" % i) for i in range(4)]
    aT = U1[:, KC * T:KC * T + GF * T].rearrange("p (f t) -> p f t", f=GF)
    AB = [[Buf("a%d_%d" % (f, b)) for b in range(4)] for f in range(GF)]
    wo_sb = U2b[:, 0:GF * D].rearrange("p (f n) -> p f n", f=GF)
    WO_B = Buf("wo")
    NWI = 3
    wi_sb = [U2b[:, GF * D + i * 2048:GF * D + (i + 1) * 2048].rearrange("p (g k n) -> p g k n", g=2, k=KC) for i in range(NWI)]
    wi_rot = Rot([(wi_sb[i], (Buf("wig%d" % i), Buf("wiu%d" % i))) for i in range(NWI)])
    st0 = GF * D + NWI * 2048
    silu_rot = Rot([(U2b[:, st0 + i * 512:st0 + (i + 1) * 512], Buf("sl%d" % i)) for i in range(4)])
    stage_rot = Rot([(U2[:, i * 1024:(i + 1) * 1024], Buf("stg%d" % i)) for i in range(2)])
    yT_rot = Rot([(U2[:, 2048 + i * 1024:2048 + (i + 1) * 1024].rearrange("p (k t) -> p k t", k=KC), Buf("yT%d" % i)) for i in range(2)])
    vrows = U2[:, 4096:4224]
    VR = Buf("vrows")

    _o = [0]

    def u2f(n):
        a = U2[:, _o[0]:_o[0] + n]
        _o[0] += n
        return a

    def u2b(n):
        a = U2b[:, 2 * _o[0]:2 * _o[0] + n]
        _o[0] += (n + 1) // 2
        return a
    hTt = u2b(1024).rearrange("p (k t) -> p k t", k=KC); HTB = Buf("hTt")
    catTs = [u2b(1024).rearrange("p (k t) -> p k t", k=KC) for _ in range(2)]
    CATBs = [Buf("catT0"), Buf("catT1")]
    tA = u2f(768); TAB = Buf("tA")
    tB = u2f(768); TBB = Buf("tB")
    tC = u2f(768); TCB = Buf("tC")
    tE = u2f(768); TEB = Buf("tE")
    tO = u2f(768); TOB = Buf("tO")
    tG = u2f(768); TGB = Buf("tG")
    tR = u2f(768); TRB = Buf("tR")
    Qt = u2b(768); QTB_ = Buf("Qt")
    Kt = u2b(768); KTB_ = Buf("Kt")
    Qot = u2b(768); QOTB_ = Buf("Qot")
    Kst = u2b(768); KSTB = Buf("Kst")
    Vt = u2b(768); VTB = Buf("Vt")
    QTf = u2b(768); QTFB = Buf("QTf")
    KTf = u2b(768); KTFB = Buf("KTf")
    QoTf = u2b(768); QOTFB = Buf("QoTf")
    Pm = u2b(768); PMB = Buf("Pm")
    sqo = Pm; SQOB = PMB
    S32H = [Buf("S32h%d" % h) for h in range(6)]
    SBFH = [Buf("Sbfh%d" % h) for h in range(6)]
    S32 = u2f(768); S32B = Buf("S32", kids=S32H)
    Sbf = u2b(768); SBFB = Buf("Sbf", kids=SBFH)
    dec = u2f(24); DECB = Buf("dec")
    qxT = Pm[:, 0:256]; QXB = PMB
    expT = u2b(1024); EXB = Buf("expT")
    rsx = u2f(256); RSXB = Buf("rsx")
    setup_base = _o[0]
    assert _o[0] <= U2W, _o[0]
    memst = [U2[:, 2048 + i * 1024:2048 + (i + 1) * 1024] for i in range(2)]
    MSB = [Buf("memst%d" % i) for i in range(2)]
    memh = U2b[:, 2 * 4096:2 * 4096 + 2048].rearrange("p (t n) -> p t n", t=2)
    MHB = Buf("memh")
    memhT = U2b[:, 2 * 5120:2 * 5120 + 2048].rearrange("p (k m) -> p k m", k=KC)
    MHTB = Buf("memhT")
    wkv = U2b[:, 2 * 6144:2 * 6144 + 4096].rearrange("p (k n) -> p k n", k=KC)
    WKVB = Buf("wkv")
    mscr = U2[:, 8192:8192 + 1024]
    MSCB = Buf("mscr")
    angt = U2[:, 9216:9216 + 1024].rearrange("p (t j) -> p t j", t=16)
    ANGB = Buf("ang")
    posr = U2[:, 10240:10240 + 128]
    posi = U2[:, 10368:10368 + 128].bitcast(I32)
    posf = U2[:, 10496:10496 + 16]
    POSB = Buf("pos")
    assert 10512 <= U2W

    R0 = PSA[:, 0:1024]; R0B = Buf("R0")
    R1 = PSA[:, 1024:2048]; R1B = Buf("R1")
    R2H = [Buf("R2h%d" % h) for h in range(6)]
    R3H = [Buf("R3h%d" % h) for h in range(6)]
    R2 = PSA[:, 2048:3072]; R2B = Buf("R2", kids=R2H)
    R3 = PSA[:, 3072:4096]; R3B = Buf("R3", kids=R3H)
    R0b = PSAb[:, 0:2048]; R1b = PSAb[:, 2048:4096]; R2b = PSAb[:, 4096:6144]; R3b = PSAb[:, 6144:8192]
    ALLPS = PB

    def rbufs(*rb):
        return list(rb)

    outs = []

    S.dma("sp", lambda e: e.dma_start(out=cst[:], in_=consts_d), writes=[CB])
    S.op("dve", lambda e: e.tensor_copy(out=identB[:], in_=identF), reads=[CB], writes=[CB])
    S.op("pool", lambda e: e.memset(onesB[:], 1.0), writes=[CB])
    S.op("pool", lambda e: e.memset(xatt[:], 0.0), writes=[XAB, OPB])
    S.op("pool", lambda e: e.memset(onespad[:, 0, 0:64], 1.0), writes=[OPB])
    S.op("pool", lambda e: e.memset(onespad[:, 1, 64:128], 1.0), writes=[OPB])
    S.op("pool", lambda e: e.memset(vrows, 0.0), writes=[VR])
    for i, (src, r0, n) in enumerate([(norm_mix_d, 0, 32), (norm_ffn_d, 32, 32), (norm_mem_d, 64, 32), (norm_fin_d, 96, 8), (hgn_d, 104, 12), (rtn_d, 116, 12)]):
        S.dma("sp", lambda e, src=src, r0=r0, n=n: e.dma_start(out=vrows[r0:r0 + n, :], in_=src.rearrange("l (k p) -> (l k) p", p=128)),
              writes=[VR], semkey="vr")
    S.op("pe", lambda e: e.transpose(out=PSA[:, 0:128], in_=vrows, identity=identF), reads=[VR, CB], writes=[PB[0]])
    S.op("dve", lambda e: e.tensor_copy(out=vcol[:], in_=PSA[:, 0:128]), reads=[PB[0]], writes=[VC])

    def col_norm_mix(l, k): return vcol[:, l * 8 + k:l * 8 + k + 1]
    def col_norm_ffn(l, k): return vcol[:, 32 + l * 8 + k:32 + l * 8 + k + 1]
    def col_norm_mem(l, k): return vcol[:, 64 + l * 8 + k:64 + l * 8 + k + 1]
    def col_norm_fin(k): return vcol[:, 96 + k:96 + k + 1]
    def col_hgn(j, h): return vcol[:, 104 + j * 6 + h:104 + j * 6 + h + 1]
    def col_rtn(j, h): return vcol[:, 116 + j * 6 + h:116 + j * 6 + h + 1]

    S.barrier()

    def load_transposed(src_d, ntile, dst, dst_bufs):
        for t in range(ntile):
            stg, stgb = stage_rot.next()
            S.dma("sp", lambda e, stg=stg, t=t: e.dma_start(out=stg, in_=src_d[t * 128:(t + 1) * 128, :]), writes=[stgb])
            for half in range(2):
                ps, pb = ps_all.next()
                for j in range(4):
                    k = half * 4 + j
                    S.op("pe", lambda e, ps=ps, stg=stg, j=j, k=k: e.transpose(out=ps[:, j * 128:(j + 1) * 128], in_=stg[:, k * 128:(k + 1) * 128], identity=identF),
                         reads=[stgb, CB], writes=[pb])
                if half == 0:
                    S.op("act", lambda e, ps=ps, half=half, t=t: e.copy(out=dst[:, half * 4:half * 4 + 4, t * 128:(t + 1) * 128],
                                                                       in_=ps.rearrange("p (k c) -> p k c", k=4)), reads=[pb], writes=[dst_bufs[t]])
                else:
                    S.op("dve", lambda e, ps=ps, half=half, t=t: e.tensor_copy(out=dst[:, half * 4:half * 4 + 4, t * 128:(t + 1) * 128],
                                                                              in_=ps.rearrange("p (k c) -> p k c", k=4)), reads=[pb], writes=[dst_bufs[t]])

    def rms_stats_blk(src_fn, src_bufs, width, scale, psreg=None):
        ps, pb = psreg if psreg is not None else ps_all.next()
        for k in range(KC):
            sq, sqb = sq_rot.next()
            S.op("act", lambda e, sq=sq, k=k: e.activation(out=sq[:, 0:width], in_=src_fn(k), func=AF.Square), reads=src_bufs, writes=[sqb])
            S.op("pe", lambda e, ps=ps, sq=sq, k=k: e.matmul(ps[:, 0:width], lhsT=onesB[:], rhs=sq[:, 0:width], start=(k == 0), stop=(k == KC - 1)),
                 reads=[sqb, CB], writes=[pb])
        rs, rsb = rstd_rot.next()
        S.op("act", lambda e, ps=ps, rs=rs: e.activation(out=rs[:, 0:width], in_=ps[:, 0:width], func=AF.Ln, scale=scale, bias=EPS), reads=[pb], writes=[rsb])
        S.op("act", lambda e, rs=rs: e.activation(out=rs[:, 0:width], in_=rs[:, 0:width], func=AF.Exp, scale=-0.5), reads=[rsb], writes=[rsb])
        return rs, rsb

    def mm(out, lhsT, rhs, start, stop, reads, writes, **kw):
        S.op("pe", lambda e: e.matmul(out, lhsT=lhsT, rhs=rhs, start=start, stop=stop, **kw), reads=reads, writes=writes)

    def act(out, in_, func, reads, writes, **kw):
        S.op("act", lambda e: e.activation(out=out, in_=in_, func=func, **kw), reads=reads, writes=writes)

    def hv(ap):
        return ap.rearrange("p (h c) -> p h c", h=6)

    weights_issued = set()
    prefetched = set()

    def issue_mixer_weights(l):
        if l in weights_issued:
            return
        weights_issued.add(l)
        S.dma("pool", lambda e: e.dma_start(out=wkv, in_=w_kv_d[l].rearrange("(k p) n -> p k n", p=128)), writes=[WKVB])
        for i in range(4):
            if (l, i) in prefetched:
                continue
            S.dma("pool", lambda e, i=i: e.dma_start(out=w_in_sb[:, 2 * i:2 * i + 2, :],
                                                    in_=w_in_d[l, 256 * i:256 * (i + 1), :].rearrange("(k p) n -> p k n", p=128)), writes=[WIN_B[i]])
        S.dma("pool", lambda e: e.dma_start(out=w_out_sb, in_=w_out_d[l].rearrange("(k p) n -> p k n", p=128)), writes=[WOUT_B])

    def mixer_layer(l, kind, j):
        issue_mixer_weights(l)
        if kind == "ret":
            S.dma("sp", lambda e: e.dma_start(out=posi[0:16, :], in_=pos_d), writes=[POSB])
            S.op("dve", lambda e: e.tensor_copy(out=posr[0:16, :], in_=posi[0:16, :]), reads=[POSB], writes=[POSB])
            S.op("pe", lambda e: e.transpose(out=R3[:, 0:16], in_=posr[0:16, :], identity=identF[0:16, 0:16]), reads=[POSB, CB], writes=[R3B])
            S.op("dve", lambda e: e.tensor_copy(out=posf, in_=R3[:, 0:16]), reads=[R3B], writes=[POSB])
            for t in range(16):
                S.op("dve", lambda e, t=t: e.tensor_scalar(out=angt[:, t, :], in0=invf, scalar1=posf[:, t:t + 1], scalar2=None, op0=ALU.mult), reads=[POSB, CB], writes=[ANGB])
            angf = angt.rearrange("p t j -> p (t j)")
            scr2 = U2[:, 1024:2048]
            scr2i = scr2.bitcast(I32)
            for (off, dst0) in ((0.5 * PI, 0), (0.0, 1024)):
                S.op("dve", lambda e, off=off: e.tensor_scalar(out=mscr, in0=angf, scalar1=off, scalar2=None, op0=ALU.add), reads=[ANGB], writes=[MSCB])
                S.op("dve", lambda e: e.tensor_scalar(out=scr2, in0=mscr, scalar1=1.0 / (2 * PI), scalar2=None, op0=ALU.mult), reads=[MSCB], writes=[TAB, TBB])
                S.op("dve", lambda e: e.tensor_copy(out=scr2i, in_=scr2), reads=[TAB, TBB], writes=[TAB, TBB])
                S.op("dve", lambda e: e.tensor_copy(out=scr2, in_=scr2i), reads=[TAB, TBB], writes=[TAB, TBB])
                S.op("dve", lambda e: e.scalar_tensor_tensor(out=mscr, in0=scr2, scalar=-2 * PI, in1=mscr, op0=ALU.mult, op1=ALU.add), reads=[TAB, TBB, MSCB], writes=[MSCB])
                S.op("dve", lambda e: e.tensor_scalar(out=scr2, in0=mscr, scalar1=PI, scalar2=2 * PI, op0=ALU.is_gt, op1=ALU.mult), reads=[MSCB], writes=[TAB, TBB])
                S.op("dve", lambda e: e.tensor_tensor(out=mscr, in0=mscr, in1=scr2, op=ALU.subtract), reads=[TAB, TBB, MSCB], writes=[MSCB])
                S.op("dve", lambda e: e.tensor_scalar(out=mscr, in0=mscr, scalar1=-PI, scalar2=PI, op0=ALU.max, op1=ALU.min), reads=[MSCB], writes=[MSCB])
                act(KIND[:, dst0:dst0 + 1024], mscr, AF.Sin, [MSCB], [KINDB])
        elif kind == "hgrn":
            lb_bc = KIND[:, 0:768]
            nom_bc = KIND[:, 768:1536]
            if j == 0:
                S.op("pool", lambda e: e.memset(lb_bc, 0.0), writes=[KINDB])
                S.op("pool", lambda e: e.memset(nom_bc, -1.0), writes=[KINDB])
            else:
                S.dma("sp", lambda e: e.dma_start(out=mscr[:, 0:768], in_=lb_d[0:1, :].partition_broadcast(128)), writes=[MSCB])
                S.dma("sp", lambda e: e.dma_start(out=angt.rearrange("p t j -> p (t j)")[:, 0:768], in_=lb_d[1:2, :].partition_broadcast(128)), writes=[ANGB])
                S.op("dve", lambda e: e.tensor_tensor(out=mscr[:, 0:768], in0=mscr[:, 0:768], in1=angt.rearrange("p t j -> p (t j)")[:, 0:768], op=ALU.subtract),
                     reads=[MSCB, ANGB], writes=[MSCB])
                act(mscr[:, 0:768], mscr[:, 0:768], AF.Exp, [MSCB], [MSCB])
                S.op("dve", lambda e: e.tensor_scalar(out=mscr[:, 0:768], in0=mscr[:, 0:768], scalar1=1.0, scalar2=None, op0=ALU.add), reads=[MSCB], writes=[MSCB])
                S.op("dve", lambda e: e.reciprocal(out=lb_bc, in_=mscr[:, 0:768]), reads=[MSCB], writes=[KINDB])
                S.op("dve", lambda e: e.tensor_scalar(out=nom_bc, in0=lb_bc, scalar1=-1.0, scalar2=None, op0=ALU.add), reads=[KINDB], writes=[KINDB])
        BIS = 99
        angf_ = angt.rearrange("p t j -> p (t j)")
        if BIS >= 1:
            S.dma("sp", lambda e: e.dma_start(out=angf_[0:1, :], in_=norm_mem_d[l:l + 1, :]), writes=[ANGB])
            for hh in range(2):
                S.op("pe", lambda e, hh=hh: e.matmul(R3[:, hh * 512:(hh + 1) * 512], lhsT=cst[0:1, 640:768], rhs=angf_[0:1, hh * 512:(hh + 1) * 512],
                                                   start=True, stop=True), reads=[ANGB, CB], writes=[R3B])
            S.op("dve", lambda e: e.tensor_copy(out=mscr, in_=R3), reads=[R3B], writes=[MSCB])
        for mt in range(2 if BIS >= 2 else 0):
            S.dma("sp", lambda e, mt=mt: e.dma_start(out=memst[mt], in_=mem_d[mt * 128:(mt + 1) * 128, :]), writes=[MSB[mt]])
            S.op("pool", lambda e, mt=mt: e.memset(posf[:, mt:mt + 1], 0.0), writes=[POSB])
            act(angf_, memst[mt], AF.Square, [MSB[mt], POSB], [ANGB, POSB], accum_out=posf[:, mt:mt + 1])
            act(posf[:, mt:mt + 1], posf[:, mt:mt + 1], AF.Sqrt, [POSB], [POSB], scale=1.0 / D, bias=EPS)
            S.op("dve", lambda e, mt=mt: e.reciprocal(out=posf[:, mt:mt + 1], in_=posf[:, mt:mt + 1]), reads=[POSB], writes=[POSB])
            S.op("dve", lambda e, mt=mt: e.scalar_tensor_tensor(out=memh[:, mt, :], in0=memst[mt], scalar=posf[:, mt:mt + 1], in1=mscr,
                                                               op0=ALU.mult, op1=ALU.mult), reads=[MSB[mt], POSB, MSCB], writes=[MHB])
            if BIS >= 3:
                for k in range(KC):
                    S.op("pe", lambda e, mt=mt, k=k: e.transpose(out=R0b[:, mt * 1024 + k * 128:mt * 1024 + (k + 1) * 128], in_=memh[:, mt, k * 128:(k + 1) * 128],
                                                                identity=identB[:]), reads=[MHB, CB], writes=[R0B])
                S.op("act", lambda e, mt=mt: e.copy(out=memhT[:, :, mt * 128:(mt + 1) * 128], in_=R0b[:, mt * 1024:(mt + 1) * 1024].rearrange("p (k c) -> p k c", k=KC)),
                     reads=[R0B], writes=[MHTB])
        for c in range(2 if BIS >= 4 else 0):
            for k in range(KC):
                mm(R1[:, c * 256:(c + 1) * 256], wkv[:, k, c * 128:(c + 1) * 128], memhT[:, k, :], k == 0, k == KC - 1, [WKVB, MHTB], [R1B])
        if BIS >= 4:
            for h in range(4):
                c, po = h // 2, 64 * (h % 2)
                S.op("act", lambda e, h=h, c=c, po=po: e.copy(out=KTpad[po:po + 64, h, :], in_=R1[po:po + 64, c * 256:(c + 1) * 256]), reads=[R1B], writes=[XAB])
        for mt in range(2 if BIS >= 5 else 0):
            for k in range(KC):
                mm(R2[:, mt * 256:(mt + 1) * 256], memhT[:, k, mt * 128:(mt + 1) * 128], wkv[:, k, 256:512], k == 0, k == KC - 1, [WKVB, MHTB], [R2B])
        for mt in range(2 if BIS >= 5 else 0):
            for h in range(4):
                S.op("dve", lambda e, mt=mt, h=h: e.tensor_copy(out=Vpad[:, mt, h, (h % 2) * 64:(h % 2) * 64 + 64], in_=R2[:, mt * 256 + h * 64:mt * 256 + (h + 1) * 64]),
                     reads=[R2B], writes=[XAB])
        S.barrier()
        S.op("pool", lambda e: e.memset(S32, 0.0), writes=[S32B])
        S.op("pool", lambda e: e.memset(Sbf, 0.0), writes=[SBFB])

        blk_rs = {}

        def head1(t):
            ts = slice(t * 128, (t + 1) * 128)
            if t % 4 == 0:
                b0 = t
                blk_rs[t // 4] = rms_stats_blk(lambda k, b0=b0: xT[:, k, b0 * 128:(b0 + 4) * 128], XB[b0:b0 + 4], 512, 1.0 / D, psreg=(R3[:, 0:512], R3B))
            rs, rsb = blk_rs[t // 4]
            tt = t % 4
            for k in range(KC):
                S.op("dve", lambda e, k=k, ts=ts, rs=rs, tt=tt: e.scalar_tensor_tensor(out=hTt[:, k, :], in0=xT[:, k, ts], scalar=col_norm_mix(l, k),
                                                                                   in1=rs[:, tt * 128:(tt + 1) * 128], op0=ALU.mult, op1=ALU.mult),
                     reads=[XB[t], rsb, VC], writes=[HTB])
            for c in range(8):
                c0 = 2304 + c * 128
                for k in range(KC):
                    mm(R3[:, c * 128:(c + 1) * 128], w_in_sb[:, k, c0:c0 + 128], hTt[:, k, :], k == 0, k == KC - 1, [WIN_B[k // 2], HTB], [R3B])

        for t in range(16):
            ts = slice(t * 128, (t + 1) * 128)
            if t == 0:
                head1(0)
            if kind == "hgrn":
                act(tG, R3[:, 0:768], AF.Tanh, [R3B], [TGB], scale=0.5)
            else:
                act(tG, R3[:, 0:768], AF.Silu, [R3B], [TGB])
            S.op("act", lambda e: e.copy(out=qxT, in_=R3[:, 768:1024]), reads=[R3B], writes=[QXB])
            for h in range(4):
                c = h // 2
                for mt in range(2):
                    mm(R3[:, (h * 2 + mt) * 128:(h * 2 + mt + 1) * 128], KTpad[:, h, mt * 128:(mt + 1) * 128], qxT[:, c * 128:(c + 1) * 128],
                       True, True, [XAB, QXB], [R3B])
            for (R, RB_, c0) in ((R0, R0B, 0), (R1, R1B, 768), (R2, R2B, 1536)):
                for (a, n) in ((0, 512), (512, 256)):
                    for k in range(KC):
                        mm(R[:, a:a + n], hTt[:, k, :], w_in_sb[:, k, c0 + a:c0 + a + n], k == 0, k == KC - 1, [WIN_B[k // 2], HTB], [RB_])
            if kind == "hgrn":
                act(tC, R0[:, 0:768], AF.Silu, [R0B], [TCB])
            act(expT, R3[:, 0:1024], AF.Exp, [R3B], [EXB], scale=0.125)
            for c in range(2):
                i = 0
                for h in (2 * c, 2 * c + 1):
                    for mt in range(2):
                        mm(R3[:, c * 128:(c + 1) * 128], Vpad[:, mt, h, :], expT[:, (h * 2 + mt) * 128:(h * 2 + mt + 1) * 128], i == 0, i == 3, [XAB, EXB], [R3B])
                        i += 1
            for c in range(2):
                i = 0
                for h in (2 * c, 2 * c + 1):
                    for mt in range(2):
                        mm(R3[:, 256 + c * 128:256 + (c + 1) * 128], onespad[:, h % 2, :], expT[:, (h * 2 + mt) * 128:(h * 2 + mt + 1) * 128], i == 0, i == 3, [OPB, EXB], [R3B])
                        i += 1
            cat, catb = catTs[t % 2], CATBs[t % 2]
            prev = t - 1 if t > 0 else None

            def xa_fin(cat=cat, catb=catb):
                act(rsx, R3[:, 256:512], AF.Ln, [R3B], [RSXB])
                act(rsx, rsx, AF.Exp, [RSXB], [RSXB], scale=-1.0)
                S.op("dve", lambda e, cat=cat: e.tensor_tensor(out=cat[:, 6:8, :].rearrange("p c t -> p (c t)"), in0=R3[:, 0:256],
                                                               in1=rsx, op=ALU.mult), reads=[R3B, RSXB], writes=[catb])
            nxt = (lambda t=t: head1(t + 1)) if t < 15 else (lambda: None)
            if kind == "ret":
                ret_tile(l, j, t, nxt, cat, catb, prev, xa_fin)
            elif kind == "hgrn":
                hgrn_tile(l, j, t, nxt, cat, catb, prev, xa_fin)
            else:
                xa_fin()
                S.op("dve", lambda e, cat=cat: e.memset(cat[:, 0:6, :], 0.0), writes=[catb])
                nxt()
                w_out_mm(t, R0, R0B, range(KC))
                w_out_res(t, R0, R0B)
        if kind in ("ret", "hgrn"):
            w_out_mm(15, R0, R0B, range(KC))
            w_out_res(15, R0, R0B)

    def w_out_mm(tp, R, RB_, dcs):
        cat, catb = catTs[tp % 2], CATBs[tp % 2]
        for dc in dcs:
            for k in range(KC):
                mm(R[:, dc * 128:(dc + 1) * 128], w_out_sb[:, k, dc * 128:(dc + 1) * 128], cat[:, k, :], k == 0, k == KC - 1, [WOUT_B, catb], [RB_])

    def w_out_res(tp, R, RB_):
        ts = slice(tp * 128, (tp + 1) * 128)
        S.op("dve", lambda e, ts=ts, R=R: e.tensor_tensor(out=xT[:, :, ts], in0=R.rearrange("p (k c) -> p k c", k=KC), in1=xT[:, :, ts], op=ALU.add),
             reads=[RB_, XB[tp]], writes=[XB[tp]])

    def ret_tile(l, j, t, nxt, cat, catb, prev, xa_fin):
        cosb = KIND[:, t * 64:(t + 1) * 64].unsqueeze(1).to_broadcast([128, 6, 64])
        sinb = KIND[:, 1024 + t * 64:1024 + (t + 1) * 64].unsqueeze(1).to_broadcast([128, 6, 64])
        for (R, RB_, sc, dstT, DSTB) in ((R0, R0B, sqc, Qt, QTB_), (R1, R1B, skc, Kt, KTB_)):
            S.op("dve", lambda e, R=R, sc=sc: e.tensor_tensor(out=hv(tA), in0=hv(R[:, 0:768]), in1=sc.unsqueeze(2).to_broadcast([128, 6, 128]), op=ALU.mult),
                 reads=[RB_, CB], writes=[TAB])
            if R is R0 and prev is not None:
                w_out_mm(prev, R0, R0B, range(KC))
            a4 = tA.rearrange("p (h two d) -> p h two d", h=6, two=2)
            c4 = tC.rearrange("p (h two d) -> p h two d", h=6, two=2)
            e4 = tE.rearrange("p (h two d) -> p h two d", h=6, two=2)
            d4 = dstT.rearrange("p (h two d) -> p h two d", h=6, two=2)
            S.op("dve", lambda e, a4=a4, c4=c4: e.tensor_tensor(out=c4[:, :, 0, :], in0=a4[:, :, 0, :], in1=cosb, op=ALU.mult), reads=[TAB, KINDB], writes=[TCB])
            S.op("dve", lambda e, a4=a4, c4=c4: e.tensor_tensor(out=c4[:, :, 1, :], in0=a4[:, :, 0, :], in1=sinb, op=ALU.mult), reads=[TAB, KINDB], writes=[TCB])
            S.op("dve", lambda e, a4=a4, e4=e4: e.tensor_tensor(out=e4[:, :, 0, :], in0=a4[:, :, 1, :], in1=sinb, op=ALU.mult), reads=[TAB, KINDB], writes=[TEB])
            S.op("dve", lambda e, a4=a4, e4=e4: e.tensor_tensor(out=e4[:, :, 1, :], in0=a4[:, :, 1, :], in1=cosb, op=ALU.mult), reads=[TAB, KINDB], writes=[TEB])
            S.op("dve", lambda e, c4=c4, e4=e4, d4=d4: e.tensor_tensor(out=d4[:, :, 0, :], in0=c4[:, :, 0, :], in1=e4[:, :, 0, :], op=ALU.subtract), reads=[TCB, TEB], writes=[DSTB])
            S.op("dve", lambda e, c4=c4, e4=e4, d4=d4: e.tensor_tensor(out=d4[:, :, 1, :], in0=c4[:, :, 1, :], in1=e4[:, :, 1, :], op=ALU.add), reads=[TCB, TEB], writes=[DSTB])
        S.op("act", lambda e: e.copy(out=Vt, in_=R2[:, 0:768]), reads=[R2B], writes=[VTB])
        xa_fin()
        if prev is not None:
            w_out_res(prev, R0, R0B)
        for h in range(6):
            hs = slice(h * 128, (h + 1) * 128)
            S.op("pe", lambda e, hs=hs: e.transpose(out=R0b[:, hs], in_=Qt[:, hs], identity=identB[:]), reads=[QTB_, CB], writes=[R0B])
            S.op("pe", lambda e, hs=hs: e.transpose(out=R1b[:, hs], in_=Kt[:, hs], identity=identB[:]), reads=[KTB_, CB], writes=[R1B])
        S.op("act", lambda e: e.copy(out=QTf, in_=R0b[:, 0:768]), reads=[R0B], writes=[QTFB])
        S.op("dve", lambda e: e.tensor_copy(out=KTf, in_=R1b[:, 0:768]), reads=[R1B], writes=[KTFB])
        for h in range(6):
            hs = slice(h * 128, (h + 1) * 128)
            mm(R2[:, hs], KTf[:, hs], QTf[:, hs], True, True, [KTFB, QTFB], [R2B])
        for h in range(6):
            hs = slice(h * 128, (h + 1) * 128)
            ginv = float(np.exp(-128.0 * GAMMA_LOG[h]))
            S.op("dve", lambda e, hs=hs, ginv=ginv: e.scalar_tensor_tensor(out=Pm[:, hs], in0=R2[:, hs], scalar=ginv, in1=CAUc, op0=ALU.mult, op1=ALU.mult),
                 reads=[R2B, CB], writes=[PMB])
        for h in range(6):
            hs = slice(h * 128, (h + 1) * 128)
            mm(R0[:, hs], Vt[:, hs], Pm[:, hs], True, True, [VTB, PMB], [R0B])
        for h in range(6):
            hs = slice(h * 128, (h + 1) * 128)
            mm(R1[:, hs], Sbf[:, hs], QTf[:, hs], True, True, [SBFB, QTFB], [R1B])
        for h in range(6):
            hs = slice(h * 128, (h + 1) * 128)
            mm(R3[:, hs], Kt[:, hs], Vt[:, hs], True, True, [KTB_, VTB], [R3B])
        S.op("act", lambda e: e.copy(out=tO, in_=R0[:, 0:768]), reads=[R0B], writes=[TOB])
        S.op("dve", lambda e: e.tensor_tensor(out=tO, in0=R1[:, 0:768], in1=tO, op=ALU.add), reads=[R1B, TOB], writes=[TOB])
        act(sqo, tO, AF.Square, [TOB], [SQOB])
        for h in range(6):
            hs = slice(h * 128, (h + 1) * 128)
            mm(R2[:, hs], onesB[:], sqo[:, hs], True, True, [SQOB, CB], [R2B])
        for h in range(6):
            hs = slice(h * 128, (h + 1) * 128)
            ch = float(np.exp(128.0 * GAMMA_LOG[h]))
            S.op("dve", lambda e, hs=hs, ch=ch: e.scalar_tensor_tensor(out=S32[:, hs], in0=S32[:, hs], scalar=ch, in1=R3[:, hs], op0=ALU.mult, op1=ALU.add),
                 reads=[S32B, R3B], writes=[S32B])
        act(tR, R2[:, 0:768], AF.Ln, [R2B], [TRB], scale=1.0 / 128, bias=EPS)
        act(tR, tR, AF.Exp, [TRB], [TRB], scale=-0.5)
        S.op("act", lambda e: e.copy(out=Sbf, in_=S32), reads=[S32B], writes=[SBFB])
        boundary = (t % 4 == 3)
        if not boundary:
            nxt()
        for h in range(6):
            hs = slice(h * 128, (h + 1) * 128)
            S.op("dve", lambda e, hs=hs, h=h: e.scalar_tensor_tensor(out=tC[:, hs], in0=tO[:, hs], scalar=col_rtn(j, h), in1=tR[:, hs], op0=ALU.mult, op1=ALU.mult),
                 reads=[TOB, TRB, VC], writes=[TCB])
        S.op("dve", lambda e, cat=cat: e.tensor_tensor(out=cat[:, 0:6, :], in0=hv(tC), in1=hv(tG), op=ALU.mult), reads=[TCB, TGB], writes=[catb])
        if boundary:
            nxt()

    def hgrn_tile(l, j, t, nxt, cat, catb, prev, xa_fin):
        lb_bc = KIND[:, 0:768]
        nom_bc = KIND[:, 768:1536]
        act(tA, R1[:, 0:768], AF.Exp, [R1B], [TAB], scale=-1.0)
        S.op("dve", lambda e: e.scalar_tensor_tensor(out=tB, in0=tA, scalar=E30, in1=lb_bc, op0=ALU.min, op1=ALU.mult), reads=[TAB, KINDB], writes=[TBB])
        act(tB, tB, AF.Ln, [TBB], [TBB], bias=1.0)
        act(tA, tA, AF.Ln, [TAB], [TAB], bias=1.0)
        S.op("dve", lambda e: e.tensor_tensor(out=tB, in0=tB, in1=tA, op=ALU.subtract), reads=[TBB, TAB], writes=[TBB])
        act(tA, tA, AF.Exp, [TAB], [TAB], scale=-1.0)
        S.op("dve", lambda e: e.scalar_tensor_tensor(out=tA, in0=tA, scalar=1.0, in1=nom_bc, op0=ALU.subtract, op1=ALU.mult), reads=[TAB, KINDB], writes=[TAB])
        S.op("act", lambda e: e.copy(out=Vt, in_=R2[:, 0:768]), reads=[R2B], writes=[VTB])
        xa_fin()
        for (R, RB_, M) in ((R0, R0B, M1c), (R1, R1B, M2c), (R2, R2B, M4c)):
            for (a, n) in ((0, 512), (512, 256)):
                mm(R[:, a:a + n], M, tB[:, a:a + n], True, True, [CB, TBB], [RB_])
        act(tE, R0[:, 0:768], AF.Exp, [R0B], [TEB])
        S.op("dve", lambda e: e.tensor_tensor(out=Qt, in0=tC, in1=tE, op=ALU.mult), reads=[TCB, TEB], writes=[QTB_])
        act(tB, R0[:, 0:768], AF.Exp, [R0B], [TBB], scale=-1.0)
        S.op("dve", lambda e: e.tensor_tensor(out=Kt, in0=tA, in1=tB, op=ALU.mult), reads=[TAB, TBB], writes=[KTB_])
        act(tE, R1[:, 0:768], AF.Exp, [R1B], [TEB])
        S.op("dve", lambda e: e.tensor_tensor(out=Qot, in0=tC, in1=tE, op=ALU.mult), reads=[TCB, TEB], writes=[QOTB_])
        for h in range(6):
            hs = slice(h * 128, (h + 1) * 128)
            S.op("pe", lambda e, hs=hs: e.transpose(out=R0[:, hs], in_=tE[:, hs], identity=identF), reads=[TEB, CB], writes=[R0B])
        S.op("dve", lambda e: e.tensor_copy(out=dec.rearrange("p (h n) -> p h n", h=6),
                                            in_=R0[:, 0:768].rearrange("p (h n c) -> p h n c", h=6, n=4)[:, :, :, 31]), reads=[R0B], writes=[DECB])
        act(tB, R2[:, 0:768], AF.Exp, [R2B], [TBB])
        S.op("dve", lambda e: e.tensor_tensor(out=Kst, in0=tA, in1=tB, op=ALU.mult), reads=[TAB, TBB], writes=[KSTB])
        for h in range(6):
            hs = slice(h * 128, (h + 1) * 128)
            S.op("pe", lambda e, hs=hs: e.transpose(out=R1b[:, hs], in_=Qt[:, hs], identity=identB[:]), reads=[QTB_, CB], writes=[R1B])
            S.op("pe", lambda e, hs=hs: e.transpose(out=R2b[:, hs], in_=Kt[:, hs], identity=identB[:]), reads=[KTB_, CB], writes=[R2B])
            S.op("pe", lambda e, hs=hs: e.transpose(out=R3b[:, hs], in_=Qot[:, hs], identity=identB[:]), reads=[QOTB_, CB], writes=[R3B])
        S.op("act", lambda e: e.copy(out=QTf, in_=R1b[:, 0:768]), reads=[R1B], writes=[QTFB])
        S.op("dve", lambda e: e.tensor_copy(out=KTf, in_=R2b[:, 0:768]), reads=[R2B], writes=[KTFB])
        S.op("act", lambda e: e.copy(out=QoTf, in_=R3b[:, 0:768]), reads=[R3B], writes=[QOTFB])
        for h in range(6):
            hs = slice(h * 128, (h + 1) * 128)
            mm(R0[:, hs], KTf[:, hs], QTf[:, hs], True, True, [KTFB, QTFB], [R0B])
        S.op("dve", lambda e: e.tensor_tensor(out=hv(Pm), in0=hv(R0[:, 0:768]), in1=BDc.unsqueeze(1).to_broadcast([128, 6, 128]), op=ALU.mult),
             reads=[R0B, CB], writes=[PMB])
        for h in range(6):
            hs = slice(h * 128, (h + 1) * 128)
            mm(R1[:, hs], Vt[:, hs], Pm[:, hs], True, True, [VTB, PMB], [R1B])
        S.op("act", lambda e: e.copy(out=tO, in_=R1[:, 0:768]), reads=[R1B], writes=[TOB])
        for n in range(4):
            ns = slice(32 * n, 32 * n + 32)
            for h in range(6):
                hs = slice(h * 128, (h + 1) * 128)
                mm(R3[:, hs], Kst[ns, hs], Vt[ns, hs], True, True, [KSTB, VTB], [R3B], tile_position=(32 * n, 0))
            if prev is not None:
                w_out_mm(prev, R0, R0B, (2 * n, 2 * n + 1))
            for h in range(6):
                hs = slice(h * 128, (h + 1) * 128)
                cs = slice(h * 128 + 32 * n, h * 128 + 32 * n + 32)
                mm(R2[:, cs], Sbf[:, hs], QoTf[:, cs], True, True, [SBFB, QOTFB], [R2B])
            for h in range(6):
                hs = slice(h * 128, (h + 1) * 128)
                S.op("dve", lambda e, hs=hs, h=h, n=n: e.scalar_tensor_tensor(out=S32[:, hs], in0=S32[:, hs], scalar=dec[:, h * 4 + n:h * 4 + n + 1], in1=R3[:, hs],
                                                                           op0=ALU.mult, op1=ALU.add), reads=[S32B, R3B, DECB], writes=[S32B])
            S.op("act", lambda e: e.copy(out=Sbf, in_=S32), reads=[S32B], writes=[SBFB])
        S.op("dve", lambda e: e.tensor_tensor(out=tO, in0=R2[:, 0:768], in1=tO, op=ALU.add), reads=[R2B, TOB], writes=[TOB])
        if prev is not None:
            w_out_res(prev, R0, R0B)
        act(sqo, tO, AF.Square, [TOB], [SQOB])
        for h in range(6):
            hs = slice(h * 128, (h + 1) * 128)
            mm(R0[:, 0:128], onesB[:], sqo[:, hs], h == 0, h == 5, [SQOB, CB], [R0B])
        act(tR[:, 0:128], R0[:, 0:128], AF.Ln, [R0B], [TRB], scale=4.0 / MIXW, bias=4.0 * EPS)
        act(tR[:, 0:128], tR[:, 0:128], AF.Exp, [TRB], [TRB], scale=-0.5)
        boundary = (t % 4 == 3)
        if not boundary:
            nxt()
        for h in range(6):
            hs = slice(h * 128, (h + 1) * 128)
            S.op("dve", lambda e, hs=hs, h=h: e.scalar_tensor_tensor(out=tC[:, hs], in0=tO[:, hs], scalar=col_hgn(j, h), in1=tR[:, 0:128], op0=ALU.mult, op1=ALU.mult),
                 reads=[TOB, TRB, VC], writes=[TCB])
        S.op("dve", lambda e, cat=cat: e.scalar_tensor_tensor(out=cat[:, 0:6, :], in0=hv(tG), scalar=1.0, in1=hv(tC),
                                                              op0=ALU.add, op1=ALU.mult), reads=[TGB, TCB], writes=[catb])
        if boundary:
            nxt()

    if do_mix and n_layers > 0:
        issue_mixer_weights(0)
    load_transposed(x_d, 16, xT, XB)

    for l in range(n_layers):
        kind = kinds[l] if kinds else ("hgrn" if l % 2 == 0 else "ret")
        j = l // 2
        if do_mix:
            S.barrier()
            mixer_layer(l, kind, j)
        if do_ffn:
            S.barrier()
            for b in range(4):
                rs, rsb = rms_stats_blk(lambda k, b=b: xT[:, k, b * 512:(b + 1) * 512], XB[4 * b:4 * b + 4], 512, 1.0 / D)
                for k in range(KC):
                    S.op("dve", lambda e, b=b, k=k, l=l, rs=rs: e.scalar_tensor_tensor(
                        out=hT[:, k, b * 512:(b + 1) * 512], in0=xT[:, k, b * 512:(b + 1) * 512], scalar=col_norm_ffn(l, k),
                        in1=rs, op0=ALU.mult, op1=ALU.mult), reads=XB[4 * b:4 * b + 4] + [rsb, VC], writes=[HB[b]])
            for (f0, nf) in GROUPS:
                S.dma("pool", lambda e, f0=f0, nf=nf, l=l: e.dma_start(
                    out=wo_sb[:, 0:nf, :], in_=w_fo_d[l, f0 * 128:(f0 + nf) * 128, :].rearrange("(f p) n -> p f n", p=128)), writes=[WO_B])
                for fi in range(nf):
                    f = f0 + fi
                    wi, wib = wi_rot.next()
                    S.dma("pool", lambda e, wi=wi, f=f, l=l: e.dma_start(
                        out=wi[:, 0], in_=w_fi_d[l, :, f * 128:(f + 1) * 128].rearrange("(k p) n -> p k n", p=128)), writes=[wib[0]])
                    S.dma("pool", lambda e, wi=wi, f=f, l=l: e.dma_start(
                        out=wi[:, 1], in_=w_fi_d[l, :, DFF + f * 128:DFF + (f + 1) * 128].rearrange("(k p) n -> p k n", p=128)), writes=[wib[1]])
                    for b in range(4):
                        pg, pgb = ps_all.next()
                        pu, pub = ps_all.next()
                        for k in range(KC):
                            S.op("pe", lambda e, pg=pg, wi=wi, k=k, b=b: e.matmul(pg, lhsT=wi[:, 0, k, :], rhs=hT[:, k, b * 512:(b + 1) * 512],
                                                                             start=(k == 0), stop=(k == KC - 1)), reads=[wib[0], HB[b]], writes=[pgb])
                        for k in range(KC):
                            S.op("pe", lambda e, pu=pu, wi=wi, k=k, b=b: e.matmul(pu, lhsT=wi[:, 1, k, :], rhs=hT[:, k, b * 512:(b + 1) * 512],
                                                                             start=(k == 0), stop=(k == KC - 1)), reads=[wib[1], HB[b]], writes=[pub])
                        sl, slb = silu_rot.next()
                        S.op("act", lambda e, sl=sl, pg=pg: e.activation(out=sl, in_=pg, func=AF.Silu), reads=[pgb], writes=[slb])
                        S.op("dve", lambda e, sl=sl, pu=pu, fi=fi, b=b: e.tensor_tensor(out=aT[:, fi, b * 512:(b + 1) * 512], in0=pu, in1=sl, op=ALU.mult),
                             reads=[pub, slb], writes=[AB[fi][b]])
                if (f0, nf) == GROUPS[-1] and do_mix and l + 1 < n_layers:
                    for i in range(2):
                        S.dma("pool", lambda e, i=i, l=l: e.dma_start(out=w_in_sb[:, 2 * i:2 * i + 2, :],
                                                                in_=w_in_d[l + 1, 256 * i:256 * (i + 1), :].rearrange("(k p) n -> p k n", p=128)),
                              writes=[WIN_B[i]] + HB)
                        prefetched.add((l + 1, i))
                for b in range(4):
                    for dc in range(KC):
                        py, pyb = ps_all.next()
                        for fi in range(nf):
                            S.op("pe", lambda e, py=py, fi=fi, dc=dc, b=b, nf=nf: e.matmul(py, lhsT=wo_sb[:, fi, dc * 128:(dc + 1) * 128],
                                                                                      rhs=aT[:, fi, b * 512:(b + 1) * 512], start=(fi == 0), stop=(fi == nf - 1)),
                                 reads=[WO_B, AB[fi][b]], writes=[pyb])
                        S.op("dve", lambda e, py=py, dc=dc, b=b: e.tensor_tensor(out=xT[:, dc, b * 512:(b + 1) * 512], in0=py,
                                                                                in1=xT[:, dc, b * 512:(b + 1) * 512], op=ALU.add),
                             reads=[pyb] + XB[4 * b:4 * b + 4], writes=XB[4 * b:4 * b + 4])

    S.barrier()
    for b in range(4):
        rs, rsb = rms_stats_blk(lambda k, b=b: xT[:, k, b * 512:(b + 1) * 512], XB[4 * b:4 * b + 4], 512, 1.0 / D)
        for tt in range(4):
            t = 4 * b + tt
            yT, ytb = yT_rot.next()
            for k in range(KC):
                S.op("dve", lambda e, yT=yT, k=k, t=t, tt=tt, rs=rs: e.scalar_tensor_tensor(
                    out=yT[:, k, :], in0=xT[:, k, t * 128:(t + 1) * 128], scalar=col_norm_fin(k),
                    in1=rs[:, tt * 128:(tt + 1) * 128], op0=ALU.mult, op1=ALU.mult), reads=[XB[t], rsb, VC], writes=[ytb])
            stg, stgb = stage_rot.next()
            for half in range(2):
                ps, pb = ps_all.next()
                for jj in range(4):
                    k = half * 4 + jj
                    S.op("pe", lambda e, ps=ps, yT=yT, jj=jj, k=k: e.transpose(out=ps[:, jj * 128:(jj + 1) * 128], in_=yT[:, k, :], identity=identF),
                         reads=[ytb, CB], writes=[pb])
                if half == 0:
                    S.op("act", lambda e, ps=ps, stg=stg, half=half: e.copy(out=stg[:, half * 512:(half + 1) * 512], in_=ps), reads=[pb], writes=[stgb])
                else:
                    S.op("dve", lambda e, ps=ps, stg=stg, half=half: e.tensor_copy(out=stg[:, half * 512:(half + 1) * 512], in_=ps), reads=[pb], writes=[stgb])
            outs.append(S.dma("sp", lambda e, stg=stg, t=t: e.dma_start(out=out_d[t * 128:(t + 1) * 128, :], in_=stg), reads=[stgb], semkey="out"))

    S.run_block(final_waits=outs)
    S.close()
    es.close()
    return nc


def _consts():
    c = np.zeros((128, 1024), np.float64)
    c[:, 0:128] = np.eye(128)
    s = np.arange(128)[:, None]
    t = np.arange(128)[None, :]
    same = (s // 32) == (t // 32)
    cs, ct = s % 32, t % 32
    c[:, 128:256] = same * ((cs <= ct).astype(np.float64) - (cs <= 15).astype(np.float64))
    c[:, 256:384] = same * (cs <= ct)
    c[:, 384:512] = same * (cs > ct)
    c[:, 512:640] = same * (s <= t)
    c[:, 640:768] = (s <= t)
    p = np.arange(128)
    for h in range(6):
        lg = GAMMA_LOG[h]
        c[:, 768 + h] = np.exp(lg * (p + 1.0))
        c[:, 774 + h] = np.exp(lg * (127.0 - p)) * (128.0 ** -0.5)
    inv = (np.float32(10000.0) ** (-np.linspace(0.0, 1.0, 64, dtype=np.float32))).astype(np.float32)
    c[:, 832:896] = inv[None, :]
    return c.astype(np.float32)


_NC_CACHE = {}


def make_in_maps(inputs, n_cores=8):
    x = np.asarray(inputs["x"], np.float32)
    mem = np.asarray(inputs["mem"], np.float32)
    pos = np.asarray(inputs["positions"], np.int32)
    f = lambda k: np.ascontiguousarray(np.asarray(inputs[k], np.float32))
    shared = {k: f(k) for k in ("norm_mix", "w_in", "w_out", "norm_mem", "w_mem_kv", "hgrn_lb_logits", "hgrn_out_norm",
                                "norm_ffn", "w_ffn_in", "w_ffn_out")}
    shared["ret_out_norm"] = np.ascontiguousarray(np.asarray(inputs["ret_out_norm"], np.float32).reshape(2, MIXW))
    shared["norm_final"] = np.ascontiguousarray(np.asarray(inputs["norm_final"], np.float32).reshape(1, D))
    shared["consts"] = _consts()
    maps = []
    for c in range(n_cores):
        m = dict(shared)
        m["x"] = np.ascontiguousarray(x[c])
        m["mem"] = np.ascontiguousarray(mem[c])
        m["positions"] = np.ascontiguousarray(pos[c].reshape(16, 128))
        maps.append(m)
    return maps


def kernel(**inputs):
    if "full" not in _NC_CACHE:
        _NC_CACHE["full"] = build()
    nc = _NC_CACHE["full"]
    maps = make_in_maps(inputs, 8)
    res = run_bass_kernel_spmd(nc, maps, core_ids=list(range(8)))
    return np.stack([np.asarray(r["out"], np.float32) for r in res.results], axis=0)
```
